# Optimizing a Trainium2 kernel written in Bass

```python
import jax
import jax.numpy as jnp
from jax import lax
import numpy as np

D_MODEL = 1024
BATCH = 4
SEQ = 8192
DEPTH = 4

GRID_W = 64
CTX_LEN = 256
N_MIXERS = 4
BLOCK = 128
WINDOW = 128
WIN_HEADS = 16
WIN_KV = 4
WIN_HD = 64
AX_HEADS = 8
AX_KV = 2
AX_HD = 128
ML_HEADS = 4
ML_DK = 128
ML_DV = 256
ML_CHUNK = 128
D_FF = 2816
ROPE_THETA = 10000.0
EPS = 1e-6
NEG = -1e30

kernel_name = 'hybrid_interleaved_diffusion_block'


def rms_norm(x, g):
    xf = x.astype(jnp.float32)
    y = xf * lax.rsqrt(jnp.mean(xf * xf, axis=-1, keepdims=True) + EPS)
    return (y * g.astype(jnp.float32)).astype(x.dtype)


def dwconv3(x, w):
    xp = jnp.pad(x, ((0, 0), (1, 1), (0, 0)))
    return xp[:, :-2] * w[0] + xp[:, 1:-1] * w[1] + xp[:, 2:] * w[2]


def adaln(cvec, w, b):
    m = (jax.nn.silu(cvec) @ w + b)[..., None, :]
    return jnp.split(m, 6, axis=-1)


def modulate(x, shift, scale):
    return x * (1 + scale) + shift


def axial_rope(n_tok, hd):
    rows = n_tok // GRID_W
    row = jnp.repeat(jnp.arange(rows, dtype=jnp.float32), GRID_W)
    col = jnp.tile(jnp.arange(GRID_W, dtype=jnp.float32), rows)
    n_freq = hd // 4
    inv = jnp.power(ROPE_THETA, -jnp.arange(n_freq, dtype=jnp.float32) / n_freq)
    ang = jnp.concatenate([row[:, None] * inv, col[:, None] * inv], axis=-1)
    return jnp.cos(ang), jnp.sin(ang)


def apply_rope(t, cos, sin):
    t1, t2 = jnp.split(t, 2, axis=-1)
    cos = cos.astype(t.dtype)
    sin = sin.astype(t.dtype)
    return jnp.concatenate([t1 * cos - t2 * sin, t2 * cos + t1 * sin], axis=-1)


def split_heads(t, n_heads, hd):
    b, s, _ = t.shape
    return t.reshape(b, s, n_heads, hd).transpose(0, 2, 1, 3)


def merge_heads(o):
    b, _, _, s, _ = o.shape
    return o.transpose(0, 3, 1, 2, 4).reshape(b, s, -1)


def qkv_heads(a, w_qkv, q_norm, k_norm, n_heads, n_kv, hd):
    b, s, _ = a.shape
    q, k, v = jnp.split(a @ w_qkv, [n_heads * hd, (n_heads + n_kv) * hd], axis=-1)
    q = rms_norm(split_heads(q, n_heads, hd), q_norm).reshape(b, n_kv, n_heads // n_kv, s, hd)
    k = rms_norm(split_heads(k, n_kv, hd), k_norm)
    v = split_heads(v, n_kv, hd)
    return q, k, v


def gqa_softmax(q, k, v, mask, sink):
    s = jnp.einsum('bkgqd,bkjd->bkgqj', q, k, preferred_element_type=jnp.float32) * (q.shape[-1] ** -0.5)
    if mask is not None:
        s = jnp.where(mask, s, NEG)
    if sink is None:
        p = jax.nn.softmax(s, axis=-1)
    else:
        sk = sink.astype(jnp.float32)[None, :, :, None, None]
        m = jnp.maximum(s.max(axis=-1, keepdims=True), sk)
        e = jnp.exp(s - m)
        p = e / (e.sum(axis=-1, keepdims=True) + jnp.exp(sk - m))
    return jnp.einsum('bkgqj,bkjd->bkgqd', p.astype(v.dtype), v)


def window_attention_mixer(a_lat, a_ctx, w_qkv, q_norm, k_norm, sink, w_o, ctx_out):
    b, s, _ = a_lat.shape
    nb = s // BLOCK
    g = WIN_HEADS // WIN_KV
    span = BLOCK + 2 * WINDOW
    q, k, v = qkv_heads(a_lat, w_qkv, q_norm, k_norm, WIN_HEADS, WIN_KV, WIN_HD)
    qc, kc, vc = qkv_heads(a_ctx, w_qkv, q_norm, k_norm, WIN_HEADS, WIN_KV, WIN_HD)
    cos, sin = axial_rope(s, WIN_HD)
    q = apply_rope(q, cos, sin)
    k = apply_rope(k, cos, sin)
    sink = sink.reshape(WIN_KV, g)
    kp = jnp.pad(k, ((0, 0), (0, 0), (WINDOW, WINDOW), (0, 0)))
    vp = jnp.pad(v, ((0, 0), (0, 0), (WINDOW, WINDOW), (0, 0)))
    qb = jnp.moveaxis(q.reshape(b, WIN_KV, g, nb, BLOCK, WIN_HD), 3, 0)
    offs = jnp.arange(span) - WINDOW
    ctx_cols = jnp.ones((BLOCK, kc.shape[2]), bool)

    def block(args):
        bi, qblk = args
        start = bi * BLOCK
        kw = jnp.concatenate([lax.dynamic_slice_in_dim(kp, start, span, axis=2), kc], axis=2)
        vw = jnp.concatenate([lax.dynamic_slice_in_dim(vp, start, span, axis=2), vc], axis=2)
        qpos = start + jnp.arange(BLOCK)
        kpos = start + offs
        band = (jnp.abs(qpos[:, None] - kpos[None, :]) <= WINDOW) & (kpos >= 0)[None, :] & (kpos < s)[None, :]
        return gqa_softmax(qblk, kw, vw, jnp.concatenate([band, ctx_cols], axis=1), sink)

    o = lax.map(block, (jnp.arange(nb), qb))
    o = jnp.moveaxis(o, 0, 3).reshape(b, WIN_KV, g, s, WIN_HD)
    y_lat = merge_heads(o) @ w_o
    y_ctx = merge_heads(gqa_softmax(qc, kc, vc, None, sink)) @ w_o if ctx_out else None
    return y_lat, y_ctx


def short_conv_mixer(a, w_in, conv_w, w_out):
    gate_b, gate_c, u = jnp.split(a @ w_in, 3, axis=-1)
    return (gate_b * dwconv3(gate_c * u, conv_w)) @ w_out


def axial_attention_mixer(a_lat, a_ctx, w_qkv, q_norm, k_norm, w_o, ctx_out):
    b, s, _ = a_lat.shape
    nb = s // BLOCK
    g = AX_HEADS // AX_KV
    q, k, v = qkv_heads(a_lat, w_qkv, q_norm, k_norm, AX_HEADS, AX_KV, AX_HD)
    qc, kc, vc = qkv_heads(a_ctx, w_qkv, q_norm, k_norm, AX_HEADS, AX_KV, AX_HD)
    cos, sin = axial_rope(s, AX_HD)
    q = apply_rope(q, cos, sin)
    k = apply_rope(k, cos, sin)
    k_all = jnp.concatenate([k, kc], axis=2)
    v_all = jnp.concatenate([v, vc], axis=2)
    qb = jnp.moveaxis(q.reshape(b, AX_KV, g, nb, BLOCK, AX_HD), 3, 0)

    def block(qblk):
        return gqa_softmax(qblk, k_all, v_all, None, None)

    o = lax.map(block, qb)
    o = jnp.moveaxis(o, 0, 3).reshape(b, AX_KV, g, s, AX_HD)
    y_lat = merge_heads(o) @ w_o
    y_ctx = merge_heads(gqa_softmax(qc, kc, vc, None, None)) @ w_o if ctx_out else None
    return y_lat, y_ctx


def mlstm_chunk_summary(k, v, log_i, log_f):
    cum = jnp.cumsum(log_f, axis=-1)
    a = cum[..., -1:] - cum + log_i
    m = a.max(axis=-1)
    w = jnp.exp(a - m[..., None])
    c_bar = jnp.einsum('...lv,...lk->...vk', v * w[..., None], k)
    n_bar = jnp.einsum('...l,...lk->...k', w, k)
    return c_bar, n_bar, m, cum


def mlstm_final_state(k, v, log_i, log_f):
    c_bar, n_bar, m, _ = mlstm_chunk_summary(k, v, log_i, log_f)
    return (c_bar, n_bar, m)


def mlstm_chunkwise(q, k, v, log_i, log_f, state):
    b, h, s, dk = q.shape
    dv = v.shape[-1]
    nc = s // ML_CHUNK
    qc = q.reshape(b, h, nc, ML_CHUNK, dk)
    kc = k.reshape(b, h, nc, ML_CHUNK, dk)
    vc = v.reshape(b, h, nc, ML_CHUNK, dv)
    lic = log_i.reshape(b, h, nc, ML_CHUNK)
    lfc = log_f.reshape(b, h, nc, ML_CHUNK)
    c_bar, n_bar, m_bar, cum = mlstm_chunk_summary(kc, vc, lic, lfc)

    def step(carry, xs):
        c_prev, n_prev, m_prev = carry
        cb, nb_, mb, gb = xs
        m_new = jnp.maximum(gb + m_prev, mb)
        decay = jnp.exp(gb + m_prev - m_new)
        inj = jnp.exp(mb - m_new)
        c_new = decay[..., None, None] * c_prev + inj[..., None, None] * cb
        n_new = decay[..., None] * n_prev + inj[..., None] * nb_
        return (c_new, n_new, m_new), (c_prev, n_prev, m_prev)

    xs = (jnp.moveaxis(c_bar, 2, 0), jnp.moveaxis(n_bar, 2, 0), jnp.moveaxis(m_bar, 2, 0), jnp.moveaxis(cum[..., -1], 2, 0))
    final, (c_start, n_start, m_start) = lax.scan(step, state, xs)
    c_start = jnp.moveaxis(c_start, 0, 2)
    n_start = jnp.moveaxis(n_start, 0, 2)
    m_start = jnp.moveaxis(m_start, 0, 2)
    tri = jnp.tril(jnp.ones((ML_CHUNK, ML_CHUNK), bool))
    d = jnp.where(tri, cum[..., :, None] - cum[..., None, :] + lic[..., None, :], NEG)
    inter = cum + m_start[..., None]
    m_t = jnp.maximum(inter, d.max(axis=-1))
    w_intra = jnp.exp(d - m_t[..., None])
    w_inter = jnp.exp(inter - m_t)
    qk = jnp.einsum('bhcld,bhcsd->bhcls', qc, kc) * w_intra
    num = jnp.einsum('bhcls,bhcsv->bhclv', qk, vc) + w_inter[..., None] * jnp.einsum('bhcld,bhcvd->bhclv', qc, c_start)
    den = qk.sum(axis=-1) + w_inter * jnp.einsum('bhcld,bhcd->bhcl', qc, n_start)
    out = num / jnp.maximum(jnp.abs(den), jnp.exp(-m_t))[..., None]
    return out.reshape(b, h, s, dv), final


def mlstm_mixer(a_lat, a_ctx, w_in, b_gate, h_norm, w_out, ctx_out):
    qk_w = ML_HEADS * ML_DK
    v_w = ML_HEADS * ML_DV

    def proj(a):
        b, s, _ = a.shape
        q, k, v, o, gates = jnp.split(a @ w_in, [qk_w, 2 * qk_w, 2 * qk_w + v_w, 2 * qk_w + 2 * v_w], axis=-1)
        q = split_heads(q, ML_HEADS, ML_DK).astype(jnp.float32) * (ML_DK ** -0.5)
        k = split_heads(k, ML_HEADS, ML_DK).astype(jnp.float32)
        v = split_heads(v, ML_HEADS, ML_DV).astype(jnp.float32)
        gates = (gates + b_gate).astype(jnp.float32).reshape(b, s, 4, ML_HEADS).transpose(2, 0, 3, 1)
        fwd = (gates[0], jax.nn.log_sigmoid(gates[1]))
        bwd = (gates[2], jax.nn.log_sigmoid(gates[3]))
        return q, k, v, o, fwd, bwd

    def flip(t):
        return jnp.flip(t, axis=2)

    def readout(h_f, h_b, o):
        hs = rms_norm(h_f + flip(h_b), h_norm)
        b, h, s, dv = hs.shape
        hs = hs.transpose(0, 2, 1, 3).reshape(b, s, h * dv).astype(o.dtype)
        return (jax.nn.sigmoid(o) * hs) @ w_out

    qc, kc, vc, oc, gfc, gbc = proj(a_ctx)
    bsz = a_ctx.shape[0]
    zero = (jnp.zeros((bsz, ML_HEADS, ML_DV, ML_DK), jnp.float32),
            jnp.zeros((bsz, ML_HEADS, ML_DK), jnp.float32),
            jnp.full((bsz, ML_HEADS), NEG, jnp.float32))
    if ctx_out:
        hc_f, st_f = mlstm_chunkwise(qc, kc, vc, gfc[0], gfc[1], zero)
        hc_b, st_b = mlstm_chunkwise(flip(qc), flip(kc), flip(vc), flip(gbc[0]), flip(gbc[1]), zero)
        y_ctx = readout(hc_f, hc_b, oc)
    else:
        st_f = mlstm_final_state(kc, vc, gfc[0], gfc[1])
        st_b = mlstm_final_state(flip(kc), flip(vc), flip(gbc[0]), flip(gbc[1]))
        y_ctx = None
    ql, kl, vl, ol, gfl, gbl = proj(a_lat)
    hl_f, _ = mlstm_chunkwise(ql, kl, vl, gfl[0], gfl[1], st_f)
    hl_b, _ = mlstm_chunkwise(flip(ql), flip(kl), flip(vl), flip(gbl[0]), flip(gbl[1]), st_b)
    return readout(hl_f, hl_b, ol), y_ctx


def conv_ffn(a, w_up, conv_w, w_down):
    g, u = jnp.split(dwconv3(a @ w_up, conv_w), 2, axis=-1)
    return (jax.nn.silu(g) * u) @ w_down


def setup_inputs(seed: int = 0) -> dict:
    d = D_MODEL
    n_win, n_sc, n_ax, n_ml = [len(range(kind, DEPTH, N_MIXERS)) for kind in range(N_MIXERS)]
    keys = iter(jax.random.split(jax.random.key(seed), 40))

    def normal(shape, std):
        return std * jax.random.normal(next(keys), shape, jnp.float32)

    def gain(shape):
        return 1.0 + normal(shape, 0.1)

    ml_in = 2 * ML_HEADS * ML_DK + 2 * ML_HEADS * ML_DV + 4 * ML_HEADS
    f_base = jnp.linspace(3.0, 6.0, ML_HEADS, dtype=jnp.float32)
    zeros_h = jnp.zeros((ML_HEADS,), jnp.float32)
    gate_base = jnp.concatenate([zeros_h, f_base, zeros_h, f_base])
    return {
        'x': normal((BATCH, SEQ, d), 1.0),
        'c': normal((BATCH, d), 1.0),
        'ctx': normal((BATCH, CTX_LEN, d), 1.0),
        'c_ctx': normal((d,), 1.0),
        'ada_w': normal((DEPTH, d, 6 * d), 0.5 * d ** -0.5),
        'ada_b': normal((DEPTH, 6 * d), 0.02),
        'norm_mix': gain((DEPTH, d)),
        'norm_ffn': gain((DEPTH, d)),
        'ffn_w_up': normal((DEPTH, d, 2 * D_FF), d ** -0.5),
        'ffn_conv': normal((DEPTH, 3, 2 * D_FF), 3 ** -0.5),
        'ffn_w_down': normal((DEPTH, D_FF, d), D_FF ** -0.5),
        'win_w_qkv': normal((n_win, d, (WIN_HEADS + 2 * WIN_KV) * WIN_HD), d ** -0.5),
        'win_q_norm': gain((n_win, WIN_HD)),
        'win_k_norm': gain((n_win, WIN_HD)),
        'win_sink': normal((n_win, WIN_HEADS), 0.5),
        'win_w_o': normal((n_win, WIN_HEADS * WIN_HD, d), (WIN_HEADS * WIN_HD) ** -0.5),
        'sc_w_in': normal((n_sc, d, 3 * d), d ** -0.5),
        'sc_conv': normal((n_sc, 3, d), 3 ** -0.5),
        'sc_w_out': normal((n_sc, d, d), d ** -0.5),
        'ax_w_qkv': normal((n_ax, d, (AX_HEADS + 2 * AX_KV) * AX_HD), d ** -0.5),
        'ax_q_norm': gain((n_ax, AX_HD)),
        'ax_k_norm': gain((n_ax, AX_HD)),
        'ax_w_o': normal((n_ax, AX_HEADS * AX_HD, d), (AX_HEADS * AX_HD) ** -0.5),
        'ml_w_in': normal((n_ml, d, ml_in), d ** -0.5),
        'ml_b_gate': gate_base + normal((n_ml, 4 * ML_HEADS), 0.1),
        'ml_h_norm': gain((n_ml, ML_DV)),
        'ml_w_out': normal((n_ml, ML_HEADS * ML_DV, d), (ML_HEADS * ML_DV) ** -0.5),
    }


def reference(x, c, ctx, c_ctx, ada_w, ada_b, norm_mix, norm_ffn, ffn_w_up, ffn_conv, ffn_w_down,
              win_w_qkv, win_q_norm, win_k_norm, win_sink, win_w_o,
              sc_w_in, sc_conv, sc_w_out,
              ax_w_qkv, ax_q_norm, ax_k_norm, ax_w_o,
              ml_w_in, ml_b_gate, ml_h_norm, ml_w_out):
    h, hc = x, ctx
    for i in range(DEPTH):
        kind, j = i % N_MIXERS, i // N_MIXERS
        ctx_out = i < DEPTH - 1
        sh1, sc1, g1, sh2, sc2, g2 = adaln(c, ada_w[i], ada_b[i])
        a_lat = modulate(rms_norm(h, norm_mix[i]), sh1, sc1)
        if ctx_out or kind != 1:
            csh1, csc1, cg1, csh2, csc2, cg2 = adaln(c_ctx, ada_w[i], ada_b[i])
            a_ctx = modulate(rms_norm(hc, norm_mix[i]), csh1, csc1)
        if kind == 0:
            y_lat, y_ctx = window_attention_mixer(a_lat, a_ctx, win_w_qkv[j], win_q_norm[j], win_k_norm[j],
                                                  win_sink[j], win_w_o[j], ctx_out)
        elif kind == 1:
            y_lat = short_conv_mixer(a_lat, sc_w_in[j], sc_conv[j], sc_w_out[j])
            y_ctx = short_conv_mixer(a_ctx, sc_w_in[j], sc_conv[j], sc_w_out[j]) if ctx_out else None
        elif kind == 2:
            y_lat, y_ctx = axial_attention_mixer(a_lat, a_ctx, ax_w_qkv[j], ax_q_norm[j], ax_k_norm[j],
                                                 ax_w_o[j], ctx_out)
        else:
            y_lat, y_ctx = mlstm_mixer(a_lat, a_ctx, ml_w_in[j], ml_b_gate[j], ml_h_norm[j], ml_w_out[j], ctx_out)
        h = h + g1 * y_lat
        h = h + g2 * conv_ffn(modulate(rms_norm(h, norm_ffn[i]), sh2, sc2), ffn_w_up[i], ffn_conv[i], ffn_w_down[i])
        if ctx_out:
            hc = hc + cg1 * y_ctx
            hc = hc + cg2 * conv_ffn(modulate(rms_norm(hc, norm_ffn[i]), csh2, csc2),
                                     ffn_w_up[i], ffn_conv[i], ffn_w_down[i])
    return h
```

```python
import numpy as np
import concourse.bass as bass
import concourse.mybir as mybir
from concourse.bass_utils import run_bass_kernel_spmd

F32 = mybir.dt.float32
BF16 = mybir.dt.bfloat16
AF = mybir.ActivationFunctionType
ALU = mybir.AluOpType

D = 1024
S = 8192
CTX = 256
DEPTH = 4
DFF = 2816
NFC = 44
EPS = 1e-6
SEM_CAP = 8000
N_DMA_SEMS = 24
ENGS = ("pe", "act", "dve", "pool", "sp")


class Buf:
    __slots__ = ("name", "last_w", "readers", "parent", "subs")

    def __init__(self, name, parent=None):
        self.name = name
        self.last_w = None
        self.readers = []
        self.parent = parent
        self.subs = {}

    def sub(self, key):
        b = self.subs.get(key)
        if b is None:
            b = Buf(f"{self.name}[{key}]", parent=self)
            self.subs[key] = b
        return b


class T:
    def __init__(self, t, name):
        self.t = t
        self.buf = Buf(name)

    def __getitem__(self, idx):
        return self.t[idx]

    def sub(self, key):
        return self.buf.sub(key)


def _bufs(xs):
    out = []
    for x in xs:
        if x is None:
            continue
        out.append(x.buf if isinstance(x, T) else x)
    return out


class Op:
    __slots__ = ("eng", "fn", "is_dma", "deps", "tick", "signal", "dsem", "dval", "waits", "pre_dma_wait")

    def __init__(self, eng, fn, is_dma):
        self.eng = eng
        self.fn = fn
        self.is_dma = is_dma
        self.deps = {}
        self.signal = False
        self.tick = None
        self.dsem = None
        self.dval = None
        self.waits = []
        self.pre_dma_wait = None


class Prog:
    def __init__(self, nc, arena_words=52736):
        self.nc = nc
        self.ops = []
        self._n = 0
        self.arena = nc.alloc_sbuf_tensor("arena", [128, arena_words], F32)
        self.arena_bf = self.arena.bitcast(BF16)
        self.arena_bytes = arena_words * 4
        self.off = 0
        self._last_compute = {}
        self._dmas_since_bar = []
        self._bar_frontier = []
        self._bar_pending = set()

    def alloc(self, name, shape, dt):
        es = 4 if dt == F32 else 2
        n = 1
        for d in shape[1:]:
            n *= d
        off = (self.off + 63) // 64 * 64
        self.off = off + n * es
        assert self.off <= self.arena_bytes, f"SBUF arena overflow at {name}: {self.off}"
        base = self.arena if dt == F32 else self.arena_bf
        e0 = off // es
        ap = base[0:shape[0], e0:e0 + n]
        if len(shape) == 3:
            ap = ap.rearrange("p (a b) -> p a b", a=shape[1])
        elif len(shape) == 4:
            ap = ap.rearrange("p (a b c) -> p a b c", a=shape[1], b=shape[2])
        return T(ap, name)

    def mark(self):
        return self.off

    def release(self, m):
        self.off = m

    def barrier(self):
        self._bar_frontier = list(self._last_compute.values()) + list(self._dmas_since_bar)
        self._dmas_since_bar = []
        self._bar_pending = set(ENGS)

    def ps(self, name, shape, dt=F32):
        self._n += 1
        return T(self.nc.alloc_psum_tensor(f"{name}_{self._n}", list(shape), dt), name)

    def dram(self, name, shape, dt, kind="Internal"):
        return T(self.nc.dram_tensor(name, list(shape), dt, kind=kind), name)

    def op(self, eng, fn, reads=(), writes=(), is_dma=False):
        o = Op(eng, fn, is_dma)
        rb = _bufs(reads)
        wb = _bufs(writes)

        def hist(b):
            hs = [b]
            if b.parent is not None:
                hs.append(b.parent)
            else:
                hs.extend(b.subs.values())
            return hs

        for b in rb:
            for h in hist(b):
                if h.last_w is not None:
                    o.deps[h.last_w] = True
        for b in wb:
            for h in hist(b):
                if h.last_w is not None and h.last_w not in o.deps:
                    o.deps[h.last_w] = False
                for r in h.readers:
                    if r not in o.deps:
                        o.deps[r] = False
        if eng in self._bar_pending:
            self._bar_pending.discard(eng)
            for d in self._bar_frontier:
                o.deps[d] = True
        if is_dma:
            self._dmas_since_bar.append(o)
        else:
            self._last_compute[eng] = o
        for b in rb:
            b.readers.append(o)
        for b in wb:
            b.last_w = o
            b.readers = []
            if b.parent is None:
                for s in b.subs.values():
                    s.last_w = o
                    s.readers = []
        self.ops.append(o)
        return o

    def dma(self, eng, out_ap, in_ap, reads=(), writes=(), **kw):
        return self.op(eng, lambda e: e.dma_start(out=out_ap, in_=in_ap, **kw), reads, writes, is_dma=True)

    def emit(self):
        nc = self.nc
        streams = {e: [] for e in ENGS}
        for o in self.ops:
            streams[o.eng].append(o)

        def skip(o, d, raw):
            return (not d.is_dma) and d.eng == o.eng and (not o.is_dma) and o.eng == "pe"

        for o in self.ops:
            for d, raw in o.deps.items():
                if d.is_dma or skip(o, d, raw):
                    continue
                d.signal = True
        ctr_sems = {}
        for e in ENGS:
            k = 0
            for o in streams[e]:
                if o.signal and not o.is_dma:
                    k += 1
                    o.tick = k
            ctr_sems[e] = [nc.alloc_semaphore(f"ctr_{e}_{i}") for i in range((k + SEM_CAP - 1) // SEM_CAP)]
        finals = {}
        for e in ENGS:
            dmas = [o for o in streams[e] if o.is_dma]
            if not dmas:
                continue
            pool = [nc.alloc_semaphore(f"dma_{e}_{i}") for i in range(min(N_DMA_SEMS, len(dmas)))]
            vals = [0] * len(pool)
            for i, o in enumerate(dmas):
                j = i % len(pool)
                if vals[j] > 0:
                    o.pre_dma_wait = (pool[j], vals[j])
                vals[j] += 16
                o.dsem, o.dval = pool[j], vals[j]
            finals[e] = [(pool[j], vals[j]) for j in range(len(pool))]
        for e in ENGS:
            waited = {}
            for o in streams[e]:
                ws = {}
                cands = []
                for d, raw in o.deps.items():
                    if d.is_dma:
                        cands.append((d.dsem, d.dval, ("dma", id(d.dsem))))
                    elif not skip(o, d, raw):
                        t = d.tick - 1
                        cands.append((ctr_sems[d.eng][t // SEM_CAP], (t % SEM_CAP) + 1, (d.eng, t // SEM_CAP)))
                if o.pre_dma_wait is not None:
                    cands.append((o.pre_dma_wait[0], o.pre_dma_wait[1], ("dma", id(o.pre_dma_wait[0]))))
                for sem, val, key in cands:
                    if waited.get(key, 0) >= val:
                        continue
                    if key not in ws or ws[key][1] < val:
                        ws[key] = (sem, val)
                for key, (sem, val) in ws.items():
                    waited[key] = val
                o.waits = list(ws.values())
        engmap = {"pe": "tensor", "act": "scalar", "dve": "vector", "pool": "gpsimd", "sp": "sync"}
        with nc.Block() as block:
            for e in ENGS:
                ops = streams[e]
                if not ops:
                    continue

                def body(eng, ops=ops, e=e):
                    for o in ops:
                        for sem, val in o.waits:
                            eng.wait_ge(sem, val)
                        ins = o.fn(eng)
                        if o.is_dma:
                            ins.then_inc(o.dsem, 16)
                        elif o.signal:
                            ins.then_inc(ctr_sems[e][(o.tick - 1) // SEM_CAP], 1)
                    for sem, val in finals.get(e, []):
                        eng.wait_ge(sem, val)

                getattr(block, engmap[e])(body)
        return len(self.ops)


def _vec_layout():
    cols = {}
    n = 0

    def add(name, k):
        nonlocal n
        cols[name] = n
        n += k

    for l in range(DEPTH):
        add(f"ada_b{l}", 48)
        add(f"nmix{l}", 8)
        add(f"nffn{l}", 8)
        add(f"fconv{l}", 3 * NFC)
    add("sc_conv", 24)
    for nm in ("ax_qg", "ax_qgs", "ax_kg", "ax_kgs", "win_qg", "win_qgs", "win_kg", "win_kgs"):
        add(nm, 1)
    add("win_sink", 16)
    add("ml_bg", 16)
    add("ml_hn", 2)
    return cols, n


VCOLS, NV = _vec_layout()


def _colmajor(v):
    v = np.asarray(v, np.float32).reshape(-1, 128)
    return v.T


def pack_vecs(inp):
    out = np.zeros((128, NV), np.float32)

    def put(name, arr):
        a = _colmajor(arr)
        out[:, VCOLS[name]:VCOLS[name] + a.shape[1]] = a

    for l in range(DEPTH):
        put(f"ada_b{l}", inp["ada_b"][l])
        put(f"nmix{l}", inp["norm_mix"][l])
        put(f"nffn{l}", inp["norm_ffn"][l])
        put(f"fconv{l}", inp["ffn_conv"][l].reshape(-1))
    put("sc_conv", inp["sc_conv"][0].reshape(-1))

    def swap(v):
        h = v.shape[0] // 2
        return np.concatenate([v[h:], v[:h]])

    put("ax_qg", inp["ax_q_norm"][0])
    put("ax_qgs", swap(inp["ax_q_norm"][0]))
    put("ax_kg", inp["ax_k_norm"][0])
    put("ax_kgs", swap(inp["ax_k_norm"][0]))
    put("win_qg", np.tile(inp["win_q_norm"][0], 2))
    put("win_qgs", np.tile(swap(inp["win_q_norm"][0]), 2))
    put("win_kg", np.tile(inp["win_k_norm"][0], 2))
    put("win_kgs", np.tile(swap(inp["win_k_norm"][0]), 2))
    out[:, VCOLS["win_sink"]:VCOLS["win_sink"] + 16] = np.broadcast_to(inp["win_sink"][0][None, :], (128, 16))
    out[:, VCOLS["ml_bg"]:VCOLS["ml_bg"] + 16] = np.broadcast_to(inp["ml_b_gate"][0][None, :], (128, 16))
    put("ml_hn", inp["ml_h_norm"][0])
    return out


def rope_tables(hd):
    n_freq = hd // 4
    rows = S // 64
    row = np.repeat(np.arange(rows, dtype=np.float32), 64)
    col = np.tile(np.arange(64, dtype=np.float32), rows)
    inv = np.power(np.float32(10000.0), -np.arange(n_freq, dtype=np.float32) / np.float32(n_freq)).astype(np.float32)
    ang = np.concatenate([row[:, None] * inv, col[:, None] * inv], axis=-1).astype(np.float32)
    cos = np.cos(ang).astype(np.float32).T
    sin = np.sin(ang).astype(np.float32).T
    cos2 = np.concatenate([cos, cos], axis=0)
    sin2 = np.concatenate([-sin, sin], axis=0)
    rep = 128 // hd
    cos2 = np.tile(cos2, (rep, 1))
    sin2 = np.tile(sin2, (rep, 1))
    c = np.ones((128, S + CTX), np.float32)
    sn = np.zeros((128, S + CTX), np.float32)
    c[:, :S] = cos2
    sn[:, :S] = sin2
    return c, sn


class K:
    def __init__(self, test=None):
        self.test = test
        nc = bass.Bass("TRN2", target_bir_lowering=False)
        self.nc = nc
        P = Prog(nc)
        self.P = P
        dr = lambda n, s, k="ExternalInput": P.dram(n, s, F32, kind=k)
        self.xT = dr("xT", [D, S])
        self.ctxT = dr("ctxT", [D, CTX])
        self.cc = dr("cc", [128, 8, 2])
        self.vecs = dr("vecs", [128, NV])
        self.ada_w = dr("ada_w", [DEPTH, D, 6 * D])
        self.w_up = dr("ffn_w_up", [DEPTH, D, 2 * DFF])
        self.w_down = dr("ffn_w_down", [DEPTH, DFF, D])
        self.sc_w_in = dr("sc_w_in", [D, 3 * D])
        self.sc_w_out = dr("sc_w_out", [D, D])
        self.ax_w_qkv = dr("ax_w_qkv", [D, 1536])
        self.ax_w_o = dr("ax_w_o", [D, D])
        self.win_w_qkv = dr("win_w_qkv", [D, 1536])
        self.win_w_o = dr("win_w_o", [D, D])
        self.cos128 = dr("cos128", [128, S + CTX])
        self.sin128 = dr("sin128", [128, S + CTX])
        self.cos64 = dr("cos64", [128, S + CTX])
        self.sin64 = dr("sin64", [128, S + CTX])
        self.wmask = dr("wmask", [128, 1024])
        self.ml_w_in = dr("ml_w_in", [D, 3088])
        self.ml_w_out = dr("ml_w_out", [D, D])
        self.trimat = dr("trimat", [128, 256])
        self.hf = dr("hf_scratch", [D, S], "Internal")
        self.outT = dr("outT", [D, S], "ExternalOutput")
        self.hA = dr("hA", [D, S], "Internal")
        self.hB = dr("hB", [D, S], "Internal")
        self.cA = dr("cA", [D, CTX], "Internal")
        self.cB = dr("cB", [D, CTX], "Internal")
        self.ones = P.alloc("ones", [128, 128], BF16)
        self.vt = P.alloc("vt", [128, NV], F32)
        self.mod = P.alloc("mod", [128, DEPTH, 48, 2], F32)
        self.gsm = P.alloc("gsm", [128, DEPTH, 8, 2], F32)
        self.gsf = P.alloc("gsf", [128, DEPTH, 8, 2], F32)
        self.epsb = P.alloc("epsb", [128, 1], F32)
        self.ones_bd = P.alloc("ones_bd", [128, 128], BF16)
        self.zero_c = P.alloc("zero_c", [128, 1], F32)
        self.ps = [P.ps(f"ps{i}", [128, 512]) for i in range(8)]
        self.base_mark = P.mark()

    def setup(self):
        P = self.P
        P.op("pool", lambda e: e.memset(self.ones[:], 1.0), writes=[self.ones])
        P.op("pool", lambda e: e.memset(self.ones_bd[:], 0.0), writes=[self.ones_bd])
        P.op("pool", lambda e: e.memset(self.ones_bd[0:64, 0:64], 1.0), writes=[self.ones_bd])
        P.op("pool", lambda e: e.memset(self.ones_bd[64:128, 64:128], 1.0), writes=[self.ones_bd])
        P.op("pool", lambda e: e.memset(self.epsb[:], EPS), writes=[self.epsb])
        P.op("pool", lambda e: e.memset(self.zero_c[:], 0.0), writes=[self.zero_c])
        P.dma("sp", self.vt[:], self.vecs[:], writes=[self.vt])

    def adaln(self):
        P = self.P
        m = P.mark()
        cct = P.alloc("cct", [128, 8, 2], F32)
        sig = P.alloc("sig", [128, 8, 2], F32)
        sc = P.alloc("sc", [128, 8, 2], F32)
        P.dma("sp", cct[:], self.cc[:], writes=[cct])
        P.op("act", lambda e: e.activation(out=sig[:], in_=cct[:], func=AF.Sigmoid), reads=[cct], writes=[sig])
        P.op("dve", lambda e: e.tensor_tensor(out=sc[:], in0=cct[:], in1=sig[:], op=ALU.mult), reads=[cct, sig], writes=[sc])
        NP = 8
        PW = 768
        wbuf = [P.alloc(f"adaw{i}", [128, 8, PW], F32) for i in range(2)]
        pst = self.ps[0]
        k = 0
        for l in range(DEPTH):
            for pc in range(NP):
                wb = wbuf[k % 2]
                k += 1
                P.dma("sp", wb[:], self.ada_w[l, :, pc * PW:(pc + 1) * PW].rearrange("(c p) f -> p c f", p=128), writes=[wb])
                for jj in range(PW // 128):
                    j = pc * (PW // 128) + jj
                    for c in range(8):
                        P.op("pe", lambda e, wb=wb, jj=jj, c=c, j=j: e.matmul(pst[:, 2 * j:2 * j + 2], wb[:, c, jj * 128:(jj + 1) * 128], sc[:, c, :], start=(c == 0), stop=(c == 7)),
                             reads=[wb, sc], writes=[pst])
            cb = VCOLS[f"ada_b{l}"]
            for s in range(2):
                P.op("dve", lambda e, l=l, s=s, cb=cb: e.tensor_tensor(out=self.mod[:, l, :, s], in0=pst[:, 0:96].rearrange("p (j s) -> p j s", s=2)[:, :, s], in1=self.vt[:, cb:cb + 48], op=ALU.add),
                     reads=[pst, self.vt], writes=[self.mod])
            for s in range(2):
                for (dst, vi, nm) in ((self.gsm, 1, f"nmix{l}"), (self.gsf, 4, f"nffn{l}")):
                    cn = VCOLS[nm]
                    P.op("dve", lambda e, l=l, s=s, dst=dst, vi=vi, cn=cn: e.scalar_tensor_tensor(out=dst[:, l, :, s], in0=self.mod[:, l, vi * 8:vi * 8 + 8, s], scalar=1.0, in1=self.vt[:, cn:cn + 8], op0=ALU.add, op1=ALU.mult),
                         reads=[self.mod, self.vt], writes=[dst])
        P.barrier()
        P.release(m)

    def modcol(self, l, vi, c, s):
        return self.mod[:, l, vi * 8 + c, s:s + 1]

    def norm_mod(self, src, t0, W, gs, l, shift_vi, s, a_out, ht, sq, rs, pst):
        P = self.P
        P.dma("sp", ht[:, :, 0:W], src[:, t0:t0 + W].rearrange("(c p) t -> p c t", p=128), writes=[ht])
        for c in range(8):
            q = sq[c % len(sq)]
            P.op("act", lambda e, c=c, q=q: e.activation(out=q[:, 0:W], in_=ht[:, c, 0:W], func=AF.Square), reads=[ht], writes=[q])
            P.op("pe", lambda e, c=c, q=q: e.matmul(pst[:, 0:W], self.ones[:], q[:, 0:W], start=(c == 0), stop=(c == 7)), reads=[self.ones, q], writes=[pst])
        P.op("act", lambda e: e.activation(out=rs[:, 0:W], in_=pst[:, 0:W], func=AF.Sqrt, bias=self.epsb[:], scale=1.0 / D), reads=[pst, self.epsb], writes=[rs])
        P.op("dve", lambda e: e.reciprocal(rs[:, 0:W], rs[:, 0:W]), reads=[rs], writes=[rs])
        for c in range(8):
            P.op("dve", lambda e, c=c: e.tensor_tensor(out=ht[:, c, 0:W], in0=ht[:, c, 0:W], in1=rs[:, 0:W], op=ALU.mult),
                 reads=[ht.sub(c), rs], writes=[ht.sub(c)])
            P.op("act", lambda e, c=c: e.activation(out=a_out[:, c, 0:W], in_=ht[:, c, 0:W], func=AF.Identity, bias=self.modcol(l, shift_vi, c, s), scale=gs[:, l, c, s:s + 1]),
                 reads=[ht.sub(c), self.mod, gs], writes=[a_out.sub(c)])

    def load_cast(self, dst, dst_kc, w_ap_rows, ncols, stage, k0=0):
        P = self.P
        PIECE = stage[0].t.shape[-1] if False else 2048
        c0 = 0
        k = k0
        while c0 < ncols:
            n = min(PIECE, ncols - c0)
            st = stage[k % len(stage)]
            P.dma("sp", st[:, 0:n], w_ap_rows[:, c0:c0 + n], writes=[st])
            eng = "pool" if k % 2 == 0 else "dve"
            P.op(eng, lambda e, st=st, n=n, c0=c0: e.tensor_copy(dst[:, dst_kc, c0:c0 + n], st[:, 0:n]), reads=[st], writes=[dst.sub(("w", dst_kc, c0))])
            c0 += n
            k += 1
        return k

    def ffn(self, l, src, dst, csrc, cdst, do_ctx, T_lat=S):
        P = self.P
        m = P.mark()
        wu = P.alloc("wu", [128, 8, 2 * DFF], BF16)
        wd = P.alloc("wd", [128, 22, D], BF16)
        m2 = P.mark()
        stage = [P.alloc(f"stg{i}", [128, 2048], F32) for i in range(2)]
        k = 0
        for kc in range(8):
            k = self.load_cast(wu, kc, self.w_up[l, kc * 128:(kc + 1) * 128, :], 2 * DFF, stage, k)
        for kc in range(22):
            k = self.load_cast(wd, kc, self.w_down[l, kc * 128:(kc + 1) * 128, :], D, stage, k)
        P.barrier()
        P.release(m2)
        ht = P.alloc("ht", [128, 8, 512], F32)
        sq = [P.alloc(f"sq{i}", [128, 512], BF16) for i in range(2)]
        rs = P.alloc("rs", [128, 512], F32)
        a = P.alloc("a", [128, 8, 512], BF16)
        NU = 3
        uc = [P.alloc(f"uc{i}", [128, 514], BF16) for i in range(NU)]
        yc = [P.alloc(f"yc{i}", [128, 512], BF16) for i in range(NU + 1)]
        sg = [P.alloc(f"sg{i}", [128, 512], BF16) for i in range(2)]
        carry = P.alloc("carry", [128, NFC, 2], BF16)
        gu = P.alloc("gu", [128, 22, 512], BF16)
        hr = [P.alloc(f"hr{i}", [128, 512], F32) for i in range(2)]
        cw = VCOLS[f"fconv{l}"]
        vt = self.vt
        ps_ss = self.ps[0]
        ps_up = self.ps[1:4]
        ps_dn = self.ps[4:8]
        cnt = {"u": 0, "y": 0, "d": 0, "g": 0, "h": 0}

        def up_conv(t0, W, flush):
            for i in range(22):
                ycs = []
                for half in range(2):
                    fc = half * 22 + i
                    u = uc[cnt["u"] % NU]
                    pu = ps_up[cnt["u"] % 3]
                    cnt["u"] += 1
                    y = yc[cnt["y"] % (NU + 1)]
                    cnt["y"] += 1
                    P.op("pool", lambda e, u=u, fc=fc: e.tensor_copy(u[:, 0:2], carry[:, fc, :]), reads=[carry.sub(fc)], writes=[u])
                    if flush:
                        P.op("pool", lambda e, u=u: e.memset(u[:, 2:3], 0.0), writes=[u])
                    else:
                        for c in range(8):
                            P.op("pe", lambda e, c=c, fc=fc, pu=pu, W=W: e.matmul(pu[:, 0:W], wu[:, c, fc * 128:(fc + 1) * 128], a[:, c, 0:W], start=(c == 0), stop=(c == 7)),
                                 reads=[wu, a.sub(c)], writes=[pu])
                        P.op("act", lambda e, u=u, pu=pu, W=W: e.activation(out=u[:, 2:2 + W], in_=pu[:, 0:W], func=AF.Copy), reads=[pu], writes=[u])
                        P.op("pool", lambda e, u=u, fc=fc, W=W: e.tensor_copy(carry[:, fc, :], u[:, W:W + 2]), reads=[u], writes=[carry.sub(fc)])
                    P.op("dve", lambda e, u=u, y=y, fc=fc, W=W: e.tensor_scalar(y[:, 0:W], u[:, 0:W], vt[:, cw + fc:cw + fc + 1], None, op0=ALU.mult), reads=[u, vt], writes=[y])
                    P.op("dve", lambda e, u=u, y=y, fc=fc, W=W: e.scalar_tensor_tensor(out=y[:, 0:W], in0=u[:, 1:1 + W], scalar=vt[:, cw + NFC + fc:cw + NFC + fc + 1], in1=y[:, 0:W], op0=ALU.mult, op1=ALU.add), reads=[u, vt, y], writes=[y])
                    P.op("dve", lambda e, u=u, y=y, fc=fc, W=W: e.scalar_tensor_tensor(out=y[:, 0:W], in0=u[:, 2:2 + W], scalar=vt[:, cw + 2 * NFC + fc:cw + 2 * NFC + fc + 1], in1=y[:, 0:W], op0=ALU.mult, op1=ALU.add), reads=[u, vt, y], writes=[y])
                    ycs.append(y)
                sgt = sg[cnt["g"] % 2]
                cnt["g"] += 1
                P.op("act", lambda e, sgt=sgt, y=ycs[0], W=W: e.activation(out=sgt[:, 0:W], in_=y[:, 0:W], func=AF.Silu), reads=[ycs[0]], writes=[sgt])
                P.op("dve", lambda e, sgt=sgt, y=ycs[1], i=i, W=W: e.tensor_tensor(out=gu[:, i, 0:W], in0=sgt[:, 0:W], in1=y[:, 0:W], op=ALU.mult), reads=[sgt, ycs[1]], writes=[gu.sub(i)])

        def down(hsrc, hdst, t0, W, s):
            o0 = 1 if t0 == 0 else 0
            for oc in range(8):
                pd = ps_dn[cnt["d"] % 4]
                cnt["d"] += 1
                hrt = hr[cnt["h"] % 2]
                cnt["h"] += 1
                kw = {"allow_slow_non_contiguous": True} if W - o0 == 1 else {}
                P.dma("sp", hrt[:, o0:W], hsrc[oc * 128:(oc + 1) * 128, t0 - 1 + o0:t0 - 1 + W], writes=[hrt], **kw)
                for i in range(22):
                    P.op("pe", lambda e, i=i, oc=oc, pd=pd, W=W: e.matmul(pd[:, 0:W], wd[:, i, oc * 128:(oc + 1) * 128], gu[:, i, 0:W], start=(i == 0), stop=(i == 21)),
                         reads=[wd, gu.sub(i)], writes=[pd])
                P.op("dve", lambda e, pd=pd, hrt=hrt, oc=oc, W=W, o0=o0: e.scalar_tensor_tensor(out=hrt[:, o0:W], in0=pd[:, o0:W], scalar=self.modcol(l, 5, oc, s), in1=hrt[:, o0:W], op0=ALU.mult, op1=ALU.add),
                     reads=[pd, hrt, self.mod], writes=[hrt])
                P.dma("sp", hdst[oc * 128:(oc + 1) * 128, t0 - 1 + o0:t0 - 1 + W], hrt[:, o0:W], reads=[hrt], **kw)

        def seq(hsrc, hdst, T_, s):
            P.op("pool", lambda e: e.memset(carry[:], 0.0), writes=[carry])
            tiles = [(t0, min(512, T_ - t0), False) for t0 in range(0, T_, 512)] + [(T_, 1, True)]
            self.norm_mod(hsrc, 0, tiles[0][1], self.gsf, l, 3, s, a, ht, sq, rs, ps_ss)
            for j, (t0, W, flush) in enumerate(tiles):
                up_conv(t0, W, flush)
                if j + 1 < len(tiles) and not tiles[j + 1][2]:
                    self.norm_mod(hsrc, tiles[j + 1][0], tiles[j + 1][1], self.gsf, l, 3, s, a, ht, sq, rs, ps_ss)
                down(hsrc, hdst, t0, W, s)

        seq(src, dst, T_lat, 0)
        if do_ctx:
            seq(csrc, cdst, CTX, 1)
        P.barrier()
        P.release(m)

    def proj_res(self, hsrc, hdst, t0, W, l, s, wmat, zin, nk, gate_vi, hr, ps_dn, cnt, shifted):
        P = self.P
        tb = t0 - 1 if shifted else t0
        o0 = 1 if (shifted and t0 == 0) else 0
        for oc in range(8):
            pd = ps_dn[cnt["d"] % len(ps_dn)]
            cnt["d"] += 1
            hrt = hr[cnt["h"] % len(hr)]
            cnt["h"] += 1
            kw = {"allow_slow_non_contiguous": True} if W - o0 == 1 else {}
            P.dma("sp", hrt[:, o0:W], hsrc[oc * 128:(oc + 1) * 128, tb + o0:tb + W], writes=[hrt], **kw)
            for i in range(nk):
                P.op("pe", lambda e, i=i, oc=oc, pd=pd: e.matmul(pd[:, 0:W], wmat[:, i, oc * 128:(oc + 1) * 128], zin[:, i, 0:W], start=(i == 0), stop=(i == nk - 1)),
                     reads=[wmat, zin.sub(i)], writes=[pd])
            P.op("dve", lambda e, pd=pd, hrt=hrt, oc=oc: e.scalar_tensor_tensor(out=hrt[:, o0:W], in0=pd[:, o0:W], scalar=self.modcol(l, gate_vi, oc, s), in1=hrt[:, o0:W], op0=ALU.mult, op1=ALU.add),
                 reads=[pd, hrt, self.mod], writes=[hrt])
            P.dma("sp", hdst[oc * 128:(oc + 1) * 128, tb + o0:tb + W], hrt[:, o0:W], reads=[hrt], **kw)

    def mixer_sc(self, l, src, dst, csrc, cdst, T_lat=S):
        P = self.P
        m = P.mark()
        wi = P.alloc("wi", [128, 8, 3 * D], BF16)
        wo = P.alloc("wo", [128, 8, D], BF16)
        m2 = P.mark()
        stage = [P.alloc(f"stg{i}", [128, 2048], F32) for i in range(2)]
        k = 0
        for kc in range(8):
            k = self.load_cast(wi, kc, self.sc_w_in[kc * 128:(kc + 1) * 128, :], 3 * D, stage, k)
        for kc in range(8):
            k = self.load_cast(wo, kc, self.sc_w_out[kc * 128:(kc + 1) * 128, :], D, stage, k)
        P.barrier()
        P.release(m2)
        ht = P.alloc("ht", [128, 8, 512], F32)
        sq = [P.alloc(f"sq{i}", [128, 512], BF16) for i in range(2)]
        rs = P.alloc("rs", [128, 512], F32)
        a = P.alloc("a", [128, 8, 512], BF16)
        cu = [P.alloc(f"cu{i}", [128, 514], BF16) for i in range(2)]
        us = [P.alloc(f"us{i}", [128, 512], F32) for i in range(2)]
        bb = [P.alloc(f"bb{i}", [128, 513], BF16) for i in range(2)]
        yc = [P.alloc(f"yc{i}", [128, 512], BF16) for i in range(2)]
        carry_cu = P.alloc("carry_cu", [128, 8, 2], BF16)
        carry_b = P.alloc("carry_b", [128, 8, 1], BF16)
        z = P.alloc("z", [128, 8, 512], BF16)
        hr = [P.alloc(f"hr{i}", [128, 512], F32) for i in range(2)]
        cw = VCOLS["sc_conv"]
        vt = self.vt
        ps_ss = self.ps[0]
        ps_in = self.ps[1:5]
        ps_dn = self.ps[5:8]
        cnt = {"u": 0, "p": 0, "d": 0, "h": 0}

        def inproj(pt, col0, W):
            for c in range(8):
                P.op("pe", lambda e, c=c: e.matmul(pt[:, 0:W], wi[:, c, col0:col0 + 128], a[:, c, 0:W], start=(c == 0), stop=(c == 7)), reads=[wi, a.sub(c)], writes=[pt])

        def body(t0, W, flush):
            for i in range(8):
                cut = cu[cnt["u"] % 2]
                ust = us[cnt["u"] % 2]
                bbt = bb[cnt["u"] % 2]
                y = yc[cnt["u"] % 2]
                cnt["u"] += 1
                P.op("pool", lambda e, cut=cut, i=i: e.tensor_copy(cut[:, 0:2], carry_cu[:, i, :]), reads=[carry_cu.sub(i)], writes=[cut])
                P.op("pool", lambda e, bbt=bbt, i=i: e.tensor_copy(bbt[:, 0:1], carry_b[:, i, :]), reads=[carry_b.sub(i)], writes=[bbt])
                if flush:
                    P.op("pool", lambda e, cut=cut: e.memset(cut[:, 2:3], 0.0), writes=[cut])
                else:
                    pb, pc, pu = (ps_in[(cnt["p"] + j) % 4] for j in range(3))
                    cnt["p"] += 3
                    inproj(pb, i * 128, W)
                    inproj(pc, D + i * 128, W)
                    inproj(pu, 2 * D + i * 128, W)
                    P.op("act", lambda e, ust=ust, pu=pu: e.activation(out=ust[:, 0:W], in_=pu[:, 0:W], func=AF.Copy), reads=[pu], writes=[ust])
                    P.op("act", lambda e, bbt=bbt, pb=pb: e.activation(out=bbt[:, 1:1 + W], in_=pb[:, 0:W], func=AF.Copy), reads=[pb], writes=[bbt])
                    P.op("dve", lambda e, cut=cut, pc=pc, ust=ust: e.tensor_tensor(out=cut[:, 2:2 + W], in0=pc[:, 0:W], in1=ust[:, 0:W], op=ALU.mult), reads=[pc, ust], writes=[cut])
                    P.op("pool", lambda e, cut=cut, i=i: e.tensor_copy(carry_cu[:, i, :], cut[:, W:W + 2]), reads=[cut], writes=[carry_cu.sub(i)])
                    P.op("pool", lambda e, bbt=bbt, i=i: e.tensor_copy(carry_b[:, i, :], bbt[:, W:W + 1]), reads=[bbt], writes=[carry_b.sub(i)])
                P.op("dve", lambda e, cut=cut, y=y, i=i: e.tensor_scalar(y[:, 0:W], cut[:, 0:W], vt[:, cw + i:cw + i + 1], None, op0=ALU.mult), reads=[cut, vt], writes=[y])
                P.op("dve", lambda e, cut=cut, y=y, i=i: e.scalar_tensor_tensor(out=y[:, 0:W], in0=cut[:, 1:1 + W], scalar=vt[:, cw + 8 + i:cw + 8 + i + 1], in1=y[:, 0:W], op0=ALU.mult, op1=ALU.add), reads=[cut, vt, y], writes=[y])
                P.op("dve", lambda e, cut=cut, y=y, i=i: e.scalar_tensor_tensor(out=y[:, 0:W], in0=cut[:, 2:2 + W], scalar=vt[:, cw + 16 + i:cw + 16 + i + 1], in1=y[:, 0:W], op0=ALU.mult, op1=ALU.add), reads=[cut, vt, y], writes=[y])
                P.op("dve", lambda e, bbt=bbt, y=y, i=i: e.tensor_tensor(out=z[:, i, 0:W], in0=y[:, 0:W], in1=bbt[:, 0:W], op=ALU.mult), reads=[y, bbt], writes=[z.sub(i)])

        def seq(hsrc, hdst, T_, s):
            P.op("pool", lambda e: e.memset(carry_cu[:], 0.0), writes=[carry_cu])
            P.op("pool", lambda e: e.memset(carry_b[:], 0.0), writes=[carry_b])
            tiles = [(t0, min(512, T_ - t0), False) for t0 in range(0, T_, 512)] + [(T_, 1, True)]
            self.norm_mod(hsrc, 0, tiles[0][1], self.gsm, l, 0, s, a, ht, sq, rs, ps_ss)
            for j, (t0, W, flush) in enumerate(tiles):
                body(t0, W, flush)
                if j + 1 < len(tiles) and not tiles[j + 1][2]:
                    self.norm_mod(hsrc, tiles[j + 1][0], tiles[j + 1][1], self.gsm, l, 0, s, a, ht, sq, rs, ps_ss)
                self.proj_res(hsrc, hdst, t0, W, l, s, wo, z, 8, 2, hr, ps_dn, cnt, True)

        seq(src, dst, T_lat, 0)
        seq(csrc, cdst, CTX, 1)
        P.barrier()
        P.release(m)

    def load_qkv_weights(self, w_qkv, w_o, hd, nq):
        P = self.P
        nk = 1536 - nq - (1536 - nq) // 2
        wq = P.alloc("wq", [128, 8, nq], BF16)
        wqs = P.alloc("wqs", [128, 8, nq], BF16)
        wk = P.alloc("wk", [128, 8, 256], BF16)
        wks = P.alloc("wks", [128, 8, 256], BF16)
        wv = P.alloc("wv", [128, 8, 256], BF16)
        wo = P.alloc("wo", [128, 8, D], BF16)
        m2 = P.mark()
        stage = [P.alloc(f"stg{i}", [128, 2048], F32) for i in range(2)]
        h2 = hd // 2
        for kc in range(8):
            st = stage[kc % 2]
            P.dma("sp", st[:, 0:1536], w_qkv[kc * 128:(kc + 1) * 128, :], writes=[st])
            P.op("dve", lambda e, st=st, kc=kc: e.tensor_copy(wq[:, kc, :], st[:, 0:nq]), reads=[st], writes=[wq.sub(kc)])
            P.op("pool", lambda e, st=st, kc=kc: e.tensor_copy(wk[:, kc, :], st[:, nq:nq + 256]), reads=[st], writes=[wk.sub(kc)])
            P.op("pool", lambda e, st=st, kc=kc: e.tensor_copy(wv[:, kc, :], st[:, nq + 256:nq + 512]), reads=[st], writes=[wv.sub(kc)])
            for (dstw, c0, n) in ((wqs, 0, nq), (wks, nq, 256)):
                for t in range(2):
                    P.op("dve" if t == 0 else "pool",
                         lambda e, st=st, kc=kc, dstw=dstw, c0=c0, n=n, t=t: e.tensor_copy(
                             dstw[:, kc, :].rearrange("p (h t d) -> p h t d", t=2, d=h2)[:, :, t, :],
                             st[:, c0:c0 + n].rearrange("p (h t d) -> p h t d", t=2, d=h2)[:, :, 1 - t, :]),
                         reads=[st], writes=[dstw.sub((kc, t))])
        k = 0
        for kc in range(8):
            k = self.load_cast(wo, kc, w_o[kc * 128:(kc + 1) * 128, :], D, stage, k)
        P.barrier()
        P.release(m2)
        return wq, wqs, wk, wks, wv, wo

    def qk_norm_rope(self, pq, pqs, gcol, gscol, hd, cost, sint, W, out_ap, out_buf, bufs, ps_ss2):
        P = self.P
        sqb, rsb, t1, t2 = bufs
        onesm = self.ones if hd == 128 else self.ones_bd
        vt = self.vt
        P.op("act", lambda e: e.activation(out=sqb[:, 0:W], in_=pq[:, 0:W], func=AF.Square), reads=[pq], writes=[sqb])
        P.op("pe", lambda e: e.matmul(ps_ss2[:, 0:W], onesm[:], sqb[:, 0:W], start=True, stop=True), reads=[onesm, sqb], writes=[ps_ss2])
        P.op("act", lambda e: e.activation(out=rsb[:, 0:W], in_=ps_ss2[:, 0:W], func=AF.Sqrt, bias=self.epsb[:], scale=1.0 / hd), reads=[ps_ss2, self.epsb], writes=[rsb])
        P.op("dve", lambda e: e.reciprocal(rsb[:, 0:W], rsb[:, 0:W]), reads=[rsb], writes=[rsb])
        P.op("dve", lambda e: e.scalar_tensor_tensor(out=t1[:, 0:W], in0=pq[:, 0:W], scalar=vt[:, gcol:gcol + 1], in1=rsb[:, 0:W], op0=ALU.mult, op1=ALU.mult), reads=[pq, vt, rsb], writes=[t1])
        P.op("dve", lambda e: e.scalar_tensor_tensor(out=t2[:, 0:W], in0=pqs[:, 0:W], scalar=vt[:, gscol:gscol + 1], in1=rsb[:, 0:W], op0=ALU.mult, op1=ALU.mult), reads=[pqs, vt, rsb], writes=[t2])
        P.op("dve", lambda e: e.tensor_tensor(out=t1[:, 0:W], in0=t1[:, 0:W], in1=cost[:, 0:W], op=ALU.mult), reads=[t1, cost], writes=[t1])
        P.op("dve", lambda e: e.tensor_tensor(out=t2[:, 0:W], in0=t2[:, 0:W], in1=sint[:, 0:W], op=ALU.mult), reads=[t2, sint], writes=[t2])
        P.op("dve", lambda e: e.tensor_tensor(out=out_ap, in0=t1[:, 0:W], in1=t2[:, 0:W], op=ALU.add), reads=[t1, t2], writes=[out_buf])

    def mixer_dense(self, l, src, dst, csrc, cdst, T_lat=S):
        P = self.P
        m = P.mark()
        HD = 128
        NKB = (T_lat + CTX) // 128
        wq, wqs, wk, wks, wv, wo = self.load_qkv_weights(self.ax_w_qkv, self.ax_w_o, HD, 1024)
        KT = P.alloc("KT", [128, 2, T_lat + CTX], BF16)
        V = P.alloc("V", [128, NKB, 256], BF16)
        ht = P.alloc("ht", [128, 8, 512], F32)
        sq = [P.alloc(f"sq{i}", [128, 512], BF16) for i in range(2)]
        rs = P.alloc("rs", [128, 512], F32)
        a = P.alloc("a", [128, 8, 512], BF16)
        cost = P.alloc("cost", [128, 512], F32)
        sint = P.alloc("sint", [128, 512], F32)
        nb = (P.alloc("nsq", [128, 512], BF16), P.alloc("nrs", [128, 512], F32), P.alloc("nt1", [128, 512], F32), P.alloc("nt2", [128, 512], F32))
        Q = P.alloc("Q", [128, 8, 512], BF16)
        E = [P.alloc(f"E{i}", [128, 512], BF16) for i in range(3)]
        rden = P.alloc("rden", [128, 512], F32)
        att = P.alloc("att", [128, 8, 512], BF16)
        hr = [P.alloc(f"hr{i}", [128, 512], F32) for i in range(2)]
        vt = self.vt
        ps = self.ps
        gq, gqs, gk, gks = VCOLS["ax_qg"], VCOLS["ax_qgs"], VCOLS["ax_kg"], VCOLS["ax_kgs"]
        cnt = {"d": 0, "h": 0, "e": 0, "s": 0, "o": 0, "p": 0, "v": 0}

        def proj(pt, w, col0, W):
            for c in range(8):
                P.op("pe", lambda e, c=c: e.matmul(pt[:, 0:W], w[:, c, col0:col0 + 128], a[:, c, 0:W], start=(c == 0), stop=(c == 7)), reads=[w, a.sub(c)], writes=[pt])

        def load_tables(pos0, W):
            P.dma("sp", cost[:, 0:W], self.cos128[:, pos0:pos0 + W], writes=[cost])
            P.dma("sp", sint[:, 0:W], self.sin128[:, pos0:pos0 + W], writes=[sint])

        def phase_a(hsrc, T_, s, pos_base):
            for t0 in range(0, T_, 512):
                W = min(512, T_ - t0)
                pos0 = pos_base + t0
                self.norm_mod(hsrc, t0, W, self.gsm, l, 0, s, a, ht, sq, rs, ps[0])
                load_tables((S if s == 1 else 0) + t0, W)
                for g in range(2):
                    pk, pks = ps[1 + 2 * (cnt["p"] % 2)], ps[2 + 2 * (cnt["p"] % 2)]
                    cnt["p"] += 1
                    proj(pk, wk, g * 128, W)
                    proj(pks, wks, g * 128, W)
                    self.qk_norm_rope(pk, pks, gk, gks, HD, cost, sint, W, KT[:, g, pos0:pos0 + W], KT.sub((g, pos0)), nb, ps[5])
                for blk in range(W // 128):
                    pv = ps[6 + cnt["v"] % 2]
                    cnt["v"] += 1
                    for c in range(8):
                        P.op("pe", lambda e, c=c, blk=blk, pv=pv: e.matmul(pv[:, 0:256], a[:, c, blk * 128:(blk + 1) * 128], wv[:, c, :], start=(c == 0), stop=(c == 7)), reads=[wv, a.sub(c)], writes=[pv])
                    kb = pos0 // 128 + blk
                    P.op("act", lambda e, pv=pv, kb=kb: e.activation(out=V[:, kb, :], in_=pv[:, 0:256], func=AF.Copy), reads=[pv], writes=[V.sub(kb)])

        phase_a(src, T_lat, 0, 0)
        phase_a(csrc, CTX, 1, T_lat)

        scale = float(HD) ** -0.5

        def phase_b(hsrc, hdst, T_, s, pos_base, kblocks):
            for t0 in range(0, T_, 512):
                W = min(512, T_ - t0)
                pos0 = pos_base + t0
                self.norm_mod(hsrc, t0, W, self.gsm, l, 0, s, a, ht, sq, rs, ps[0])
                load_tables((S if s == 1 else 0) + t0, W)
                for h in range(8):
                    proj(ps[1], wq, h * 128, W)
                    proj(ps[2], wqs, h * 128, W)
                    self.qk_norm_rope(ps[1], ps[2], gq, gqs, HD, cost, sint, W, Q[:, h, 0:W], Q.sub(h), nb, ps[0])
                for h in range(8):
                    g = h // 4
                    po = ps[5 + cnt["o"] % 2]
                    pden = ps[7] if cnt["o"] % 2 == 0 else ps[0]
                    cnt["o"] += 1
                    for j, kb in enumerate(kblocks):
                        pS = ps[3 + cnt["s"] % 2]
                        cnt["s"] += 1
                        Et = E[cnt["e"] % 3]
                        cnt["e"] += 1
                        P.op("pe", lambda e, kb=kb, pS=pS, g=g, h=h: e.matmul(pS[:, 0:W], KT[:, g, kb * 128:(kb + 1) * 128], Q[:, h, 0:W], start=True, stop=True), reads=[KT, Q.sub(h)], writes=[pS])
                        P.op("act", lambda e, pS=pS, Et=Et: e.activation(out=Et[:, 0:W], in_=pS[:, 0:W], func=AF.Exp, scale=scale), reads=[pS], writes=[Et])
                        st, sp_ = (j == 0), (j == len(kblocks) - 1)
                        P.op("pe", lambda e, kb=kb, Et=Et, po=po, g=g, st=st, sp_=sp_: e.matmul(po[:, 0:W], V[:, kb, g * 128:(g + 1) * 128], Et[:, 0:W], start=st, stop=sp_), reads=[V, Et], writes=[po])
                        P.op("pe", lambda e, Et=Et, pden=pden, st=st, sp_=sp_: e.matmul(pden[:, 0:W], self.ones[:], Et[:, 0:W], start=st, stop=sp_), reads=[self.ones, Et], writes=[pden])
                    P.op("dve", lambda e, pden=pden: e.reciprocal(rden[:, 0:W], pden[:, 0:W]), reads=[pden], writes=[rden])
                    P.op("dve", lambda e, po=po, h=h: e.tensor_tensor(out=att[:, h, 0:W], in0=po[:, 0:W], in1=rden[:, 0:W], op=ALU.mult), reads=[po, rden], writes=[att.sub(h)])
                self.proj_res(hsrc, hdst, t0, W, l, s, wo, att, 8, 2, hr, [ps[1], ps[2]], cnt, False)

        phase_b(src, dst, T_lat, 0, 0, list(range(NKB)))
        phase_b(csrc, cdst, CTX, 1, T_lat, list(range(T_lat // 128, NKB)))
        P.barrier()
        P.release(m)

    def mixer_win(self, l, src, dst, csrc, cdst, T_lat=S):
        P = self.P
        m = P.mark()
        HD = 64
        NKB = (T_lat + CTX) // 128
        NLB = T_lat // 128
        wq = P.alloc("wq", [128, 8, 1024], BF16)
        wqs = P.alloc("wqs", [128, 8, 1024], BF16)
        wk = P.alloc("wk", [128, 8, 256], BF16)
        wks = P.alloc("wks", [128, 8, 256], BF16)
        wv = P.alloc("wv", [128, 8, 256], BF16)
        wo = P.alloc("wo", [128, 8, D], BF16)
        mk = P.alloc("mk", [128, 2, 512], BF16)
        es = P.alloc("es", [128, 16], F32)
        m2 = P.mark()
        stage = [P.alloc(f"stg{i}", [128, 2048], F32) for i in range(2)]
        k = 0
        for kc in range(8):
            st = stage[kc % 2]
            P.dma("sp", st[:, 0:1536], self.win_w_qkv[kc * 128:(kc + 1) * 128, :], writes=[st])
            P.op("pool", lambda e, st=st, kc=kc: e.tensor_copy(wv[:, kc, :], st[:, 1280:1536]), reads=[st], writes=[wv.sub(kc)])
            for (dw, dws, c0, n) in ((wq, wqs, 0, 1024), (wk, wks, 1024, 256)):
                for t in range(2):
                    P.op("dve" if t == 0 else "pool",
                         lambda e, st=st, kc=kc, dw=dw, c0=c0, n=n, t=t: e.tensor_copy(
                             dw[:, kc, :].rearrange("p (i t d) -> p i t d", t=2, d=64)[:, :, t, :],
                             st[:, c0:c0 + n].rearrange("p (t i d) -> p t i d", t=2, d=64)[:, t, :, :]),
                         reads=[st], writes=[dw.sub((kc, t))])
                    for u in range(2):
                        P.op("dve" if u == 0 else "pool",
                             lambda e, st=st, kc=kc, dws=dws, c0=c0, n=n, t=t, u=u: e.tensor_copy(
                                 dws[:, kc, :].rearrange("p (i t u d) -> p i t u d", t=2, u=2, d=32)[:, :, t, u, :],
                                 st[:, c0:c0 + n].rearrange("p (t i u d) -> p t i u d", t=2, u=2, d=32)[:, t, :, 1 - u, :]),
                             reads=[st], writes=[dws.sub((kc, t, u))])
        for kc in range(8):
            k = self.load_cast(wo, kc, self.win_w_o[kc * 128:(kc + 1) * 128, :], D, stage, k)
        st = stage[0]
        P.dma("sp", st[:, 0:1024], self.wmask[:, :], writes=[st])
        P.op("dve", lambda e: e.tensor_copy(mk[:].rearrange("p a b -> p (a b)"), st[:, 0:1024]), reads=[st], writes=[mk])
        sc0 = VCOLS["win_sink"]
        P.op("act", lambda e: e.activation(out=es[:], in_=self.vt[:, sc0:sc0 + 16], func=AF.Exp), reads=[self.vt], writes=[es])
        P.barrier()
        P.release(m2)
        KT = P.alloc("KT", [128, 2, T_lat + CTX], BF16)
        V = P.alloc("V", [128, NKB, 256], BF16)
        ht = P.alloc("ht", [128, 8, 512], F32)
        sq = [P.alloc(f"sq{i}", [128, 512], BF16) for i in range(2)]
        rs = P.alloc("rs", [128, 512], F32)
        a = P.alloc("a", [128, 8, 512], BF16)
        cost = P.alloc("cost", [128, 512], F32)
        sint = P.alloc("sint", [128, 512], F32)
        nb = (P.alloc("nsq", [128, 512], BF16), P.alloc("nrs", [128, 512], F32), P.alloc("nt1", [128, 512], F32), P.alloc("nt2", [128, 512], F32))
        Q = P.alloc("Q", [128, 8, 512], BF16)
        E = [P.alloc(f"E{i}", [128, 512], BF16) for i in range(3)]
        dtmp = P.alloc("dtmp", [128, 512], F32)
        rden = P.alloc("rden", [128, 512], F32)
        att = P.alloc("att", [128, 8, 512], BF16)
        hr = [P.alloc(f"hr{i}", [128, 512], F32) for i in range(2)]
        ps = self.ps
        gq, gqs, gk, gks = VCOLS["win_qg"], VCOLS["win_qgs"], VCOLS["win_kg"], VCOLS["win_kgs"]
        cnt = {"d": 0, "h": 0, "e": 0, "s": 0, "o": 0, "p": 0, "v": 0}

        def proj(pt, w, col0, W):
            for c in range(8):
                P.op("pe", lambda e, c=c: e.matmul(pt[:, 0:W], w[:, c, col0:col0 + 128], a[:, c, 0:W], start=(c == 0), stop=(c == 7)), reads=[w, a.sub(c)], writes=[pt])

        def load_tables(pos0, W):
            P.dma("sp", cost[:, 0:W], self.cos64[:, pos0:pos0 + W], writes=[cost])
            P.dma("sp", sint[:, 0:W], self.sin64[:, pos0:pos0 + W], writes=[sint])

        def phase_a(hsrc, T_, s, pos_base):
            for t0 in range(0, T_, 512):
                W = min(512, T_ - t0)
                pos0 = pos_base + t0
                self.norm_mod(hsrc, t0, W, self.gsm, l, 0, s, a, ht, sq, rs, ps[0])
                load_tables((S if s == 1 else 0) + t0, W)
                for j in range(2):
                    pk, pks = ps[1 + 2 * (cnt["p"] % 2)], ps[2 + 2 * (cnt["p"] % 2)]
                    cnt["p"] += 1
                    proj(pk, wk, j * 128, W)
                    proj(pks, wks, j * 128, W)
                    self.qk_norm_rope(pk, pks, gk, gks, HD, cost, sint, W, KT[:, j, pos0:pos0 + W], KT.sub((j, pos0)), nb, ps[5])
                for blk in range(W // 128):
                    pv = ps[6 + cnt["v"] % 2]
                    cnt["v"] += 1
                    for c in range(8):
                        P.op("pe", lambda e, c=c, blk=blk, pv=pv: e.matmul(pv[:, 0:256], a[:, c, blk * 128:(blk + 1) * 128], wv[:, c, :], start=(c == 0), stop=(c == 7)), reads=[wv, a.sub(c)], writes=[pv])
                    kb = pos0 // 128 + blk
                    P.op("act", lambda e, pv=pv, kb=kb: e.activation(out=V[:, kb, :], in_=pv[:, 0:256], func=AF.Copy), reads=[pv], writes=[V.sub(kb)])

        phase_a(src, T_lat, 0, 0)
        phase_a(csrc, CTX, 1, T_lat)
        scale = float(HD) ** -0.5

        def phase_b(hsrc, hdst, T_, s, pos_base, windowed):
            for t0 in range(0, T_, 512):
                W = min(512, T_ - t0)
                self.norm_mod(hsrc, t0, W, self.gsm, l, 0, s, a, ht, sq, rs, ps[0])
                load_tables((S if s == 1 else 0) + t0, W)
                for i in range(8):
                    proj(ps[1], wq, i * 128, W)
                    proj(ps[2], wqs, i * 128, W)
                    self.qk_norm_rope(ps[1], ps[2], gq, gqs, HD, cost, sint, W, Q[:, i, 0:W], Q.sub(i), nb, ps[0])
                for qbl in range(W // 128):
                    qs = slice(qbl * 128, (qbl + 1) * 128)
                    if windowed:
                        qb = t0 // 128 + qbl
                        kbl = []
                        if qb > 0:
                            kbl.append((qb - 1, 0))
                        kbl.append((qb, None))
                        if qb < NLB - 1:
                            kbl.append((qb + 1, 1))
                        kbl += [(NLB, None), (NLB + 1, None)]
                    else:
                        kbl = [(NLB, None), (NLB + 1, None)]
                    for g in range(4):
                        half, j, i0 = g // 2, g % 2, 4 * (g % 2)
                        hp = slice(half * 64, (half + 1) * 64)
                        po = ps[5 + cnt["o"] % 2]
                        pden = ps[7] if cnt["o"] % 2 == 0 else ps[0]
                        cnt["o"] += 1
                        for n_, (kb, mi) in enumerate(kbl):
                            pS = ps[3 + cnt["s"] % 2]
                            cnt["s"] += 1
                            Et = E[cnt["e"] % 3]
                            cnt["e"] += 1
                            P.op("pe", lambda e, kb=kb, pS=pS, hp=hp, j=j, i0=i0, qs=qs: e.matmul(pS[:, 0:512], KT[hp, j, kb * 128:(kb + 1) * 128], Q[hp, i0:i0 + 4, qs], start=True, stop=True),
                                 reads=[KT, Q], writes=[pS])
                            P.op("act", lambda e, pS=pS, Et=Et: e.activation(out=Et[:, :], in_=pS[:, 0:512], func=AF.Exp, scale=scale), reads=[pS], writes=[Et])
                            if mi is not None:
                                P.op("dve", lambda e, Et=Et, mi=mi: e.tensor_tensor(out=Et[:, :], in0=Et[:, :], in1=mk[:, mi, :], op=ALU.mult), reads=[Et, mk], writes=[Et])
                            st_, sp_ = (n_ == 0), (n_ == len(kbl) - 1)
                            Ev = Et[:, :].rearrange("p (r q) -> p r q", q=128)
                            for par in range(2):
                                P.op("pe", lambda e, kb=kb, Ev=Ev, po=po, g=g, par=par, st_=st_, sp_=sp_: e.matmul(po[par * 64:(par + 1) * 64, 0:256], V[:, kb, g * 64:(g + 1) * 64], Ev[:, par::2, :], start=st_, stop=sp_, tile_position=(0, par * 64)),
                                     reads=[V, Et], writes=[po])
                            P.op("pe", lambda e, Et=Et, pden=pden, st_=st_, sp_=sp_: e.matmul(pden[:, 0:512], self.ones[:], Et[:, :], start=st_, stop=sp_), reads=[self.ones, Et], writes=[pden])
                        for r in range(4):
                            hh = 4 * g + r
                            P.op("dve", lambda e, pden=pden, r=r, hh=hh: e.tensor_scalar(dtmp[:, r * 128:(r + 1) * 128], pden[:, r * 128:(r + 1) * 128], es[:, hh:hh + 1], None, op0=ALU.add), reads=[pden, es], writes=[dtmp])
                        P.op("dve", lambda e: e.reciprocal(rden[:, :], dtmp[:, :]), reads=[dtmp], writes=[rden])
                        for r in range(4):
                            rp = slice((r % 2) * 64, (r % 2 + 1) * 64)
                            cb = (r // 2) * 128
                            P.op("dve", lambda e, po=po, r=r, rp=rp, cb=cb, g=g, qs=qs: e.tensor_tensor(out=att[rp, 2 * g + r // 2, qs], in0=po[rp, cb:cb + 128], in1=rden[rp, r * 128:(r + 1) * 128], op=ALU.mult),
                                 reads=[po, rden], writes=[att.sub(2 * g + r // 2)])
                self.proj_res(hsrc, hdst, t0, W, l, s, wo, att, 8, 2, hr, [ps[1], ps[2]], cnt, False)

        phase_b(src, dst, T_lat, 0, 0, True)
        phase_b(csrc, cdst, CTX, 1, T_lat, False)
        P.barrier()
        P.release(m)

    def mixer_mlstm(self, l, src, dst, csrc, T_lat=S):
        P = self.P
        m = P.mark()
        wi = P.alloc("wi", [128, 8, 3088], BF16)
        wo = P.alloc("wo", [128, 8, D], BF16)
        tri = P.alloc("tri", [128, 2, 128], F32)
        trb = P.alloc("trb", [128, 2, 128], BF16)
        onesf = P.alloc("onesf", [128, 128], F32)
        m2 = P.mark()
        stage = [P.alloc(f"stg{i}", [128, 2048], F32) for i in range(2)]
        k = 0
        for kc in range(8):
            k = self.load_cast(wi, kc, self.ml_w_in[kc * 128:(kc + 1) * 128, :], 3088, stage, k)
        for kc in range(8):
            k = self.load_cast(wo, kc, self.ml_w_out[kc * 128:(kc + 1) * 128, :], D, stage, k)
        P.dma("sp", tri[:].rearrange("p a b -> p (a b)"), self.trimat[:, :], writes=[tri])
        P.op("dve", lambda e: e.tensor_copy(trb[:], tri[:]), reads=[tri], writes=[trb])
        P.op("pool", lambda e: e.memset(onesf[:], 1.0), writes=[onesf])
        P.barrier()
        P.release(m2)
        ht = P.alloc("ht", [128, 8, 512], F32)
        sq = [P.alloc(f"sq{i}", [128, 512], BF16) for i in range(2)]
        rs = P.alloc("rs", [128, 512], F32)
        a = P.alloc("a", [128, 8, 512], BF16)
        QT = P.alloc("QT", [128, 4, 512], BF16)
        KT = P.alloc("KT", [128, 4, 512], BF16)
        Ktm = P.alloc("Ktm", [128, 4, 512], F32)
        Vtm = P.alloc("Vtm", [128, 4, 1024], BF16)
        gtm = P.alloc("gtm", [128, 4, 16], F32)
        sgo = P.alloc("sgo", [128, 8, 512], BF16)
        hb = P.alloc("hb", [128, 8, 512], F32)
        hfT = P.alloc("hfT", [128, 8, 512], F32)
        z = P.alloc("z", [128, 8, 512], BF16)
        hr = [P.alloc(f"hr{i}", [128, 512], F32) for i in range(2)]
        e1 = P.alloc("e1", [128, 4], F32)
        lf = P.alloc("lf", [128, 4], F32)
        bcol = P.alloc("bcol", [128, 4], F32)
        wl = P.alloc("wl", [128, 4], F32)
        wcol = P.alloc("wcol", [128, 4], F32)
        eG = P.alloc("eG", [128, 4], F32)
        Rh = [P.alloc(f"Rh{i}", [128, 128], F32) for i in range(2)]
        Ah = [P.alloc(f"Ah{i}", [128, 128], BF16) for i in range(2)]
        ech = [P.alloc(f"ech{i}", [128, 128], F32) for i in range(2)]
        Ph = [P.alloc(f"Ph{i}", [128, 128], BF16) for i in range(2)]
        Qe = [P.alloc(f"Qe{i}", [128, 128], BF16) for i in range(2)]
        Kw = [P.alloc(f"Kw{i}", [128, 128], BF16) for i in range(2)]
        ad = [P.alloc(f"ad{i}", [128, 128], F32) for i in range(2)]
        Cst = P.alloc("Cst", [128, 4, 256], F32)
        Cb = P.alloc("Cb", [128, 4, 256], BF16)
        nst = P.alloc("nst", [128, 4, 128], F32)
        nbb = P.alloc("nbb", [128, 4, 128], BF16)
        vt = self.vt
        ps = self.ps
        cnt = {"d": 0, "h": 0, "p": 0, "r": 0}
        bg = VCOLS["ml_bg"]
        hn = VCOLS["ml_hn"]
        qscale = 128.0 ** -0.5

        def pp():
            t = ps[1 + cnt["p"] % 2]
            cnt["p"] += 1
            return t

        def project(W, with_o):
            nch = W // 128
            for h in range(4):
                for (dstT, c0, sc) in ((QT, 0, qscale), (KT, 512, 1.0)):
                    pt = pp()
                    for c in range(8):
                        P.op("pe", lambda e, c=c, pt=pt, c0=c0, h=h: e.matmul(pt[:, 0:W], wi[:, c, c0 + h * 128:c0 + (h + 1) * 128], a[:, c, 0:W], start=(c == 0), stop=(c == 7)), reads=[wi, a.sub(c)], writes=[pt])
                    P.op("act", lambda e, pt=pt, dstT=dstT, h=h, sc=sc: e.activation(out=dstT[:, h, 0:W], in_=pt[:, 0:W], func=AF.Copy, scale=sc), reads=[pt], writes=[dstT.sub(h)])
            for ch in range(nch):
                cs = slice(ch * 128, (ch + 1) * 128)
                pt = pp()
                for c in range(8):
                    P.op("pe", lambda e, c=c, pt=pt, cs=cs: e.matmul(pt[:, 0:512], a[:, c, cs], wi[:, c, 512:1024], start=(c == 0), stop=(c == 7)), reads=[wi, a.sub(c)], writes=[pt])
                P.op("act", lambda e, pt=pt, ch=ch: e.activation(out=Ktm[:, ch, :], in_=pt[:, 0:512], func=AF.Copy), reads=[pt], writes=[Ktm.sub(ch)])
                for half in range(2):
                    pt = pp()
                    for c in range(8):
                        P.op("pe", lambda e, c=c, pt=pt, cs=cs, half=half: e.matmul(pt[:, 0:512], a[:, c, cs], wi[:, c, 1024 + half * 512:1536 + half * 512], start=(c == 0), stop=(c == 7)), reads=[wi, a.sub(c)], writes=[pt])
                    P.op("act", lambda e, pt=pt, ch=ch, half=half: e.activation(out=Vtm[:, ch, half * 512:(half + 1) * 512], in_=pt[:, 0:512], func=AF.Copy), reads=[pt], writes=[Vtm.sub(ch)])
                pt = pp()
                for c in range(8):
                    P.op("pe", lambda e, c=c, pt=pt, cs=cs: e.matmul(pt[:, 0:16], a[:, c, cs], wi[:, c, 3072:3088], start=(c == 0), stop=(c == 7)), reads=[wi, a.sub(c)], writes=[pt])
                P.op("dve", lambda e, pt=pt, ch=ch: e.tensor_tensor(out=gtm[:, ch, :], in0=pt[:, 0:16], in1=vt[:, bg:bg + 16], op=ALU.add), reads=[pt, vt], writes=[gtm.sub(ch)])
            if with_o:
                for c8 in range(8):
                    pt = pp()
                    for c in range(8):
                        P.op("pe", lambda e, c=c, pt=pt, c8=c8: e.matmul(pt[:, 0:W], wi[:, c, 2048 + c8 * 128:2048 + (c8 + 1) * 128], a[:, c, 0:W], start=(c == 0), stop=(c == 7)), reads=[wi, a.sub(c)], writes=[pt])
                    P.op("act", lambda e, pt=pt, c8=c8: e.activation(out=sgo[:, c8, 0:W], in_=pt[:, 0:W], func=AF.Sigmoid), reads=[pt], writes=[sgo.sub(c8)])

        def core(ch, dirn, with_out, out_tile):
            cs = slice(ch * 128, (ch + 1) * 128)
            g0 = dirn * 8
            psA, pcr, pS, pnum, pden = ps[0], ps[3], ps[4], (ps[5], ps[6]), ps[7]
            P.op("act", lambda e: e.activation(out=e1[:], in_=gtm[:, ch, g0 + 4:g0 + 8], func=AF.Exp, scale=-1.0), reads=[gtm.sub(ch)], writes=[e1])
            P.op("act", lambda e: e.activation(out=e1[:], in_=e1[:], func=AF.Ln, bias=1.0, scale=1.0), reads=[e1], writes=[e1])
            P.op("dve", lambda e: e.tensor_scalar(lf[:], e1[:], -1.0, None, op0=ALU.mult), reads=[e1], writes=[lf])
            P.op("pe", lambda e: e.matmul(psA[:, 0:4], tri[:, dirn, :], lf[:], start=True, stop=True), reads=[tri, lf], writes=[psA])
            P.op("pe", lambda e: e.matmul(psA[:, 4:8], onesf[:], lf[:], start=True, stop=True), reads=[onesf, lf], writes=[psA])
            P.op("dve", lambda e: e.tensor_tensor(out=bcol[:], in0=gtm[:, ch, g0:g0 + 4], in1=psA[:, 0:4], op=ALU.subtract), reads=[gtm.sub(ch), psA], writes=[bcol])
            P.op("dve", lambda e: e.tensor_tensor(out=wl[:], in0=bcol[:], in1=psA[:, 4:8], op=ALU.add), reads=[bcol, psA], writes=[wl])
            P.op("act", lambda e: e.activation(out=wcol[:], in_=wl[:], func=AF.Exp), reads=[wl], writes=[wcol])
            P.op("act", lambda e: e.activation(out=eG[:], in_=psA[:, 4:8], func=AF.Exp), reads=[psA], writes=[eG])
            for h in range(4):
                i2 = cnt["r"] % 2
                cnt["r"] += 1
                R, A, ec, Pm, Qm, Kwm, adm = Rh[i2], Ah[i2], ech[i2], Ph[i2], Qe[i2], Kw[i2], ad[i2]
                hs_ = slice(h * 128, (h + 1) * 128)
                P.op("dve", lambda e, R=R, h=h: e.tensor_scalar(R[:], tri[:, dirn, :], lf[:, h:h + 1], None, op0=ALU.mult), reads=[tri, lf], writes=[R])
                P.op("pe", lambda e, R=R, hs_=hs_: e.matmul(pcr[:, hs_], onesf[:], R[:], start=True, stop=True), reads=[onesf, R], writes=[pcr])
                P.op("act", lambda e, A=A, hs_=hs_, h=h: e.activation(out=A[:], in_=pcr[:, hs_], func=AF.Exp, bias=bcol[:, h:h + 1], scale=1.0), reads=[pcr, bcol], writes=[A])
                P.op("act", lambda e, ec=ec, hs_=hs_: e.activation(out=ec[:], in_=pcr[:, hs_], func=AF.Exp), reads=[pcr], writes=[ec])
                P.op("pe", lambda e, h=h, hs_=hs_: e.matmul(pS[:, hs_], KT[:, h, cs], QT[:, h, cs], start=True, stop=True), reads=[KT.sub(h), QT.sub(h)], writes=[pS])
                P.op("dve", lambda e, A=A: e.tensor_tensor(out=A[:], in0=A[:], in1=trb[:, dirn, :], op=ALU.mult), reads=[A, trb], writes=[A])
                P.op("dve", lambda e, A=A, Pm=Pm, hs_=hs_: e.tensor_tensor(out=Pm[:], in0=pS[:, hs_], in1=A[:], op=ALU.mult), reads=[pS, A], writes=[Pm])
                if with_out:
                    P.op("dve", lambda e, Qm=Qm, ec=ec, h=h: e.tensor_tensor(out=Qm[:], in0=QT[:, h, cs], in1=ec[:], op=ALU.mult), reads=[QT.sub(h), ec], writes=[Qm])
                    pn = pnum[h // 2]
                    for vc in range(2):
                        ncol = slice((h % 2) * 256 + vc * 128, (h % 2) * 256 + (vc + 1) * 128)
                        P.op("pe", lambda e, pn=pn, ncol=ncol, h=h, vc=vc, Pm=Pm: e.matmul(pn[:, ncol], Vtm[:, ch, h * 256 + vc * 128:h * 256 + (vc + 1) * 128], Pm[:], start=True, stop=False), reads=[Vtm.sub(ch), Pm], writes=[pn])
                        P.op("pe", lambda e, pn=pn, ncol=ncol, h=h, vc=vc, Qm=Qm: e.matmul(pn[:, ncol], Cb[:, h, vc * 128:(vc + 1) * 128], Qm[:], start=False, stop=True), reads=[Cb.sub(h), Qm], writes=[pn])
                    P.op("pe", lambda e, hs_=hs_, Pm=Pm: e.matmul(pden[:, hs_], self.ones[:], Pm[:], start=True, stop=False), reads=[self.ones, Pm], writes=[pden])
                    P.op("pe", lambda e, hs_=hs_, Qm=Qm, h=h: e.matmul(pden[:, hs_], nbb[:, h, :], Qm[:], start=False, stop=True), reads=[nbb.sub(h), Qm], writes=[pden])
                    P.op("act", lambda e, adm=adm, hs_=hs_: e.activation(out=adm[:], in_=pden[:, hs_], func=AF.Abs), reads=[pden], writes=[adm])
                    P.op("dve", lambda e, adm=adm: e.tensor_scalar_max(adm[:], adm[:], 1.0), reads=[adm], writes=[adm])
                    P.op("dve", lambda e, adm=adm: e.reciprocal(adm[:], adm[:]), reads=[adm], writes=[adm])
                    for vc in range(2):
                        ncol = slice((h % 2) * 256 + vc * 128, (h % 2) * 256 + (vc + 1) * 128)
                        P.op("dve", lambda e, pn=pn, ncol=ncol, adm=adm, h=h, vc=vc: e.tensor_tensor(out=out_tile[:, 2 * h + vc, cs], in0=pn[:, ncol], in1=adm[:], op=ALU.mult), reads=[pn, adm], writes=[out_tile.sub((2 * h + vc, ch))])
                P.op("dve", lambda e, Kwm=Kwm, h=h: e.tensor_scalar(Kwm[:], Ktm[:, ch, h * 128:(h + 1) * 128], wcol[:, h:h + 1], None, op0=ALU.mult), reads=[Ktm.sub(ch), wcol], writes=[Kwm])
                pc = ps[1 + h // 2]
                ccol = slice((h % 2) * 256, (h % 2 + 1) * 256)
                P.op("pe", lambda e, pc=pc, ccol=ccol, Kwm=Kwm, h=h: e.matmul(pc[:, ccol], Kwm[:], Vtm[:, ch, h * 256:(h + 1) * 256], start=True, stop=True), reads=[Kwm, Vtm.sub(ch)], writes=[pc])
                P.op("pe", lambda e, Kwm=Kwm, hs_=hs_: e.matmul(psA[:, 128 + hs_.start // 4 * 0 + 0:128 + 0 + 128] if False else ps[0][:, 128:256], Kwm[:], self.ones[:], start=True, stop=True), reads=[Kwm, self.ones], writes=[psA])
                P.op("dve", lambda e, pc=pc, ccol=ccol, h=h: e.scalar_tensor_tensor(out=Cst[:, h, :], in0=Cst[:, h, :], scalar=eG[:, h:h + 1], in1=pc[:, ccol], op0=ALU.mult, op1=ALU.add), reads=[Cst.sub(h), eG, pc], writes=[Cst.sub(h)])
                P.op("dve", lambda e, h=h: e.scalar_tensor_tensor(out=nst[:, h, :], in0=nst[:, h, :], scalar=eG[:, h:h + 1], in1=psA[:, 128:256], op0=ALU.mult, op1=ALU.add), reads=[nst.sub(h), eG, psA], writes=[nst.sub(h)])
                P.op("act", lambda e, h=h: e.activation(out=Cb[:, h, :], in_=Cst[:, h, :], func=AF.Copy), reads=[Cst.sub(h)], writes=[Cb.sub(h)])
                P.op("pool", lambda e, h=h: e.tensor_copy(nbb[:, h, :], nst[:, h, :]), reads=[nst.sub(h)], writes=[nbb.sub(h)])

        def reset_state():
            P.op("pool", lambda e: e.memset(Cst[:], 0.0), writes=[Cst])
            P.op("pool", lambda e: e.memset(nst[:], 0.0), writes=[nst])
            P.op("pool", lambda e: e.memset(Cb[:], 0.0), writes=[Cb])
            P.op("pool", lambda e: e.memset(nbb[:], 0.0), writes=[nbb])

        reset_state()
        self.norm_mod(csrc, 0, CTX, self.gsm, l, 0, 1, a, ht, sq, rs, ps[0])
        project(CTX, False)
        for ch in range(2):
            core(ch, 0, False, None)
        for t0 in range(0, T_lat, 512):
            self.norm_mod(src, t0, 512, self.gsm, l, 0, 0, a, ht, sq, rs, ps[0])
            project(512, False)
            for ch in range(4):
                core(ch, 0, True, hb)
            P.dma("sp", self.hf[:, t0:t0 + 512].rearrange("(c p) t -> p c t", p=128), hb[:], reads=[hb], writes=[self.hf.sub(t0)])
        reset_state()
        self.norm_mod(csrc, 0, CTX, self.gsm, l, 0, 1, a, ht, sq, rs, ps[0])
        project(CTX, False)
        for ch in (1, 0):
            core(ch, 1, False, None)
        for t0 in range(T_lat - 512, -1, -512):
            self.norm_mod(src, t0, 512, self.gsm, l, 0, 0, a, ht, sq, rs, ps[0])
            project(512, True)
            P.dma("sp", hfT[:], self.hf[:, t0:t0 + 512].rearrange("(c p) t -> p c t", p=128), reads=[self.hf.sub(t0)], writes=[hfT])
            for ch in (3, 2, 1, 0):
                core(ch, 1, True, hb)
            for h in range(4):
                for vc in range(2):
                    c8 = 2 * h + vc
                    P.op("dve", lambda e, c8=c8: e.tensor_tensor(out=hb[:, c8, :], in0=hb[:, c8, :], in1=hfT[:, c8, :], op=ALU.add), reads=[hb, hfT], writes=[hb])
                    q = sq[vc]
                    P.op("act", lambda e, c8=c8, q=q: e.activation(out=q[:], in_=hb[:, c8, :], func=AF.Square), reads=[hb], writes=[q])
                    P.op("pe", lambda e, q=q, vc=vc: e.matmul(ps[0][:, :], self.ones[:], q[:], start=(vc == 0), stop=(vc == 1)), reads=[self.ones, q], writes=[ps[0]])
                P.op("act", lambda e: e.activation(out=rs[:], in_=ps[0][:, :], func=AF.Sqrt, bias=self.epsb[:], scale=1.0 / 256), reads=[ps[0], self.epsb], writes=[rs])
                P.op("dve", lambda e: e.reciprocal(rs[:], rs[:]), reads=[rs], writes=[rs])
                for vc in range(2):
                    c8 = 2 * h + vc
                    P.op("dve", lambda e, c8=c8, vc=vc: e.scalar_tensor_tensor(out=hb[:, c8, :], in0=hb[:, c8, :], scalar=vt[:, hn + vc:hn + vc + 1], in1=rs[:], op0=ALU.mult, op1=ALU.mult), reads=[hb, vt, rs], writes=[hb])
                    P.op("dve", lambda e, c8=c8: e.tensor_tensor(out=z[:, c8, :], in0=hb[:, c8, :], in1=sgo[:, c8, :], op=ALU.mult), reads=[hb, sgo.sub(c8)], writes=[z.sub(c8)])
            self.proj_res(src, dst, t0, 512, l, 0, wo, z, 8, 2, hr, [ps[1], ps[2]], cnt, False)
        P.barrier()
        P.release(m)

    def copy_dram(self, src, dst, T_):
        P = self.P
        P.dma("sp", dst[:, 0:T_], src[:, 0:T_])
        P.barrier()


def build(test=None):
    k = K(test)
    P = k.P
    k.setup()
    k.adaln()
    if test is None or test in ("full", "fulls"):
        TL = 1024 if test == "fulls" else S
        k.mixer_win(0, k.xT, k.hA, k.ctxT, k.cA, T_lat=TL)
        k.ffn(0, k.hA, k.hB, k.cA, k.cB, True, T_lat=TL)
        k.mixer_sc(1, k.hB, k.hA, k.cB, k.cA, T_lat=TL)
        k.ffn(1, k.hA, k.hB, k.cA, k.cB, True, T_lat=TL)
        k.mixer_dense(2, k.hB, k.hA, k.cB, k.cA, T_lat=TL)
        k.ffn(2, k.hA, k.hB, k.cA, k.cB, True, T_lat=TL)
        k.mixer_mlstm(3, k.hB, k.hA, k.cB, T_lat=TL)
        k.ffn(3, k.hA, k.outT, None, None, False, T_lat=TL)
    if test in ("ffn0", "small"):
        k.ffn(0, k.xT, k.outT, k.ctxT, k.cB, True, T_lat=(S if test == "ffn0" else 1024))
    if test in ("sc1", "sc1s"):
        k.mixer_sc(1, k.xT, k.outT, k.ctxT, k.cB, T_lat=(S if test == "sc1" else 1024))
    if test in ("ax2", "ax2s"):
        k.mixer_dense(2, k.xT, k.outT, k.ctxT, k.cB, T_lat=(S if test == "ax2" else 1024))
    if test in ("win0", "win0s"):
        k.mixer_win(0, k.xT, k.outT, k.ctxT, k.cB, T_lat=(S if test == "win0" else 1024))
    if test in ("ml3", "ml3s"):
        k.mixer_mlstm(3, k.xT, k.outT, k.ctxT, T_lat=(S if test == "ml3" else 1024))
    n = P.emit()
    return k.nc, n


def make_in_maps(inputs, cores):
    vecs = pack_vecs(inputs)
    c128, s128 = rope_tables(128)
    c64, s64 = rope_tables(64)
    ii = np.arange(128)[:, None]
    jj = np.arange(128)[None, :]
    wmask = np.concatenate([np.tile((ii >= jj).astype(np.float32), (1, 4)), np.tile((ii <= jj).astype(np.float32), (1, 4))], axis=1)
    trimat = np.concatenate([(ii <= jj).astype(np.float32), (ii >= jj).astype(np.float32)], axis=1)
    maps = []
    for b in cores:
        cc = np.zeros((128, 8, 2), np.float32)
        cc[:, :, 0] = inputs["c"][b].reshape(8, 128).T
        cc[:, :, 1] = inputs["c_ctx"].reshape(8, 128).T
        maps.append({
            "xT": np.ascontiguousarray(inputs["x"][b].T),
            "ctxT": np.ascontiguousarray(inputs["ctx"][b].T),
            "cc": cc, "vecs": vecs,
            "ada_w": inputs["ada_w"], "ffn_w_up": inputs["ffn_w_up"], "ffn_w_down": inputs["ffn_w_down"],
            "sc_w_in": inputs["sc_w_in"][0], "sc_w_out": inputs["sc_w_out"][0],
            "ax_w_qkv": inputs["ax_w_qkv"][0], "ax_w_o": inputs["ax_w_o"][0],
            "win_w_qkv": inputs["win_w_qkv"][0], "win_w_o": inputs["win_w_o"][0],
            "cos128": c128, "sin128": s128, "cos64": c64, "sin64": s64, "wmask": wmask,
            "ml_w_in": inputs["ml_w_in"][0], "ml_w_out": inputs["ml_w_out"][0], "trimat": trimat,
        })
    return maps


def kernel(**inputs):
    inputs = {k: np.asarray(v) for k, v in inputs.items()}
    nc, _ = build()
    cores = [0, 1, 2, 3]
    res = run_bass_kernel_spmd(nc, make_in_maps(inputs, cores), core_ids=cores)
    out = np.stack([np.ascontiguousarray(res.results[i]["outT"].T) for i in range(4)], axis=0)
    return out.astype(np.float32)
```

```python
import numpy as np
import concourse.bass as bass
import concourse.mybir as mybir
from concourse.bass_utils import run_bass_kernel_spmd

F32 = mybir.dt.float32
BF16 = mybir.dt.bfloat16
AF = mybir.ActivationFunctionType
ALU = mybir.AluOpType

D = 1024
S = 8192
CTX = 256
DEPTH = 4
DFF = 2816
NFC = 44
EPS = 1e-6
SEM_CAP = 8000
N_DMA_SEMS = 24
ENGS = ("pe", "act", "dve", "pool", "sp")


class Buf:
    __slots__ = ("name", "last_w", "readers", "parent", "subs")

    def __init__(self, name, parent=None):
        self.name = name
        self.last_w = None
        self.readers = []
        self.parent = parent
        self.subs = {}

    def sub(self, key):
        b = self.subs.get(key)
        if b is None:
            b = Buf(f"{self.name}[{key}]", parent=self)
            self.subs[key] = b
        return b


class T:
    def __init__(self, t, name):
        self.t = t
        self.buf = Buf(name)

    def __getitem__(self, idx):
        return self.t[idx]

    def sub(self, key):
        return self.buf.sub(key)


def _bufs(xs):
    out = []
    for x in xs:
        if x is None:
            continue
        out.append(x.buf if isinstance(x, T) else x)
    return out


class Op:
    __slots__ = ("eng", "fn", "is_dma", "deps", "tick", "signal", "dsem", "dval", "waits", "pre_dma_wait")

    def __init__(self, eng, fn, is_dma):
        self.eng = eng
        self.fn = fn
        self.is_dma = is_dma
        self.deps = {}
        self.signal = False
        self.tick = None
        self.dsem = None
        self.dval = None
        self.waits = []
        self.pre_dma_wait = None


class Prog:
    def __init__(self, nc, arena_words=52736):
        self.nc = nc
        self.ops = []
        self._n = 0
        self.arena = nc.alloc_sbuf_tensor("arena", [128, arena_words], F32)
        self.arena_bf = self.arena.bitcast(BF16)
        self.arena_bytes = arena_words * 4
        self.off = 0
        self._last_compute = {}
        self._dmas_since_bar = []
        self._bar_frontier = []
        self._bar_pending = set()

    def alloc(self, name, shape, dt):
        es = 4 if dt == F32 else 2
        n = 1
        for d in shape[1:]:
            n *= d
        off = (self.off + 63) // 64 * 64
        self.off = off + n * es
        assert self.off <= self.arena_bytes, f"SBUF arena overflow at {name}: {self.off}"
        base = self.arena if dt == F32 else self.arena_bf
        e0 = off // es
        ap = base[0:shape[0], e0:e0 + n]
        if len(shape) == 3:
            ap = ap.rearrange("p (a b) -> p a b", a=shape[1])
        elif len(shape) == 4:
            ap = ap.rearrange("p (a b c) -> p a b c", a=shape[1], b=shape[2])
        return T(ap, name)

    def mark(self):
        return self.off

    def release(self, m):
        self.off = m

    def barrier(self):
        self._bar_frontier = list(self._last_compute.values()) + list(self._dmas_since_bar)
        self._dmas_since_bar = []
        self._bar_pending = set(ENGS)

    def ps(self, name, shape, dt=F32):
        self._n += 1
        return T(self.nc.alloc_psum_tensor(f"{name}_{self._n}", list(shape), dt), name)

    def dram(self, name, shape, dt, kind="Internal"):
        return T(self.nc.dram_tensor(name, list(shape), dt, kind=kind), name)

    def op(self, eng, fn, reads=(), writes=(), is_dma=False):
        o = Op(eng, fn, is_dma)
        rb = _bufs(reads)
        wb = _bufs(writes)

        def hist(b):
            hs = [b]
            if b.parent is not None:
                hs.append(b.parent)
            else:
                hs.extend(b.subs.values())
            return hs

        for b in rb:
            for h in hist(b):
                if h.last_w is not None:
                    o.deps[h.last_w] = True
        for b in wb:
            for h in hist(b):
                if h.last_w is not None and h.last_w not in o.deps:
                    o.deps[h.last_w] = False
                for r in h.readers:
                    if r not in o.deps:
                        o.deps[r] = False
        if eng in self._bar_pending:
            self._bar_pending.discard(eng)
            for d in self._bar_frontier:
                o.deps[d] = True
        if is_dma:
            self._dmas_since_bar.append(o)
        else:
            self._last_compute[eng] = o
        for b in rb:
            b.readers.append(o)
        for b in wb:
            b.last_w = o
            b.readers = []
            if b.parent is None:
                for s in b.subs.values():
                    s.last_w = o
                    s.readers = []
        self.ops.append(o)
        return o

    def dma(self, eng, out_ap, in_ap, reads=(), writes=(), **kw):
        return self.op(eng, lambda e: e.dma_start(out=out_ap, in_=in_ap, **kw), reads, writes, is_dma=True)

    def emit(self):
        nc = self.nc
        streams = {e: [] for e in ENGS}
        for o in self.ops:
            streams[o.eng].append(o)

        def skip(o, d, raw):
            return (not d.is_dma) and d.eng == o.eng and (not o.is_dma) and o.eng == "pe"

        for o in self.ops:
            for d, raw in o.deps.items():
                if d.is_dma or skip(o, d, raw):
                    continue
                d.signal = True
        ctr_sems = {}
        for e in ENGS:
            k = 0
            for o in streams[e]:
                if o.signal and not o.is_dma:
                    k += 1
                    o.tick = k
            ctr_sems[e] = [nc.alloc_semaphore(f"ctr_{e}_{i}") for i in range((k + SEM_CAP - 1) // SEM_CAP)]
        finals = {}
        for e in ENGS:
            dmas = [o for o in streams[e] if o.is_dma]
            if not dmas:
                continue
            pool = [nc.alloc_semaphore(f"dma_{e}_{i}") for i in range(min(N_DMA_SEMS, len(dmas)))]
            vals = [0] * len(pool)
            for i, o in enumerate(dmas):
                j = i % len(pool)
                if vals[j] > 0:
                    o.pre_dma_wait = (pool[j], vals[j])
                vals[j] += 16
                o.dsem, o.dval = pool[j], vals[j]
            finals[e] = [(pool[j], vals[j]) for j in range(len(pool))]
        for e in ENGS:
            waited = {}
            for o in streams[e]:
                ws = {}
                cands = []
                for d, raw in o.deps.items():
                    if d.is_dma:
                        cands.append((d.dsem, d.dval, ("dma", id(d.dsem))))
                    elif not skip(o, d, raw):
                        t = d.tick - 1
                        cands.append((ctr_sems[d.eng][t // SEM_CAP], (t % SEM_CAP) + 1, (d.eng, t // SEM_CAP)))
                if o.pre_dma_wait is not None:
                    cands.append((o.pre_dma_wait[0], o.pre_dma_wait[1], ("dma", id(o.pre_dma_wait[0]))))
                for sem, val, key in cands:
                    if waited.get(key, 0) >= val:
                        continue
                    if key not in ws or ws[key][1] < val:
                        ws[key] = (sem, val)
                for key, (sem, val) in ws.items():
                    waited[key] = val
                o.waits = list(ws.values())
        engmap = {"pe": "tensor", "act": "scalar", "dve": "vector", "pool": "gpsimd", "sp": "sync"}
        with nc.Block() as block:
            for e in ENGS:
                ops = streams[e]
                if not ops:
                    continue

                def body(eng, ops=ops, e=e):
                    for o in ops:
                        for sem, val in o.waits:
                            eng.wait_ge(sem, val)
                        ins = o.fn(eng)
                        if o.is_dma:
                            ins.then_inc(o.dsem, 16)
                        elif o.signal:
                            ins.then_inc(ctr_sems[e][(o.tick - 1) // SEM_CAP], 1)
                    for sem, val in finals.get(e, []):
                        eng.wait_ge(sem, val)

                getattr(block, engmap[e])(body)
        return len(self.ops)


def _vec_layout():
    cols = {}
    n = 0

    def add(name, k):
        nonlocal n
        cols[name] = n
        n += k

    for l in range(DEPTH):
        add(f"ada_b{l}", 48)
        add(f"nmix{l}", 8)
        add(f"nffn{l}", 8)
        add(f"fconv{l}", 3 * NFC)
    add("sc_conv", 24)
    for nm in ("ax_qg", "ax_qgs", "ax_kg", "ax_kgs", "win_qg", "win_qgs", "win_kg", "win_kgs"):
        add(nm, 1)
    add("win_sink", 16)
    add("ml_bg", 16)
    add("ml_hn", 2)
    return cols, n


VCOLS, NV = _vec_layout()


def _colmajor(v):
    v = np.asarray(v, np.float32).reshape(-1, 128)
    return v.T


def pack_vecs(inp):
    out = np.zeros((128, NV), np.float32)

    def put(name, arr):
        a = _colmajor(arr)
        out[:, VCOLS[name]:VCOLS[name] + a.shape[1]] = a

    for l in range(DEPTH):
        put(f"ada_b{l}", inp["ada_b"][l])
        put(f"nmix{l}", inp["norm_mix"][l])
        put(f"nffn{l}", inp["norm_ffn"][l])
        put(f"fconv{l}", inp["ffn_conv"][l].reshape(-1))
    put("sc_conv", inp["sc_conv"][0].reshape(-1))

    def swap(v):
        h = v.shape[0] // 2
        return np.concatenate([v[h:], v[:h]])

    put("ax_qg", inp["ax_q_norm"][0])
    put("ax_qgs", swap(inp["ax_q_norm"][0]))
    put("ax_kg", inp["ax_k_norm"][0])
    put("ax_kgs", swap(inp["ax_k_norm"][0]))
    put("win_qg", np.tile(inp["win_q_norm"][0], 2))
    put("win_qgs", np.tile(swap(inp["win_q_norm"][0]), 2))
    put("win_kg", np.tile(inp["win_k_norm"][0], 2))
    put("win_kgs", np.tile(swap(inp["win_k_norm"][0]), 2))
    out[:, VCOLS["win_sink"]:VCOLS["win_sink"] + 16] = np.broadcast_to(inp["win_sink"][0][None, :], (128, 16))
    out[:, VCOLS["ml_bg"]:VCOLS["ml_bg"] + 16] = np.broadcast_to(inp["ml_b_gate"][0][None, :], (128, 16))
    put("ml_hn", inp["ml_h_norm"][0])
    return out


def rope_tables(hd):
    n_freq = hd // 4
    rows = S // 64
    row = np.repeat(np.arange(rows, dtype=np.float32), 64)
    col = np.tile(np.arange(64, dtype=np.float32), rows)
    inv = np.power(np.float32(10000.0), -np.arange(n_freq, dtype=np.float32) / np.float32(n_freq)).astype(np.float32)
    ang = np.concatenate([row[:, None] * inv, col[:, None] * inv], axis=-1).astype(np.float32)
    cos = np.cos(ang).astype(np.float32).T
    sin = np.sin(ang).astype(np.float32).T
    cos2 = np.concatenate([cos, cos], axis=0)
    sin2 = np.concatenate([-sin, sin], axis=0)
    rep = 128 // hd
    cos2 = np.tile(cos2, (rep, 1))
    sin2 = np.tile(sin2, (rep, 1))
    c = np.ones((128, S + CTX), np.float32)
    sn = np.zeros((128, S + CTX), np.float32)
    c[:, :S] = cos2
    sn[:, :S] = sin2
    return c, sn


class K:
    def __init__(self, test=None):
        self.test = test
        nc = bass.Bass("TRN2", target_bir_lowering=False)
        self.nc = nc
        P = Prog(nc)
        self.P = P
        dr = lambda n, s, k="ExternalInput": P.dram(n, s, F32, kind=k)
        self.xT = dr("xT", [D, S])
        self.ctxT = dr("ctxT", [D, CTX])
        self.cc = dr("cc", [128, 8, 2])
        self.vecs = dr("vecs", [128, NV])
        self.ada_w = dr("ada_w", [DEPTH, D, 6 * D])
        self.w_up = dr("ffn_w_up", [DEPTH, D, 2 * DFF])
        self.w_down = dr("ffn_w_down", [DEPTH, DFF, D])
        self.sc_w_in = dr("sc_w_in", [D, 3 * D])
        self.sc_w_out = dr("sc_w_out", [D, D])
        self.ax_w_qkv = dr("ax_w_qkv", [D, 1536])
        self.ax_w_o = dr("ax_w_o", [D, D])
        self.win_w_qkv = dr("win_w_qkv", [D, 1536])
        self.win_w_o = dr("win_w_o", [D, D])
        self.cos128 = dr("cos128", [128, S + CTX])
        self.sin128 = dr("sin128", [128, S + CTX])
        self.cos64 = dr("cos64", [128, S + CTX])
        self.sin64 = dr("sin64", [128, S + CTX])
        self.wmask = dr("wmask", [128, 1024])
        self.ml_w_in = dr("ml_w_in", [D, 3088])
        self.ml_w_out = dr("ml_w_out", [D, D])
        self.trimat = dr("trimat", [128, 256])
        self.hf = dr("hf_scratch", [D, S], "Internal")
        self.outT = dr("outT", [D, S], "ExternalOutput")
        self.hA = dr("hA", [D, S], "Internal")
        self.hB = dr("hB", [D, S], "Internal")
        self.cA = dr("cA", [D, CTX], "Internal")
        self.cB = dr("cB", [D, CTX], "Internal")
        self.ones = P.alloc("ones", [128, 128], BF16)
        self.vt = P.alloc("vt", [128, NV], F32)
        self.mod = P.alloc("mod", [128, DEPTH, 48, 2], F32)
        self.gsm = P.alloc("gsm", [128, DEPTH, 8, 2], F32)
        self.gsf = P.alloc("gsf", [128, DEPTH, 8, 2], F32)
        self.epsb = P.alloc("epsb", [128, 1], F32)
        self.ones_bd = P.alloc("ones_bd", [128, 128], BF16)
        self.zero_c = P.alloc("zero_c", [128, 1], F32)
        self.ps = [P.ps(f"ps{i}", [128, 512]) for i in range(8)]
        self.base_mark = P.mark()

    def setup(self):
        P = self.P
        P.op("pool", lambda e: e.memset(self.ones[:], 1.0), writes=[self.ones])
        P.op("pool", lambda e: e.memset(self.ones_bd[:], 0.0), writes=[self.ones_bd])
        P.op("pool", lambda e: e.memset(self.ones_bd[0:64, 0:64], 1.0), writes=[self.ones_bd])
        P.op("pool", lambda e: e.memset(self.ones_bd[64:128, 64:128], 1.0), writes=[self.ones_bd])
        P.op("pool", lambda e: e.memset(self.epsb[:], EPS), writes=[self.epsb])
        P.op("pool", lambda e: e.memset(self.zero_c[:], 0.0), writes=[self.zero_c])
        P.dma("sp", self.vt[:], self.vecs[:], writes=[self.vt])

    def adaln(self):
        P = self.P
        m = P.mark()
        cct = P.alloc("cct", [128, 8, 2], F32)
        sig = P.alloc("sig", [128, 8, 2], F32)
        sc = P.alloc("sc", [128, 8, 2], F32)
        P.dma("sp", cct[:], self.cc[:], writes=[cct])
        P.op("act", lambda e: e.activation(out=sig[:], in_=cct[:], func=AF.Sigmoid), reads=[cct], writes=[sig])
        P.op("dve", lambda e: e.tensor_tensor(out=sc[:], in0=cct[:], in1=sig[:], op=ALU.mult), reads=[cct, sig], writes=[sc])
        NP = 8
        PW = 768
        wbuf = [P.alloc(f"adaw{i}", [128, 8, PW], F32) for i in range(2)]
        pst = self.ps[0]
        k = 0
        for l in range(DEPTH):
            for pc in range(NP):
                wb = wbuf[k % 2]
                k += 1
                P.dma("sp", wb[:], self.ada_w[l, :, pc * PW:(pc + 1) * PW].rearrange("(c p) f -> p c f", p=128), writes=[wb])
                for jj in range(PW // 128):
                    j = pc * (PW // 128) + jj
                    for c in range(8):
                        P.op("pe", lambda e, wb=wb, jj=jj, c=c, j=j: e.matmul(pst[:, 2 * j:2 * j + 2], wb[:, c, jj * 128:(jj + 1) * 128], sc[:, c, :], start=(c == 0), stop=(c == 7)),
                             reads=[wb, sc], writes=[pst])
            cb = VCOLS[f"ada_b{l}"]
            for s in range(2):
                P.op("dve", lambda e, l=l, s=s, cb=cb: e.tensor_tensor(out=self.mod[:, l, :, s], in0=pst[:, 0:96].rearrange("p (j s) -> p j s", s=2)[:, :, s], in1=self.vt[:, cb:cb + 48], op=ALU.add),
                     reads=[pst, self.vt], writes=[self.mod])
            for s in range(2):
                for (dst, vi, nm) in ((self.gsm, 1, f"nmix{l}"), (self.gsf, 4, f"nffn{l}")):
                    cn = VCOLS[nm]
                    P.op("dve", lambda e, l=l, s=s, dst=dst, vi=vi, cn=cn: e.scalar_tensor_tensor(out=dst[:, l, :, s], in0=self.mod[:, l, vi * 8:vi * 8 + 8, s], scalar=1.0, in1=self.vt[:, cn:cn + 8], op0=ALU.add, op1=ALU.mult),
                         reads=[self.mod, self.vt], writes=[dst])
        P.barrier()
        P.release(m)

    def modcol(self, l, vi, c, s):
        return self.mod[:, l, vi * 8 + c, s:s + 1]

    def norm_mod(self, src, t0, W, gs, l, shift_vi, s, a_out, ht, sq, rs, pst):
        P = self.P
        P.dma("sp", ht[:, :, 0:W], src[:, t0:t0 + W].rearrange("(c p) t -> p c t", p=128), writes=[ht])
        for c in range(8):
            q = sq[c % len(sq)]
            P.op("act", lambda e, c=c, q=q: e.activation(out=q[:, 0:W], in_=ht[:, c, 0:W], func=AF.Square), reads=[ht], writes=[q])
            P.op("pe", lambda e, c=c, q=q: e.matmul(pst[:, 0:W], self.ones[:], q[:, 0:W], start=(c == 0), stop=(c == 7)), reads=[self.ones, q], writes=[pst])
        P.op("act", lambda e: e.activation(out=rs[:, 0:W], in_=pst[:, 0:W], func=AF.Sqrt, bias=self.epsb[:], scale=1.0 / D), reads=[pst, self.epsb], writes=[rs])
        P.op("dve", lambda e: e.reciprocal(rs[:, 0:W], rs[:, 0:W]), reads=[rs], writes=[rs])
        for c in range(8):
            P.op("dve", lambda e, c=c: e.tensor_tensor(out=ht[:, c, 0:W], in0=ht[:, c, 0:W], in1=rs[:, 0:W], op=ALU.mult),
                 reads=[ht.sub(c), rs], writes=[ht.sub(c)])
            P.op("act", lambda e, c=c: e.activation(out=a_out[:, c, 0:W], in_=ht[:, c, 0:W], func=AF.Identity, bias=self.modcol(l, shift_vi, c, s), scale=gs[:, l, c, s:s + 1]),
                 reads=[ht.sub(c), self.mod, gs], writes=[a_out.sub(c)])

    def load_cast(self, dst, dst_kc, w_ap_rows, ncols, stage, k0=0):
        P = self.P
        PIECE = stage[0].t.shape[-1] if False else 2048
        c0 = 0
        k = k0
        while c0 < ncols:
            n = min(PIECE, ncols - c0)
            st = stage[k % len(stage)]
            P.dma("sp", st[:, 0:n], w_ap_rows[:, c0:c0 + n], writes=[st])
            eng = "pool" if k % 2 == 0 else "dve"
            P.op(eng, lambda e, st=st, n=n, c0=c0: e.tensor_copy(dst[:, dst_kc, c0:c0 + n], st[:, 0:n]), reads=[st], writes=[dst.sub(("w", dst_kc, c0))])
            c0 += n
            k += 1
        return k

    def ffn(self, l, src, dst, csrc, cdst, do_ctx, T_lat=S):
        P = self.P
        m = P.mark()
        wu = P.alloc("wu", [128, 8, 2 * DFF], BF16)
        wd = P.alloc("wd", [128, 22, D], BF16)
        m2 = P.mark()
        stage = [P.alloc(f"stg{i}", [128, 2048], F32) for i in range(2)]
        k = 0
        for kc in range(8):
            k = self.load_cast(wu, kc, self.w_up[l, kc * 128:(kc + 1) * 128, :], 2 * DFF, stage, k)
        for kc in range(22):
            k = self.load_cast(wd, kc, self.w_down[l, kc * 128:(kc + 1) * 128, :], D, stage, k)
        P.barrier()
        P.release(m2)
        ht = P.alloc("ht", [128, 8, 512], F32)
        sq = [P.alloc(f"sq{i}", [128, 512], BF16) for i in range(2)]
        rs = P.alloc("rs", [128, 512], F32)
        a = P.alloc("a", [128, 8, 512], BF16)
        NU = 3
        uc = [P.alloc(f"uc{i}", [128, 514], BF16) for i in range(NU)]
        yc = [P.alloc(f"yc{i}", [128, 512], BF16) for i in range(NU + 1)]
        sg = [P.alloc(f"sg{i}", [128, 512], BF16) for i in range(2)]
        carry = P.alloc("carry", [128, NFC, 2], BF16)
        gu = P.alloc("gu", [128, 22, 512], BF16)
        hr = [P.alloc(f"hr{i}", [128, 512], F32) for i in range(2)]
        cw = VCOLS[f"fconv{l}"]
        vt = self.vt
        ps_ss = self.ps[0]
        ps_up = self.ps[1:4]
        ps_dn = self.ps[4:8]
        cnt = {"u": 0, "y": 0, "d": 0, "g": 0, "h": 0}

        def up_conv(t0, W, flush):
            for i in range(22):
                ycs = []
                for half in range(2):
                    fc = half * 22 + i
                    u = uc[cnt["u"] % NU]
                    pu = ps_up[cnt["u"] % 3]
                    cnt["u"] += 1
                    y = yc[cnt["y"] % (NU + 1)]
                    cnt["y"] += 1
                    P.op("pool", lambda e, u=u, fc=fc: e.tensor_copy(u[:, 0:2], carry[:, fc, :]), reads=[carry.sub(fc)], writes=[u])
                    if flush:
                        P.op("pool", lambda e, u=u: e.memset(u[:, 2:3], 0.0), writes=[u])
                    else:
                        for c in range(8):
                            P.op("pe", lambda e, c=c, fc=fc, pu=pu, W=W: e.matmul(pu[:, 0:W], wu[:, c, fc * 128:(fc + 1) * 128], a[:, c, 0:W], start=(c == 0), stop=(c == 7)),
                                 reads=[wu, a.sub(c)], writes=[pu])
                        P.op("act", lambda e, u=u, pu=pu, W=W: e.activation(out=u[:, 2:2 + W], in_=pu[:, 0:W], func=AF.Copy), reads=[pu], writes=[u])
                        P.op("pool", lambda e, u=u, fc=fc, W=W: e.tensor_copy(carry[:, fc, :], u[:, W:W + 2]), reads=[u], writes=[carry.sub(fc)])
                    P.op("dve", lambda e, u=u, y=y, fc=fc, W=W: e.tensor_scalar(y[:, 0:W], u[:, 0:W], vt[:, cw + fc:cw + fc + 1], None, op0=ALU.mult), reads=[u, vt], writes=[y])
                    P.op("dve", lambda e, u=u, y=y, fc=fc, W=W: e.scalar_tensor_tensor(out=y[:, 0:W], in0=u[:, 1:1 + W], scalar=vt[:, cw + NFC + fc:cw + NFC + fc + 1], in1=y[:, 0:W], op0=ALU.mult, op1=ALU.add), reads=[u, vt, y], writes=[y])
                    P.op("dve", lambda e, u=u, y=y, fc=fc, W=W: e.scalar_tensor_tensor(out=y[:, 0:W], in0=u[:, 2:2 + W], scalar=vt[:, cw + 2 * NFC + fc:cw + 2 * NFC + fc + 1], in1=y[:, 0:W], op0=ALU.mult, op1=ALU.add), reads=[u, vt, y], writes=[y])
                    ycs.append(y)
                sgt = sg[cnt["g"] % 2]
                cnt["g"] += 1
                P.op("act", lambda e, sgt=sgt, y=ycs[0], W=W: e.activation(out=sgt[:, 0:W], in_=y[:, 0:W], func=AF.Silu), reads=[ycs[0]], writes=[sgt])
                P.op("dve", lambda e, sgt=sgt, y=ycs[1], i=i, W=W: e.tensor_tensor(out=gu[:, i, 0:W], in0=sgt[:, 0:W], in1=y[:, 0:W], op=ALU.mult), reads=[sgt, ycs[1]], writes=[gu.sub(i)])

        def down(hsrc, hdst, t0, W, s):
            o0 = 1 if t0 == 0 else 0
            for oc in range(8):
                pd = ps_dn[cnt["d"] % 4]
                cnt["d"] += 1
                hrt = hr[cnt["h"] % 2]
                cnt["h"] += 1
                kw = {"allow_slow_non_contiguous": True} if W - o0 == 1 else {}
                P.dma("sp", hrt[:, o0:W], hsrc[oc * 128:(oc + 1) * 128, t0 - 1 + o0:t0 - 1 + W], writes=[hrt], **kw)
                for i in range(22):
                    P.op("pe", lambda e, i=i, oc=oc, pd=pd, W=W: e.matmul(pd[:, 0:W], wd[:, i, oc * 128:(oc + 1) * 128], gu[:, i, 0:W], start=(i == 0), stop=(i == 21)),
                         reads=[wd, gu.sub(i)], writes=[pd])
                P.op("dve", lambda e, pd=pd, hrt=hrt, oc=oc, W=W, o0=o0: e.scalar_tensor_tensor(out=hrt[:, o0:W], in0=pd[:, o0:W], scalar=self.modcol(l, 5, oc, s), in1=hrt[:, o0:W], op0=ALU.mult, op1=ALU.add),
                     reads=[pd, hrt, self.mod], writes=[hrt])
                P.dma("sp", hdst[oc * 128:(oc + 1) * 128, t0 - 1 + o0:t0 - 1 + W], hrt[:, o0:W], reads=[hrt], **kw)

        def seq(hsrc, hdst, T_, s):
            P.op("pool", lambda e: e.memset(carry[:], 0.0), writes=[carry])
            tiles = [(t0, min(512, T_ - t0), False) for t0 in range(0, T_, 512)] + [(T_, 1, True)]
            self.norm_mod(hsrc, 0, tiles[0][1], self.gsf, l, 3, s, a, ht, sq, rs, ps_ss)
            for j, (t0, W, flush) in enumerate(tiles):
                up_conv(t0, W, flush)
                if j + 1 < len(tiles) and not tiles[j + 1][2]:
                    self.norm_mod(hsrc, tiles[j + 1][0], tiles[j + 1][1], self.gsf, l, 3, s, a, ht, sq, rs, ps_ss)
                down(hsrc, hdst, t0, W, s)

        seq(src, dst, T_lat, 0)
        if do_ctx:
            seq(csrc, cdst, CTX, 1)
        P.barrier()
        P.release(m)

    def proj_res(self, hsrc, hdst, t0, W, l, s, wmat, zin, nk, gate_vi, hr, ps_dn, cnt, shifted):
        P = self.P
        tb = t0 - 1 if shifted else t0
        o0 = 1 if (shifted and t0 == 0) else 0
        for oc in range(8):
            pd = ps_dn[cnt["d"] % len(ps_dn)]
            cnt["d"] += 1
            hrt = hr[cnt["h"] % len(hr)]
            cnt["h"] += 1
            kw = {"allow_slow_non_contiguous": True} if W - o0 == 1 else {}
            P.dma("sp", hrt[:, o0:W], hsrc[oc * 128:(oc + 1) * 128, tb + o0:tb + W], writes=[hrt], **kw)
            for i in range(nk):
                P.op("pe", lambda e, i=i, oc=oc, pd=pd: e.matmul(pd[:, 0:W], wmat[:, i, oc * 128:(oc + 1) * 128], zin[:, i, 0:W], start=(i == 0), stop=(i == nk - 1)),
                     reads=[wmat, zin.sub(i)], writes=[pd])
            P.op("dve", lambda e, pd=pd, hrt=hrt, oc=oc: e.scalar_tensor_tensor(out=hrt[:, o0:W], in0=pd[:, o0:W], scalar=self.modcol(l, gate_vi, oc, s), in1=hrt[:, o0:W], op0=ALU.mult, op1=ALU.add),
                 reads=[pd, hrt, self.mod], writes=[hrt])
            P.dma("sp", hdst[oc * 128:(oc + 1) * 128, tb + o0:tb + W], hrt[:, o0:W], reads=[hrt], **kw)

    def mixer_sc(self, l, src, dst, csrc, cdst, T_lat=S):
        P = self.P
        m = P.mark()
        wi = P.alloc("wi", [128, 8, 3 * D], BF16)
        wo = P.alloc("wo", [128, 8, D], BF16)
        m2 = P.mark()
        stage = [P.alloc(f"stg{i}", [128, 2048], F32) for i in range(2)]
        k = 0
        for kc in range(8):
            k = self.load_cast(wi, kc, self.sc_w_in[kc * 128:(kc + 1) * 128, :], 3 * D, stage, k)
        for kc in range(8):
            k = self.load_cast(wo, kc, self.sc_w_out[kc * 128:(kc + 1) * 128, :], D, stage, k)
        P.barrier()
        P.release(m2)
        ht = P.alloc("ht", [128, 8, 512], F32)
        sq = [P.alloc(f"sq{i}", [128, 512], BF16) for i in range(2)]
        rs = P.alloc("rs", [128, 512], F32)
        a = P.alloc("a", [128, 8, 512], BF16)
        cu = [P.alloc(f"cu{i}", [128, 514], BF16) for i in range(2)]
        us = [P.alloc(f"us{i}", [128, 512], F32) for i in range(2)]
        bb = [P.alloc(f"bb{i}", [128, 513], BF16) for i in range(2)]
        yc = [P.alloc(f"yc{i}", [128, 512], BF16) for i in range(2)]
        carry_cu = P.alloc("carry_cu", [128, 8, 2], BF16)
        carry_b = P.alloc("carry_b", [128, 8, 1], BF16)
        z = P.alloc("z", [128, 8, 512], BF16)
        hr = [P.alloc(f"hr{i}", [128, 512], F32) for i in range(2)]
        cw = VCOLS["sc_conv"]
        vt = self.vt
        ps_ss = self.ps[0]
        ps_in = self.ps[1:5]
        ps_dn = self.ps[5:8]
        cnt = {"u": 0, "p": 0, "d": 0, "h": 0}

        def inproj(pt, col0, W):
            for c in range(8):
                P.op("pe", lambda e, c=c: e.matmul(pt[:, 0:W], wi[:, c, col0:col0 + 128], a[:, c, 0:W], start=(c == 0), stop=(c == 7)), reads=[wi, a.sub(c)], writes=[pt])

        def body(t0, W, flush):
            for i in range(8):
                cut = cu[cnt["u"] % 2]
                ust = us[cnt["u"] % 2]
                bbt = bb[cnt["u"] % 2]
                y = yc[cnt["u"] % 2]
                cnt["u"] += 1
                P.op("pool", lambda e, cut=cut, i=i: e.tensor_copy(cut[:, 0:2], carry_cu[:, i, :]), reads=[carry_cu.sub(i)], writes=[cut])
                P.op("pool", lambda e, bbt=bbt, i=i: e.tensor_copy(bbt[:, 0:1], carry_b[:, i, :]), reads=[carry_b.sub(i)], writes=[bbt])
                if flush:
                    P.op("pool", lambda e, cut=cut: e.memset(cut[:, 2:3], 0.0), writes=[cut])
                else:
                    pb, pc, pu = (ps_in[(cnt["p"] + j) % 4] for j in range(3))
                    cnt["p"] += 3
                    inproj(pb, i * 128, W)
                    inproj(pc, D + i * 128, W)
                    inproj(pu, 2 * D + i * 128, W)
                    P.op("act", lambda e, ust=ust, pu=pu: e.activation(out=ust[:, 0:W], in_=pu[:, 0:W], func=AF.Copy), reads=[pu], writes=[ust])
                    P.op("act", lambda e, bbt=bbt, pb=pb: e.activation(out=bbt[:, 1:1 + W], in_=pb[:, 0:W], func=AF.Copy), reads=[pb], writes=[bbt])
                    P.op("dve", lambda e, cut=cut, pc=pc, ust=ust: e.tensor_tensor(out=cut[:, 2:2 + W], in0=pc[:, 0:W], in1=ust[:, 0:W], op=ALU.mult), reads=[pc, ust], writes=[cut])
                    P.op("pool", lambda e, cut=cut, i=i: e.tensor_copy(carry_cu[:, i, :], cut[:, W:W + 2]), reads=[cut], writes=[carry_cu.sub(i)])
                    P.op("pool", lambda e, bbt=bbt, i=i: e.tensor_copy(carry_b[:, i, :], bbt[:, W:W + 1]), reads=[bbt], writes=[carry_b.sub(i)])
                P.op("dve", lambda e, cut=cut, y=y, i=i: e.tensor_scalar(y[:, 0:W], cut[:, 0:W], vt[:, cw + i:cw + i + 1], None, op0=ALU.mult), reads=[cut, vt], writes=[y])
                P.op("dve", lambda e, cut=cut, y=y, i=i: e.scalar_tensor_tensor(out=y[:, 0:W], in0=cut[:, 1:1 + W], scalar=vt[:, cw + 8 + i:cw + 8 + i + 1], in1=y[:, 0:W], op0=ALU.mult, op1=ALU.add), reads=[cut, vt, y], writes=[y])
                P.op("dve", lambda e, cut=cut, y=y, i=i: e.scalar_tensor_tensor(out=y[:, 0:W], in0=cut[:, 2:2 + W], scalar=vt[:, cw + 16 + i:cw + 16 + i + 1], in1=y[:, 0:W], op0=ALU.mult, op1=ALU.add), reads=[cut, vt, y], writes=[y])
                P.op("dve", lambda e, bbt=bbt, y=y, i=i: e.tensor_tensor(out=z[:, i, 0:W], in0=y[:, 0:W], in1=bbt[:, 0:W], op=ALU.mult), reads=[y, bbt], writes=[z.sub(i)])

        def seq(hsrc, hdst, T_, s):
            P.op("pool", lambda e: e.memset(carry_cu[:], 0.0), writes=[carry_cu])
            P.op("pool", lambda e: e.memset(carry_b[:], 0.0), writes=[carry_b])
            tiles = [(t0, min(512, T_ - t0), False) for t0 in range(0, T_, 512)] + [(T_, 1, True)]
            self.norm_mod(hsrc, 0, tiles[0][1], self.gsm, l, 0, s, a, ht, sq, rs, ps_ss)
            for j, (t0, W, flush) in enumerate(tiles):
                body(t0, W, flush)
                if j + 1 < len(tiles) and not tiles[j + 1][2]:
                    self.norm_mod(hsrc, tiles[j + 1][0], tiles[j + 1][1], self.gsm, l, 0, s, a, ht, sq, rs, ps_ss)
                self.proj_res(hsrc, hdst, t0, W, l, s, wo, z, 8, 2, hr, ps_dn, cnt, True)

        seq(src, dst, T_lat, 0)
        seq(csrc, cdst, CTX, 1)
        P.barrier()
        P.release(m)

    def load_qkv_weights(self, w_qkv, w_o, hd, nq):
        P = self.P
        nk = 1536 - nq - (1536 - nq) // 2
        wq = P.alloc("wq", [128, 8, nq], BF16)
        wqs = P.alloc("wqs", [128, 8, nq], BF16)
        wk = P.alloc("wk", [128, 8, 256], BF16)
        wks = P.alloc("wks", [128, 8, 256], BF16)
        wv = P.alloc("wv", [128, 8, 256], BF16)
        wo = P.alloc("wo", [128, 8, D], BF16)
        m2 = P.mark()
        stage = [P.alloc(f"stg{i}", [128, 2048], F32) for i in range(2)]
        h2 = hd // 2
        for kc in range(8):
            st = stage[kc % 2]
            P.dma("sp", st[:, 0:1536], w_qkv[kc * 128:(kc + 1) * 128, :], writes=[st])
            P.op("dve", lambda e, st=st, kc=kc: e.tensor_copy(wq[:, kc, :], st[:, 0:nq]), reads=[st], writes=[wq.sub(kc)])
            P.op("pool", lambda e, st=st, kc=kc: e.tensor_copy(wk[:, kc, :], st[:, nq:nq + 256]), reads=[st], writes=[wk.sub(kc)])
            P.op("pool", lambda e, st=st, kc=kc: e.tensor_copy(wv[:, kc, :], st[:, nq + 256:nq + 512]), reads=[st], writes=[wv.sub(kc)])
            for (dstw, c0, n) in ((wqs, 0, nq), (wks, nq, 256)):
                for t in range(2):
                    P.op("dve" if t == 0 else "pool",
                         lambda e, st=st, kc=kc, dstw=dstw, c0=c0, n=n, t=t: e.tensor_copy(
                             dstw[:, kc, :].rearrange("p (h t d) -> p h t d", t=2, d=h2)[:, :, t, :],
                             st[:, c0:c0 + n].rearrange("p (h t d) -> p h t d", t=2, d=h2)[:, :, 1 - t, :]),
                         reads=[st], writes=[dstw.sub((kc, t))])
        k = 0
        for kc in range(8):
            k = self.load_cast(wo, kc, w_o[kc * 128:(kc + 1) * 128, :], D, stage, k)
        P.barrier()
        P.release(m2)
        return wq, wqs, wk, wks, wv, wo

    def qk_norm_rope(self, pq, pqs, gcol, gscol, hd, cost, sint, W, out_ap, out_buf, bufs, ps_ss2):
        P = self.P
        sqb, rsb, t1, t2 = bufs
        onesm = self.ones if hd == 128 else self.ones_bd
        vt = self.vt
        P.op("act", lambda e: e.activation(out=sqb[:, 0:W], in_=pq[:, 0:W], func=AF.Square), reads=[pq], writes=[sqb])
        P.op("pe", lambda e: e.matmul(ps_ss2[:, 0:W], onesm[:], sqb[:, 0:W], start=True, stop=True), reads=[onesm, sqb], writes=[ps_ss2])
        P.op("act", lambda e: e.activation(out=rsb[:, 0:W], in_=ps_ss2[:, 0:W], func=AF.Sqrt, bias=self.epsb[:], scale=1.0 / hd), reads=[ps_ss2, self.epsb], writes=[rsb])
        P.op("dve", lambda e: e.reciprocal(rsb[:, 0:W], rsb[:, 0:W]), reads=[rsb], writes=[rsb])
        P.op("dve", lambda e: e.scalar_tensor_tensor(out=t1[:, 0:W], in0=pq[:, 0:W], scalar=vt[:, gcol:gcol + 1], in1=rsb[:, 0:W], op0=ALU.mult, op1=ALU.mult), reads=[pq, vt, rsb], writes=[t1])
        P.op("dve", lambda e: e.scalar_tensor_tensor(out=t2[:, 0:W], in0=pqs[:, 0:W], scalar=vt[:, gscol:gscol + 1], in1=rsb[:, 0:W], op0=ALU.mult, op1=ALU.mult), reads=[pqs, vt, rsb], writes=[t2])
        P.op("dve", lambda e: e.tensor_tensor(out=t1[:, 0:W], in0=t1[:, 0:W], in1=cost[:, 0:W], op=ALU.mult), reads=[t1, cost], writes=[t1])
        P.op("dve", lambda e: e.tensor_tensor(out=t2[:, 0:W], in0=t2[:, 0:W], in1=sint[:, 0:W], op=ALU.mult), reads=[t2, sint], writes=[t2])
        P.op("dve", lambda e: e.tensor_tensor(out=out_ap, in0=t1[:, 0:W], in1=t2[:, 0:W], op=ALU.add), reads=[t1, t2], writes=[out_buf])

    def mixer_dense(self, l, src, dst, csrc, cdst, T_lat=S):
        P = self.P
        m = P.mark()
        HD = 128
        NKB = (T_lat + CTX) // 128
        wq, wqs, wk, wks, wv, wo = self.load_qkv_weights(self.ax_w_qkv, self.ax_w_o, HD, 1024)
        KT = P.alloc("KT", [128, 2, T_lat + CTX], BF16)
        V = P.alloc("V", [128, NKB, 256], BF16)
        ht = P.alloc("ht", [128, 8, 512], F32)
        sq = [P.alloc(f"sq{i}", [128, 512], BF16) for i in range(2)]
        rs = P.alloc("rs", [128, 512], F32)
        a = P.alloc("a", [128, 8, 512], BF16)
        cost = P.alloc("cost", [128, 512], F32)
        sint = P.alloc("sint", [128, 512], F32)
        nb = (P.alloc("nsq", [128, 512], BF16), P.alloc("nrs", [128, 512], F32), P.alloc("nt1", [128, 512], F32), P.alloc("nt2", [128, 512], F32))
        Q = P.alloc("Q", [128, 8, 512], BF16)
        E = [P.alloc(f"E{i}", [128, 512], BF16) for i in range(3)]
        rden = P.alloc("rden", [128, 512], F32)
        att = P.alloc("att", [128, 8, 512], BF16)
        hr = [P.alloc(f"hr{i}", [128, 512], F32) for i in range(2)]
        vt = self.vt
        ps = self.ps
        gq, gqs, gk, gks = VCOLS["ax_qg"], VCOLS["ax_qgs"], VCOLS["ax_kg"], VCOLS["ax_kgs"]
        cnt = {"d": 0, "h": 0, "e": 0, "s": 0, "o": 0, "p": 0, "v": 0}

        def proj(pt, w, col0, W):
            for c in range(8):
                P.op("pe", lambda e, c=c: e.matmul(pt[:, 0:W], w[:, c, col0:col0 + 128], a[:, c, 0:W], start=(c == 0), stop=(c == 7)), reads=[w, a.sub(c)], writes=[pt])

        def load_tables(pos0, W):
            P.dma("sp", cost[:, 0:W], self.cos128[:, pos0:pos0 + W], writes=[cost])
            P.dma("sp", sint[:, 0:W], self.sin128[:, pos0:pos0 + W], writes=[sint])

        def phase_a(hsrc, T_, s, pos_base):
            for t0 in range(0, T_, 512):
                W = min(512, T_ - t0)
                pos0 = pos_base + t0
                self.norm_mod(hsrc, t0, W, self.gsm, l, 0, s, a, ht, sq, rs, ps[0])
                load_tables((S if s == 1 else 0) + t0, W)
                for g in range(2):
                    pk, pks = ps[1 + 2 * (cnt["p"] % 2)], ps[2 + 2 * (cnt["p"] % 2)]
                    cnt["p"] += 1
                    proj(pk, wk, g * 128, W)
                    proj(pks, wks, g * 128, W)
                    self.qk_norm_rope(pk, pks, gk, gks, HD, cost, sint, W, KT[:, g, pos0:pos0 + W], KT.sub((g, pos0)), nb, ps[5])
                for blk in range(W // 128):
                    pv = ps[6 + cnt["v"] % 2]
                    cnt["v"] += 1
                    for c in range(8):
                        P.op("pe", lambda e, c=c, blk=blk, pv=pv: e.matmul(pv[:, 0:256], a[:, c, blk * 128:(blk + 1) * 128], wv[:, c, :], start=(c == 0), stop=(c == 7)), reads=[wv, a.sub(c)], writes=[pv])
                    kb = pos0 // 128 + blk
                    P.op("act", lambda e, pv=pv, kb=kb: e.activation(out=V[:, kb, :], in_=pv[:, 0:256], func=AF.Copy), reads=[pv], writes=[V.sub(kb)])

        phase_a(src, T_lat, 0, 0)
        phase_a(csrc, CTX, 1, T_lat)

        scale = float(HD) ** -0.5

        def phase_b(hsrc, hdst, T_, s, pos_base, kblocks):
            for t0 in range(0, T_, 512):
                W = min(512, T_ - t0)
                pos0 = pos_base + t0
                self.norm_mod(hsrc, t0, W, self.gsm, l, 0, s, a, ht, sq, rs, ps[0])
                load_tables((S if s == 1 else 0) + t0, W)
                for h in range(8):
                    pa_, pb_ = (ps[1], ps[2]) if h % 2 == 0 else (ps[3], ps[4])
                    proj(pa_, wq, h * 128, W)
                    proj(pb_, wqs, h * 128, W)
                    self.qk_norm_rope(pa_, pb_, gq, gqs, HD, cost, sint, W, Q[:, h, 0:W], Q.sub(h), nb, ps[0])
                items = [(h, j, kb) for h in range(8) for j, kb in enumerate(kblocks)]
                nkb = len(kblocks)
                banks = {}

                def issue_qk(n):
                    h, j, kb = items[n]
                    pS = ps[2 + n % 3]
                    P.op("pe", lambda e, kb=kb, pS=pS, h=h: e.matmul(pS[:, 0:W], KT[:, h // 4, kb * 128:(kb + 1) * 128], Q[:, h, 0:W], start=True, stop=True), reads=[KT, Q.sub(h)], writes=[pS])
                    return pS

                pS_next = issue_qk(0)
                for n, (h, j, kb) in enumerate(items):
                    g = h // 4
                    if j == 0:
                        banks[h] = (ps[5 + h % 2], ps[7] if h % 2 == 0 else ps[0])
                    po, pden = banks[h]
                    pS = pS_next
                    Et = E[n % 3]
                    P.op("act", lambda e, pS=pS, Et=Et: e.activation(out=Et[:, 0:W], in_=pS[:, 0:W], func=AF.Exp, scale=scale), reads=[pS], writes=[Et])
                    if n + 1 < len(items):
                        pS_next = issue_qk(n + 1)
                    st, sp_ = (j == 0), (j == nkb - 1)
                    P.op("pe", lambda e, kb=kb, Et=Et, po=po, g=g, st=st, sp_=sp_: e.matmul(po[:, 0:W], V[:, kb, g * 128:(g + 1) * 128], Et[:, 0:W], start=st, stop=sp_), reads=[V, Et], writes=[po])
                    P.op("pe", lambda e, Et=Et, pden=pden, st=st, sp_=sp_: e.matmul(pden[:, 0:W], self.ones[:], Et[:, 0:W], start=st, stop=sp_), reads=[self.ones, Et], writes=[pden])
                    if sp_:
                        P.op("dve", lambda e, pden=pden: e.reciprocal(rden[:, 0:W], pden[:, 0:W]), reads=[pden], writes=[rden])
                        P.op("dve", lambda e, po=po, h=h: e.tensor_tensor(out=att[:, h, 0:W], in0=po[:, 0:W], in1=rden[:, 0:W], op=ALU.mult), reads=[po, rden], writes=[att.sub(h)])
                self.proj_res(hsrc, hdst, t0, W, l, s, wo, att, 8, 2, hr, [ps[1], ps[2]], cnt, False)

        phase_b(src, dst, T_lat, 0, 0, list(range(NKB)))
        phase_b(csrc, cdst, CTX, 1, T_lat, list(range(T_lat // 128, NKB)))
        P.barrier()
        P.release(m)

    def mixer_win(self, l, src, dst, csrc, cdst, T_lat=S):
        P = self.P
        m = P.mark()
        HD = 64
        NKB = (T_lat + CTX) // 128
        NLB = T_lat // 128
        wq = P.alloc("wq", [128, 8, 1024], BF16)
        wqs = P.alloc("wqs", [128, 8, 1024], BF16)
        wk = P.alloc("wk", [128, 8, 256], BF16)
        wks = P.alloc("wks", [128, 8, 256], BF16)
        wv = P.alloc("wv", [128, 8, 256], BF16)
        wo = P.alloc("wo", [128, 8, D], BF16)
        mk = P.alloc("mk", [128, 2, 512], BF16)
        es = P.alloc("es", [128, 16], F32)
        m2 = P.mark()
        stage = [P.alloc(f"stg{i}", [128, 2048], F32) for i in range(2)]
        k = 0
        for kc in range(8):
            st = stage[kc % 2]
            P.dma("sp", st[:, 0:1536], self.win_w_qkv[kc * 128:(kc + 1) * 128, :], writes=[st])
            P.op("pool", lambda e, st=st, kc=kc: e.tensor_copy(wv[:, kc, :], st[:, 1280:1536]), reads=[st], writes=[wv.sub(kc)])
            for (dw, dws, c0, n) in ((wq, wqs, 0, 1024), (wk, wks, 1024, 256)):
                for t in range(2):
                    P.op("dve" if t == 0 else "pool",
                         lambda e, st=st, kc=kc, dw=dw, c0=c0, n=n, t=t: e.tensor_copy(
                             dw[:, kc, :].rearrange("p (i t d) -> p i t d", t=2, d=64)[:, :, t, :],
                             st[:, c0:c0 + n].rearrange("p (t i d) -> p t i d", t=2, d=64)[:, t, :, :]),
                         reads=[st], writes=[dw.sub((kc, t))])
                    for u in range(2):
                        P.op("dve" if u == 0 else "pool",
                             lambda e, st=st, kc=kc, dws=dws, c0=c0, n=n, t=t, u=u: e.tensor_copy(
                                 dws[:, kc, :].rearrange("p (i t u d) -> p i t u d", t=2, u=2, d=32)[:, :, t, u, :],
                                 st[:, c0:c0 + n].rearrange("p (t i u d) -> p t i u d", t=2, u=2, d=32)[:, t, :, 1 - u, :]),
                             reads=[st], writes=[dws.sub((kc, t, u))])
        for kc in range(8):
            k = self.load_cast(wo, kc, self.win_w_o[kc * 128:(kc + 1) * 128, :], D, stage, k)
        st = stage[0]
        P.dma("sp", st[:, 0:1024], self.wmask[:, :], writes=[st])
        P.op("dve", lambda e: e.tensor_copy(mk[:].rearrange("p a b -> p (a b)"), st[:, 0:1024]), reads=[st], writes=[mk])
        sc0 = VCOLS["win_sink"]
        P.op("act", lambda e: e.activation(out=es[:], in_=self.vt[:, sc0:sc0 + 16], func=AF.Exp), reads=[self.vt], writes=[es])
        P.barrier()
        P.release(m2)
        KT = P.alloc("KT", [128, 2, T_lat + CTX], BF16)
        V = P.alloc("V", [128, NKB, 256], BF16)
        ht = P.alloc("ht", [128, 8, 512], F32)
        sq = [P.alloc(f"sq{i}", [128, 512], BF16) for i in range(2)]
        rs = P.alloc("rs", [128, 512], F32)
        a = P.alloc("a", [128, 8, 512], BF16)
        cost = P.alloc("cost", [128, 512], F32)
        sint = P.alloc("sint", [128, 512], F32)
        nb = (P.alloc("nsq", [128, 512], BF16), P.alloc("nrs", [128, 512], F32), P.alloc("nt1", [128, 512], F32), P.alloc("nt2", [128, 512], F32))
        Q = P.alloc("Q", [128, 8, 512], BF16)
        E = [P.alloc(f"E{i}", [128, 512], BF16) for i in range(3)]
        dtmp = P.alloc("dtmp", [128, 512], F32)
        rden = P.alloc("rden", [128, 512], F32)
        att = P.alloc("att", [128, 8, 512], BF16)
        hr = [P.alloc(f"hr{i}", [128, 512], F32) for i in range(2)]
        ps = self.ps
        gq, gqs, gk, gks = VCOLS["win_qg"], VCOLS["win_qgs"], VCOLS["win_kg"], VCOLS["win_kgs"]
        cnt = {"d": 0, "h": 0, "e": 0, "s": 0, "o": 0, "p": 0, "v": 0}

        def proj(pt, w, col0, W):
            for c in range(8):
                P.op("pe", lambda e, c=c: e.matmul(pt[:, 0:W], w[:, c, col0:col0 + 128], a[:, c, 0:W], start=(c == 0), stop=(c == 7)), reads=[w, a.sub(c)], writes=[pt])

        def load_tables(pos0, W):
            P.dma("sp", cost[:, 0:W], self.cos64[:, pos0:pos0 + W], writes=[cost])
            P.dma("sp", sint[:, 0:W], self.sin64[:, pos0:pos0 + W], writes=[sint])

        def phase_a(hsrc, T_, s, pos_base):
            for t0 in range(0, T_, 512):
                W = min(512, T_ - t0)
                pos0 = pos_base + t0
                self.norm_mod(hsrc, t0, W, self.gsm, l, 0, s, a, ht, sq, rs, ps[0])
                load_tables((S if s == 1 else 0) + t0, W)
                for j in range(2):
                    pk, pks = ps[1 + 2 * (cnt["p"] % 2)], ps[2 + 2 * (cnt["p"] % 2)]
                    cnt["p"] += 1
                    proj(pk, wk, j * 128, W)
                    proj(pks, wks, j * 128, W)
                    self.qk_norm_rope(pk, pks, gk, gks, HD, cost, sint, W, KT[:, j, pos0:pos0 + W], KT.sub((j, pos0)), nb, ps[5])
                for blk in range(W // 128):
                    pv = ps[6 + cnt["v"] % 2]
                    cnt["v"] += 1
                    for c in range(8):
                        P.op("pe", lambda e, c=c, blk=blk, pv=pv: e.matmul(pv[:, 0:256], a[:, c, blk * 128:(blk + 1) * 128], wv[:, c, :], start=(c == 0), stop=(c == 7)), reads=[wv, a.sub(c)], writes=[pv])
                    kb = pos0 // 128 + blk
                    P.op("act", lambda e, pv=pv, kb=kb: e.activation(out=V[:, kb, :], in_=pv[:, 0:256], func=AF.Copy), reads=[pv], writes=[V.sub(kb)])

        phase_a(src, T_lat, 0, 0)
        phase_a(csrc, CTX, 1, T_lat)
        scale = float(HD) ** -0.5

        def phase_b(hsrc, hdst, T_, s, pos_base, windowed):
            for t0 in range(0, T_, 512):
                W = min(512, T_ - t0)
                self.norm_mod(hsrc, t0, W, self.gsm, l, 0, s, a, ht, sq, rs, ps[0])
                load_tables((S if s == 1 else 0) + t0, W)
                for i in range(8):
                    pa_, pb_ = (ps[1], ps[2]) if i % 2 == 0 else (ps[3], ps[4])
                    proj(pa_, wq, i * 128, W)
                    proj(pb_, wqs, i * 128, W)
                    self.qk_norm_rope(pa_, pb_, gq, gqs, HD, cost, sint, W, Q[:, i, 0:W], Q.sub(i), nb, ps[0])
                items = []
                for qbl in range(W // 128):
                    if windowed:
                        qb = t0 // 128 + qbl
                        kbl = []
                        if qb > 0:
                            kbl.append((qb - 1, 0))
                        kbl.append((qb, None))
                        if qb < NLB - 1:
                            kbl.append((qb + 1, 1))
                        kbl += [(NLB, None), (NLB + 1, None)]
                    else:
                        kbl = [(NLB, None), (NLB + 1, None)]
                    for g in range(4):
                        for n_, (kb, mi) in enumerate(kbl):
                            items.append((qbl, g, kb, mi, n_ == 0, n_ == len(kbl) - 1))

                def issue_qk(n):
                    qbl, g, kb, mi, _, _ = items[n]
                    qs = slice(qbl * 128, (qbl + 1) * 128)
                    half, j, i0 = g // 2, g % 2, 4 * (g % 2)
                    hp = slice(half * 64, (half + 1) * 64)
                    pS = ps[2 + n % 3]
                    P.op("pe", lambda e, kb=kb, pS=pS, hp=hp, j=j, i0=i0, qs=qs: e.matmul(pS[:, 0:512], KT[hp, j, kb * 128:(kb + 1) * 128], Q[hp, i0:i0 + 4, qs], start=True, stop=True),
                         reads=[KT, Q], writes=[pS])
                    return pS

                pS_next = issue_qk(0)
                grp = 0
                for n, (qbl, g, kb, mi, st_, sp_) in enumerate(items):
                    qs = slice(qbl * 128, (qbl + 1) * 128)
                    if st_:
                        po = ps[5 + grp % 2]
                        pden = ps[7] if grp % 2 == 0 else ps[0]
                        grp += 1
                    pS = pS_next
                    Et = E[n % 3]
                    P.op("act", lambda e, pS=pS, Et=Et: e.activation(out=Et[:, :], in_=pS[:, 0:512], func=AF.Exp, scale=scale), reads=[pS], writes=[Et])
                    if n + 1 < len(items):
                        pS_next = issue_qk(n + 1)
                    if mi is not None:
                        P.op("dve", lambda e, Et=Et, mi=mi: e.tensor_tensor(out=Et[:, :], in0=Et[:, :], in1=mk[:, mi, :], op=ALU.mult), reads=[Et, mk], writes=[Et])
                    Ev = Et[:, :].rearrange("p (r q) -> p r q", q=128)
                    for par in range(2):
                        P.op("pe", lambda e, kb=kb, Ev=Ev, po=po, g=g, par=par, st_=st_, sp_=sp_: e.matmul(po[par * 64:(par + 1) * 64, 0:256], V[:, kb, g * 64:(g + 1) * 64], Ev[:, par::2, :], start=st_, stop=sp_, tile_position=(0, par * 64)),
                             reads=[V, Et], writes=[po])
                    P.op("pe", lambda e, Et=Et, pden=pden, st_=st_, sp_=sp_: e.matmul(pden[:, 0:512], self.ones[:], Et[:, :], start=st_, stop=sp_), reads=[self.ones, Et], writes=[pden])
                    if sp_:
                        for r in range(4):
                            hh = 4 * g + r
                            P.op("dve", lambda e, pden=pden, r=r, hh=hh: e.tensor_scalar(dtmp[:, r * 128:(r + 1) * 128], pden[:, r * 128:(r + 1) * 128], es[:, hh:hh + 1], None, op0=ALU.add), reads=[pden, es], writes=[dtmp])
                        P.op("dve", lambda e: e.reciprocal(rden[:, :], dtmp[:, :]), reads=[dtmp], writes=[rden])
                        for r in range(4):
                            rp = slice((r % 2) * 64, (r % 2 + 1) * 64)
                            cb = (r // 2) * 128
                            P.op("dve", lambda e, po=po, r=r, rp=rp, cb=cb, g=g, qs=qs: e.tensor_tensor(out=att[rp, 2 * g + r // 2, qs], in0=po[rp, cb:cb + 128], in1=rden[rp, r * 128:(r + 1) * 128], op=ALU.mult),
                                 reads=[po, rden], writes=[att.sub(2 * g + r // 2)])
                self.proj_res(hsrc, hdst, t0, W, l, s, wo, att, 8, 2, hr, [ps[1], ps[2]], cnt, False)

        phase_b(src, dst, T_lat, 0, 0, True)
        phase_b(csrc, cdst, CTX, 1, T_lat, False)
        P.barrier()
        P.release(m)

    def mixer_mlstm(self, l, src, dst, csrc, T_lat=S):
        P = self.P
        m = P.mark()
        wi = P.alloc("wi", [128, 8, 3088], BF16)
        wo = P.alloc("wo", [128, 8, D], BF16)
        tri = P.alloc("tri", [128, 2, 128], F32)
        trb = P.alloc("trb", [128, 2, 128], BF16)
        onesf = P.alloc("onesf", [128, 128], F32)
        m2 = P.mark()
        stage = [P.alloc(f"stg{i}", [128, 2048], F32) for i in range(2)]
        k = 0
        for kc in range(8):
            k = self.load_cast(wi, kc, self.ml_w_in[kc * 128:(kc + 1) * 128, :], 3088, stage, k)
        for kc in range(8):
            k = self.load_cast(wo, kc, self.ml_w_out[kc * 128:(kc + 1) * 128, :], D, stage, k)
        P.dma("sp", tri[:].rearrange("p a b -> p (a b)"), self.trimat[:, :], writes=[tri])
        P.op("dve", lambda e: e.tensor_copy(trb[:], tri[:]), reads=[tri], writes=[trb])
        P.op("pool", lambda e: e.memset(onesf[:], 1.0), writes=[onesf])
        P.barrier()
        P.release(m2)
        ht = P.alloc("ht", [128, 8, 512], F32)
        sq = [P.alloc(f"sq{i}", [128, 512], BF16) for i in range(2)]
        rs = P.alloc("rs", [128, 512], F32)
        a = P.alloc("a", [128, 8, 512], BF16)
        QT = P.alloc("QT", [128, 4, 512], BF16)
        KT = P.alloc("KT", [128, 4, 512], BF16)
        Ktm = P.alloc("Ktm", [128, 4, 512], F32)
        Vtm = P.alloc("Vtm", [128, 4, 1024], BF16)
        gtm = P.alloc("gtm", [128, 4, 16], F32)
        sgo = P.alloc("sgo", [128, 8, 512], BF16)
        hb = P.alloc("hb", [128, 8, 512], F32)
        hfT = P.alloc("hfT", [128, 8, 512], F32)
        z = P.alloc("z", [128, 8, 512], BF16)
        hr = [P.alloc(f"hr{i}", [128, 512], F32) for i in range(2)]
        e1 = P.alloc("e1", [128, 4], F32)
        lf = P.alloc("lf", [128, 4], F32)
        bcol = P.alloc("bcol", [128, 4], F32)
        wl = P.alloc("wl", [128, 4], F32)
        wcol = P.alloc("wcol", [128, 4], F32)
        eG = P.alloc("eG", [128, 4], F32)
        Rh = [P.alloc(f"Rh{i}", [128, 128], F32) for i in range(2)]
        Ah = [P.alloc(f"Ah{i}", [128, 128], BF16) for i in range(2)]
        ech = [P.alloc(f"ech{i}", [128, 128], F32) for i in range(2)]
        Ph = [P.alloc(f"Ph{i}", [128, 128], BF16) for i in range(2)]
        Qe = [P.alloc(f"Qe{i}", [128, 128], BF16) for i in range(2)]
        Kw = [P.alloc(f"Kw{i}", [128, 128], BF16) for i in range(2)]
        ad = [P.alloc(f"ad{i}", [128, 128], F32) for i in range(2)]
        Cst = P.alloc("Cst", [128, 4, 256], F32)
        Cb = P.alloc("Cb", [128, 4, 256], BF16)
        nst = P.alloc("nst", [128, 4, 128], F32)
        nbb = P.alloc("nbb", [128, 4, 128], BF16)
        vt = self.vt
        ps = self.ps
        cnt = {"d": 0, "h": 0, "p": 0, "r": 0}
        bg = VCOLS["ml_bg"]
        hn = VCOLS["ml_hn"]
        qscale = 128.0 ** -0.5

        def pp():
            t = ps[1 + cnt["p"] % 2]
            cnt["p"] += 1
            return t

        def project(W, with_o):
            nch = W // 128
            for h in range(4):
                for (dstT, c0, sc) in ((QT, 0, qscale), (KT, 512, 1.0)):
                    pt = pp()
                    for c in range(8):
                        P.op("pe", lambda e, c=c, pt=pt, c0=c0, h=h: e.matmul(pt[:, 0:W], wi[:, c, c0 + h * 128:c0 + (h + 1) * 128], a[:, c, 0:W], start=(c == 0), stop=(c == 7)), reads=[wi, a.sub(c)], writes=[pt])
                    P.op("act", lambda e, pt=pt, dstT=dstT, h=h, sc=sc: e.activation(out=dstT[:, h, 0:W], in_=pt[:, 0:W], func=AF.Copy, scale=sc), reads=[pt], writes=[dstT.sub(h)])
            for ch in range(nch):
                cs = slice(ch * 128, (ch + 1) * 128)
                pt = pp()
                for c in range(8):
                    P.op("pe", lambda e, c=c, pt=pt, cs=cs: e.matmul(pt[:, 0:512], a[:, c, cs], wi[:, c, 512:1024], start=(c == 0), stop=(c == 7)), reads=[wi, a.sub(c)], writes=[pt])
                P.op("act", lambda e, pt=pt, ch=ch: e.activation(out=Ktm[:, ch, :], in_=pt[:, 0:512], func=AF.Copy), reads=[pt], writes=[Ktm.sub(ch)])
                for half in range(2):
                    pt = pp()
                    for c in range(8):
                        P.op("pe", lambda e, c=c, pt=pt, cs=cs, half=half: e.matmul(pt[:, 0:512], a[:, c, cs], wi[:, c, 1024 + half * 512:1536 + half * 512], start=(c == 0), stop=(c == 7)), reads=[wi, a.sub(c)], writes=[pt])
                    P.op("act", lambda e, pt=pt, ch=ch, half=half: e.activation(out=Vtm[:, ch, half * 512:(half + 1) * 512], in_=pt[:, 0:512], func=AF.Copy), reads=[pt], writes=[Vtm.sub(ch)])
                pt = pp()
                for c in range(8):
                    P.op("pe", lambda e, c=c, pt=pt, cs=cs: e.matmul(pt[:, 0:16], a[:, c, cs], wi[:, c, 3072:3088], start=(c == 0), stop=(c == 7)), reads=[wi, a.sub(c)], writes=[pt])
                P.op("dve", lambda e, pt=pt, ch=ch: e.tensor_tensor(out=gtm[:, ch, :], in0=pt[:, 0:16], in1=vt[:, bg:bg + 16], op=ALU.add), reads=[pt, vt], writes=[gtm.sub(ch)])
            if with_o:
                for c8 in range(8):
                    pt = pp()
                    for c in range(8):
                        P.op("pe", lambda e, c=c, pt=pt, c8=c8: e.matmul(pt[:, 0:W], wi[:, c, 2048 + c8 * 128:2048 + (c8 + 1) * 128], a[:, c, 0:W], start=(c == 0), stop=(c == 7)), reads=[wi, a.sub(c)], writes=[pt])
                    P.op("act", lambda e, pt=pt, c8=c8: e.activation(out=sgo[:, c8, 0:W], in_=pt[:, 0:W], func=AF.Sigmoid), reads=[pt], writes=[sgo.sub(c8)])

        def core(ch, dirn, with_out, out_tile):
            cs = slice(ch * 128, (ch + 1) * 128)
            g0 = dirn * 8
            psA, pcr, pS, pnum, pden = ps[0], ps[3], ps[4], (ps[5], ps[6]), ps[7]
            P.op("act", lambda e: e.activation(out=e1[:], in_=gtm[:, ch, g0 + 4:g0 + 8], func=AF.Exp, scale=-1.0), reads=[gtm.sub(ch)], writes=[e1])
            P.op("act", lambda e: e.activation(out=e1[:], in_=e1[:], func=AF.Ln, bias=1.0, scale=1.0), reads=[e1], writes=[e1])
            P.op("dve", lambda e: e.tensor_scalar(lf[:], e1[:], -1.0, None, op0=ALU.mult), reads=[e1], writes=[lf])
            P.op("pe", lambda e: e.matmul(psA[:, 0:4], tri[:, dirn, :], lf[:], start=True, stop=True), reads=[tri, lf], writes=[psA])
            P.op("pe", lambda e: e.matmul(psA[:, 4:8], onesf[:], lf[:], start=True, stop=True), reads=[onesf, lf], writes=[psA])
            P.op("dve", lambda e: e.tensor_tensor(out=bcol[:], in0=gtm[:, ch, g0:g0 + 4], in1=psA[:, 0:4], op=ALU.subtract), reads=[gtm.sub(ch), psA], writes=[bcol])
            P.op("dve", lambda e: e.tensor_tensor(out=wl[:], in0=bcol[:], in1=psA[:, 4:8], op=ALU.add), reads=[bcol, psA], writes=[wl])
            P.op("act", lambda e: e.activation(out=wcol[:], in_=wl[:], func=AF.Exp), reads=[wl], writes=[wcol])
            P.op("act", lambda e: e.activation(out=eG[:], in_=psA[:, 4:8], func=AF.Exp), reads=[psA], writes=[eG])
            for h in range(4):
                i2 = cnt["r"] % 2
                cnt["r"] += 1
                R, A, ec, Pm, Qm, Kwm, adm = Rh[i2], Ah[i2], ech[i2], Ph[i2], Qe[i2], Kw[i2], ad[i2]
                hs_ = slice(h * 128, (h + 1) * 128)
                P.op("dve", lambda e, R=R, h=h: e.tensor_scalar(R[:], tri[:, dirn, :], lf[:, h:h + 1], None, op0=ALU.mult), reads=[tri, lf], writes=[R])
                P.op("pe", lambda e, R=R, hs_=hs_: e.matmul(pcr[:, hs_], onesf[:], R[:], start=True, stop=True), reads=[onesf, R], writes=[pcr])
                P.op("act", lambda e, A=A, hs_=hs_, h=h: e.activation(out=A[:], in_=pcr[:, hs_], func=AF.Exp, bias=bcol[:, h:h + 1], scale=1.0), reads=[pcr, bcol], writes=[A])
                P.op("act", lambda e, ec=ec, hs_=hs_: e.activation(out=ec[:], in_=pcr[:, hs_], func=AF.Exp), reads=[pcr], writes=[ec])
                P.op("pe", lambda e, h=h, hs_=hs_: e.matmul(pS[:, hs_], KT[:, h, cs], QT[:, h, cs], start=True, stop=True), reads=[KT.sub(h), QT.sub(h)], writes=[pS])
                P.op("dve", lambda e, A=A: e.tensor_tensor(out=A[:], in0=A[:], in1=trb[:, dirn, :], op=ALU.mult), reads=[A, trb], writes=[A])
                P.op("dve", lambda e, A=A, Pm=Pm, hs_=hs_: e.tensor_tensor(out=Pm[:], in0=pS[:, hs_], in1=A[:], op=ALU.mult), reads=[pS, A], writes=[Pm])
                if with_out:
                    P.op("dve", lambda e, Qm=Qm, ec=ec, h=h: e.tensor_tensor(out=Qm[:], in0=QT[:, h, cs], in1=ec[:], op=ALU.mult), reads=[QT.sub(h), ec], writes=[Qm])
                    pn = pnum[h // 2]
                    for vc in range(2):
                        ncol = slice((h % 2) * 256 + vc * 128, (h % 2) * 256 + (vc + 1) * 128)
                        P.op("pe", lambda e, pn=pn, ncol=ncol, h=h, vc=vc, Pm=Pm: e.matmul(pn[:, ncol], Vtm[:, ch, h * 256 + vc * 128:h * 256 + (vc + 1) * 128], Pm[:], start=True, stop=False), reads=[Vtm.sub(ch), Pm], writes=[pn])
                        P.op("pe", lambda e, pn=pn, ncol=ncol, h=h, vc=vc, Qm=Qm: e.matmul(pn[:, ncol], Cb[:, h, vc * 128:(vc + 1) * 128], Qm[:], start=False, stop=True), reads=[Cb.sub(h), Qm], writes=[pn])
                    P.op("pe", lambda e, hs_=hs_, Pm=Pm: e.matmul(pden[:, hs_], self.ones[:], Pm[:], start=True, stop=False), reads=[self.ones, Pm], writes=[pden])
                    P.op("pe", lambda e, hs_=hs_, Qm=Qm, h=h: e.matmul(pden[:, hs_], nbb[:, h, :], Qm[:], start=False, stop=True), reads=[nbb.sub(h), Qm], writes=[pden])
                    P.op("act", lambda e, adm=adm, hs_=hs_: e.activation(out=adm[:], in_=pden[:, hs_], func=AF.Abs), reads=[pden], writes=[adm])
                    P.op("dve", lambda e, adm=adm: e.tensor_scalar_max(adm[:], adm[:], 1.0), reads=[adm], writes=[adm])
                    P.op("dve", lambda e, adm=adm: e.reciprocal(adm[:], adm[:]), reads=[adm], writes=[adm])
                    for vc in range(2):
                        ncol = slice((h % 2) * 256 + vc * 128, (h % 2) * 256 + (vc + 1) * 128)
                        P.op("dve", lambda e, pn=pn, ncol=ncol, adm=adm, h=h, vc=vc: e.tensor_tensor(out=out_tile[:, 2 * h + vc, cs], in0=pn[:, ncol], in1=adm[:], op=ALU.mult), reads=[pn, adm], writes=[out_tile.sub((2 * h + vc, ch))])
                P.op("dve", lambda e, Kwm=Kwm, h=h: e.tensor_scalar(Kwm[:], Ktm[:, ch, h * 128:(h + 1) * 128], wcol[:, h:h + 1], None, op0=ALU.mult), reads=[Ktm.sub(ch), wcol], writes=[Kwm])
                pc = ps[1 + h // 2]
                ccol = slice((h % 2) * 256, (h % 2 + 1) * 256)
                P.op("pe", lambda e, pc=pc, ccol=ccol, Kwm=Kwm, h=h: e.matmul(pc[:, ccol], Kwm[:], Vtm[:, ch, h * 256:(h + 1) * 256], start=True, stop=True), reads=[Kwm, Vtm.sub(ch)], writes=[pc])
                P.op("pe", lambda e, Kwm=Kwm, hs_=hs_: e.matmul(psA[:, 128 + hs_.start // 4 * 0 + 0:128 + 0 + 128] if False else ps[0][:, 128:256], Kwm[:], self.ones[:], start=True, stop=True), reads=[Kwm, self.ones], writes=[psA])
                P.op("dve", lambda e, pc=pc, ccol=ccol, h=h: e.scalar_tensor_tensor(out=Cst[:, h, :], in0=Cst[:, h, :], scalar=eG[:, h:h + 1], in1=pc[:, ccol], op0=ALU.mult, op1=ALU.add), reads=[Cst.sub(h), eG, pc], writes=[Cst.sub(h)])
                P.op("dve", lambda e, h=h: e.scalar_tensor_tensor(out=nst[:, h, :], in0=nst[:, h, :], scalar=eG[:, h:h + 1], in1=psA[:, 128:256], op0=ALU.mult, op1=ALU.add), reads=[nst.sub(h), eG, psA], writes=[nst.sub(h)])
                P.op("act", lambda e, h=h: e.activation(out=Cb[:, h, :], in_=Cst[:, h, :], func=AF.Copy), reads=[Cst.sub(h)], writes=[Cb.sub(h)])
                P.op("pool", lambda e, h=h: e.tensor_copy(nbb[:, h, :], nst[:, h, :]), reads=[nst.sub(h)], writes=[nbb.sub(h)])

        def reset_state():
            P.op("pool", lambda e: e.memset(Cst[:], 0.0), writes=[Cst])
            P.op("pool", lambda e: e.memset(nst[:], 0.0), writes=[nst])
            P.op("pool", lambda e: e.memset(Cb[:], 0.0), writes=[Cb])
            P.op("pool", lambda e: e.memset(nbb[:], 0.0), writes=[nbb])

        reset_state()
        self.norm_mod(csrc, 0, CTX, self.gsm, l, 0, 1, a, ht, sq, rs, ps[0])
        project(CTX, False)
        for ch in range(2):
            core(ch, 0, False, None)
        for t0 in range(0, T_lat, 512):
            self.norm_mod(src, t0, 512, self.gsm, l, 0, 0, a, ht, sq, rs, ps[0])
            project(512, False)
            for ch in range(4):
                core(ch, 0, True, hb)
            P.dma("sp", self.hf[:, t0:t0 + 512].rearrange("(c p) t -> p c t", p=128), hb[:], reads=[hb], writes=[self.hf.sub(t0)])
        reset_state()
        self.norm_mod(csrc, 0, CTX, self.gsm, l, 0, 1, a, ht, sq, rs, ps[0])
        project(CTX, False)
        for ch in (1, 0):
            core(ch, 1, False, None)
        for t0 in range(T_lat - 512, -1, -512):
            self.norm_mod(src, t0, 512, self.gsm, l, 0, 0, a, ht, sq, rs, ps[0])
            project(512, True)
            P.dma("sp", hfT[:], self.hf[:, t0:t0 + 512].rearrange("(c p) t -> p c t", p=128), reads=[self.hf.sub(t0)], writes=[hfT])
            for ch in (3, 2, 1, 0):
                core(ch, 1, True, hb)
            for h in range(4):
                for vc in range(2):
                    c8 = 2 * h + vc
                    P.op("dve", lambda e, c8=c8: e.tensor_tensor(out=hb[:, c8, :], in0=hb[:, c8, :], in1=hfT[:, c8, :], op=ALU.add), reads=[hb, hfT], writes=[hb])
                    q = sq[vc]
                    P.op("act", lambda e, c8=c8, q=q: e.activation(out=q[:], in_=hb[:, c8, :], func=AF.Square), reads=[hb], writes=[q])
                    P.op("pe", lambda e, q=q, vc=vc: e.matmul(ps[0][:, :], self.ones[:], q[:], start=(vc == 0), stop=(vc == 1)), reads=[self.ones, q], writes=[ps[0]])
                P.op("act", lambda e: e.activation(out=rs[:], in_=ps[0][:, :], func=AF.Sqrt, bias=self.epsb[:], scale=1.0 / 256), reads=[ps[0], self.epsb], writes=[rs])
                P.op("dve", lambda e: e.reciprocal(rs[:], rs[:]), reads=[rs], writes=[rs])
                for vc in range(2):
                    c8 = 2 * h + vc
                    P.op("dve", lambda e, c8=c8, vc=vc: e.scalar_tensor_tensor(out=hb[:, c8, :], in0=hb[:, c8, :], scalar=vt[:, hn + vc:hn + vc + 1], in1=rs[:], op0=ALU.mult, op1=ALU.mult), reads=[hb, vt, rs], writes=[hb])
                    P.op("dve", lambda e, c8=c8: e.tensor_tensor(out=z[:, c8, :], in0=hb[:, c8, :], in1=sgo[:, c8, :], op=ALU.mult), reads=[hb, sgo.sub(c8)], writes=[z.sub(c8)])
            self.proj_res(src, dst, t0, 512, l, 0, wo, z, 8, 2, hr, [ps[1], ps[2]], cnt, False)
        P.barrier()
        P.release(m)

    def copy_dram(self, src, dst, T_):
        P = self.P
        P.dma("sp", dst[:, 0:T_], src[:, 0:T_])
        P.barrier()


def build(test=None):
    k = K(test)
    P = k.P
    k.setup()
    k.adaln()
    if test is None or test in ("full", "fulls"):
        TL = 1024 if test == "fulls" else S
        k.mixer_win(0, k.xT, k.hA, k.ctxT, k.cA, T_lat=TL)
        k.ffn(0, k.hA, k.hB, k.cA, k.cB, True, T_lat=TL)
        k.mixer_sc(1, k.hB, k.hA, k.cB, k.cA, T_lat=TL)
        k.ffn(1, k.hA, k.hB, k.cA, k.cB, True, T_lat=TL)
        k.mixer_dense(2, k.hB, k.hA, k.cB, k.cA, T_lat=TL)
        k.ffn(2, k.hA, k.hB, k.cA, k.cB, True, T_lat=TL)
        k.mixer_mlstm(3, k.hB, k.hA, k.cB, T_lat=TL)
        k.ffn(3, k.hA, k.outT, None, None, False, T_lat=TL)
    if test in ("ffn0", "small"):
        k.ffn(0, k.xT, k.outT, k.ctxT, k.cB, True, T_lat=(S if test == "ffn0" else 1024))
    if test in ("sc1", "sc1s"):
        k.mixer_sc(1, k.xT, k.outT, k.ctxT, k.cB, T_lat=(S if test == "sc1" else 1024))
    if test in ("ax2", "ax2s"):
        k.mixer_dense(2, k.xT, k.outT, k.ctxT, k.cB, T_lat=(S if test == "ax2" else 1024))
    if test in ("win0", "win0s"):
        k.mixer_win(0, k.xT, k.outT, k.ctxT, k.cB, T_lat=(S if test == "win0" else 1024))
    if test in ("ml3", "ml3s"):
        k.mixer_mlstm(3, k.xT, k.outT, k.ctxT, T_lat=(S if test == "ml3" else 1024))
    n = P.emit()
    return k.nc, n


def make_in_maps(inputs, cores):
    vecs = pack_vecs(inputs)
    c128, s128 = rope_tables(128)
    c64, s64 = rope_tables(64)
    ii = np.arange(128)[:, None]
    jj = np.arange(128)[None, :]
    wmask = np.concatenate([np.tile((ii >= jj).astype(np.float32), (1, 4)), np.tile((ii <= jj).astype(np.float32), (1, 4))], axis=1)
    trimat = np.concatenate([(ii <= jj).astype(np.float32), (ii >= jj).astype(np.float32)], axis=1)
    maps = []
    for b in cores:
        cc = np.zeros((128, 8, 2), np.float32)
        cc[:, :, 0] = inputs["c"][b].reshape(8, 128).T
        cc[:, :, 1] = inputs["c_ctx"].reshape(8, 128).T
        maps.append({
            "xT": np.ascontiguousarray(inputs["x"][b].T),
            "ctxT": np.ascontiguousarray(inputs["ctx"][b].T),
            "cc": cc, "vecs": vecs,
            "ada_w": inputs["ada_w"], "ffn_w_up": inputs["ffn_w_up"], "ffn_w_down": inputs["ffn_w_down"],
            "sc_w_in": inputs["sc_w_in"][0], "sc_w_out": inputs["sc_w_out"][0],
            "ax_w_qkv": inputs["ax_w_qkv"][0], "ax_w_o": inputs["ax_w_o"][0],
            "win_w_qkv": inputs["win_w_qkv"][0], "win_w_o": inputs["win_w_o"][0],
            "cos128": c128, "sin128": s128, "cos64": c64, "sin64": s64, "wmask": wmask,
            "ml_w_in": inputs["ml_w_in"][0], "ml_w_out": inputs["ml_w_out"][0], "trimat": trimat,
        })
    return maps


def kernel(**inputs):
    inputs = {k: np.asarray(v) for k, v in inputs.items()}
    nc, _ = build()
    cores = [0, 1, 2, 3]
    res = run_bass_kernel_spmd(nc, make_in_maps(inputs, cores), core_ids=cores)
    out = np.stack([np.ascontiguousarray(res.results[i]["outT"].T) for i in range(4)], axis=0)
    return out.astype(np.float32)
```

```python
import numpy as np
import concourse.bass as bass
import concourse.mybir as mybir
from concourse.bass_utils import run_bass_kernel_spmd

F32 = mybir.dt.float32
BF16 = mybir.dt.bfloat16
AF = mybir.ActivationFunctionType
ALU = mybir.AluOpType

D = 1024
S = 8192
CTX = 256
DEPTH = 4
DFF = 2816
NFC = 44
EPS = 1e-6
SEM_CAP = 8000
N_DMA_SEMS = 24
ENGS = ("pe", "act", "dve", "pool", "sp")


class Buf:
    __slots__ = ("name", "last_w", "readers", "parent", "subs")

    def __init__(self, name, parent=None):
        self.name = name
        self.last_w = None
        self.readers = []
        self.parent = parent
        self.subs = {}

    def sub(self, key):
        b = self.subs.get(key)
        if b is None:
            b = Buf(f"{self.name}[{key}]", parent=self)
            self.subs[key] = b
        return b


class T:
    def __init__(self, t, name):
        self.t = t
        self.buf = Buf(name)

    def __getitem__(self, idx):
        return self.t[idx]

    def sub(self, key):
        return self.buf.sub(key)


def _bufs(xs):
    out = []
    for x in xs:
        if x is None:
            continue
        out.append(x.buf if isinstance(x, T) else x)
    return out


class Op:
    __slots__ = ("eng", "fn", "is_dma", "deps", "tick", "signal", "dsem", "dval", "waits", "pre_dma_wait")

    def __init__(self, eng, fn, is_dma):
        self.eng = eng
        self.fn = fn
        self.is_dma = is_dma
        self.deps = {}
        self.signal = False
        self.tick = None
        self.dsem = None
        self.dval = None
        self.waits = []
        self.pre_dma_wait = None


class Prog:
    def __init__(self, nc, arena_words=52736):
        self.nc = nc
        self.ops = []
        self._n = 0
        self.arena = nc.alloc_sbuf_tensor("arena", [128, arena_words], F32)
        self.arena_bf = self.arena.bitcast(BF16)
        self.arena_bytes = arena_words * 4
        self.off = 0
        self._last_compute = {}
        self._dmas_since_bar = []
        self._bar_frontier = []
        self._bar_pending = set()

    def alloc(self, name, shape, dt):
        es = 4 if dt == F32 else 2
        n = 1
        for d in shape[1:]:
            n *= d
        off = (self.off + 63) // 64 * 64
        self.off = off + n * es
        assert self.off <= self.arena_bytes, f"SBUF arena overflow at {name}: {self.off}"
        base = self.arena if dt == F32 else self.arena_bf
        e0 = off // es
        ap = base[0:shape[0], e0:e0 + n]
        if len(shape) == 3:
            ap = ap.rearrange("p (a b) -> p a b", a=shape[1])
        elif len(shape) == 4:
            ap = ap.rearrange("p (a b c) -> p a b c", a=shape[1], b=shape[2])
        return T(ap, name)

    def mark(self):
        return self.off

    def release(self, m):
        self.off = m

    def barrier(self):
        self._bar_frontier = list(self._last_compute.values()) + list(self._dmas_since_bar)
        self._dmas_since_bar = []
        self._bar_pending = set(ENGS)

    def ps(self, name, shape, dt=F32):
        self._n += 1
        return T(self.nc.alloc_psum_tensor(f"{name}_{self._n}", list(shape), dt), name)

    def dram(self, name, shape, dt, kind="Internal"):
        return T(self.nc.dram_tensor(name, list(shape), dt, kind=kind), name)

    def op(self, eng, fn, reads=(), writes=(), is_dma=False):
        o = Op(eng, fn, is_dma)
        rb = _bufs(reads)
        wb = _bufs(writes)

        def hist(b):
            hs = [b]
            if b.parent is not None:
                hs.append(b.parent)
            else:
                hs.extend(b.subs.values())
            return hs

        for b in rb:
            for h in hist(b):
                if h.last_w is not None:
                    o.deps[h.last_w] = True
        for b in wb:
            for h in hist(b):
                if h.last_w is not None and h.last_w not in o.deps:
                    o.deps[h.last_w] = False
                for r in h.readers:
                    if r not in o.deps:
                        o.deps[r] = False
        if eng in self._bar_pending:
            self._bar_pending.discard(eng)
            for d in self._bar_frontier:
                o.deps[d] = True
        if is_dma:
            self._dmas_since_bar.append(o)
        else:
            self._last_compute[eng] = o
        for b in rb:
            b.readers.append(o)
        for b in wb:
            b.last_w = o
            b.readers = []
            if b.parent is None:
                for s in b.subs.values():
                    s.last_w = o
                    s.readers = []
        self.ops.append(o)
        return o

    def dma(self, eng, out_ap, in_ap, reads=(), writes=(), **kw):
        return self.op(eng, lambda e: e.dma_start(out=out_ap, in_=in_ap, **kw), reads, writes, is_dma=True)

    def emit(self):
        nc = self.nc
        streams = {e: [] for e in ENGS}
        for o in self.ops:
            streams[o.eng].append(o)

        def skip(o, d, raw):
            return (not d.is_dma) and d.eng == o.eng and (not o.is_dma) and o.eng == "pe"

        for o in self.ops:
            for d, raw in o.deps.items():
                if d.is_dma or skip(o, d, raw):
                    continue
                d.signal = True
        ctr_sems = {}
        for e in ENGS:
            k = 0
            for o in streams[e]:
                if o.signal and not o.is_dma:
                    k += 1
                    o.tick = k
            ctr_sems[e] = [nc.alloc_semaphore(f"ctr_{e}_{i}") for i in range((k + SEM_CAP - 1) // SEM_CAP)]
        finals = {}
        for e in ENGS:
            dmas = [o for o in streams[e] if o.is_dma]
            if not dmas:
                continue
            pool = [nc.alloc_semaphore(f"dma_{e}_{i}") for i in range(min(N_DMA_SEMS, len(dmas)))]
            vals = [0] * len(pool)
            for i, o in enumerate(dmas):
                j = i % len(pool)
                if vals[j] > 0:
                    o.pre_dma_wait = (pool[j], vals[j])
                vals[j] += 16
                o.dsem, o.dval = pool[j], vals[j]
            finals[e] = [(pool[j], vals[j]) for j in range(len(pool))]
        for e in ENGS:
            waited = {}
            for o in streams[e]:
                ws = {}
                cands = []
                for d, raw in o.deps.items():
                    if d.is_dma:
                        cands.append((d.dsem, d.dval, ("dma", id(d.dsem))))
                    elif not skip(o, d, raw):
                        t = d.tick - 1
                        cands.append((ctr_sems[d.eng][t // SEM_CAP], (t % SEM_CAP) + 1, (d.eng, t // SEM_CAP)))
                if o.pre_dma_wait is not None:
                    cands.append((o.pre_dma_wait[0], o.pre_dma_wait[1], ("dma", id(o.pre_dma_wait[0]))))
                for sem, val, key in cands:
                    if waited.get(key, 0) >= val:
                        continue
                    if key not in ws or ws[key][1] < val:
                        ws[key] = (sem, val)
                for key, (sem, val) in ws.items():
                    waited[key] = val
                o.waits = list(ws.values())
        engmap = {"pe": "tensor", "act": "scalar", "dve": "vector", "pool": "gpsimd", "sp": "sync"}
        with nc.Block() as block:
            for e in ENGS:
                ops = streams[e]
                if not ops:
                    continue

                def body(eng, ops=ops, e=e):
                    for o in ops:
                        for sem, val in o.waits:
                            eng.wait_ge(sem, val)
                        ins = o.fn(eng)
                        if o.is_dma:
                            ins.then_inc(o.dsem, 16)
                        elif o.signal:
                            ins.then_inc(ctr_sems[e][(o.tick - 1) // SEM_CAP], 1)
                    for sem, val in finals.get(e, []):
                        eng.wait_ge(sem, val)

                getattr(block, engmap[e])(body)
        return len(self.ops)


def _vec_layout():
    cols = {}
    n = 0

    def add(name, k):
        nonlocal n
        cols[name] = n
        n += k

    for l in range(DEPTH):
        add(f"ada_b{l}", 48)
        add(f"nmix{l}", 8)
        add(f"nffn{l}", 8)
        add(f"fconv{l}", 3 * NFC)
    add("sc_conv", 24)
    for nm in ("ax_qg", "ax_qgs", "ax_kg", "ax_kgs", "win_qg", "win_qgs", "win_kg", "win_kgs"):
        add(nm, 1)
    add("win_sink", 16)
    add("ml_bg", 16)
    add("ml_hn", 2)
    return cols, n


VCOLS, NV = _vec_layout()


def _colmajor(v):
    v = np.asarray(v, np.float32).reshape(-1, 128)
    return v.T


def pack_vecs(inp):
    out = np.zeros((128, NV), np.float32)

    def put(name, arr):
        a = _colmajor(arr)
        out[:, VCOLS[name]:VCOLS[name] + a.shape[1]] = a

    for l in range(DEPTH):
        put(f"ada_b{l}", inp["ada_b"][l])
        put(f"nmix{l}", inp["norm_mix"][l])
        put(f"nffn{l}", inp["norm_ffn"][l])
        put(f"fconv{l}", inp["ffn_conv"][l].reshape(-1))
    put("sc_conv", inp["sc_conv"][0].reshape(-1))

    def swap(v):
        h = v.shape[0] // 2
        return np.concatenate([v[h:], v[:h]])

    put("ax_qg", inp["ax_q_norm"][0])
    put("ax_qgs", swap(inp["ax_q_norm"][0]))
    put("ax_kg", inp["ax_k_norm"][0])
    put("ax_kgs", swap(inp["ax_k_norm"][0]))
    put("win_qg", np.tile(inp["win_q_norm"][0], 2))
    put("win_qgs", np.tile(swap(inp["win_q_norm"][0]), 2))
    put("win_kg", np.tile(inp["win_k_norm"][0], 2))
    put("win_kgs", np.tile(swap(inp["win_k_norm"][0]), 2))
    out[:, VCOLS["win_sink"]:VCOLS["win_sink"] + 16] = np.broadcast_to(inp["win_sink"][0][None, :], (128, 16))
    out[:, VCOLS["ml_bg"]:VCOLS["ml_bg"] + 16] = np.broadcast_to(inp["ml_b_gate"][0][None, :], (128, 16))
    put("ml_hn", inp["ml_h_norm"][0])
    return out


def rope_tables(hd):
    n_freq = hd // 4
    rows = S // 64
    row = np.repeat(np.arange(rows, dtype=np.float32), 64)
    col = np.tile(np.arange(64, dtype=np.float32), rows)
    inv = np.power(np.float32(10000.0), -np.arange(n_freq, dtype=np.float32) / np.float32(n_freq)).astype(np.float32)
    ang = np.concatenate([row[:, None] * inv, col[:, None] * inv], axis=-1).astype(np.float32)
    cos = np.cos(ang).astype(np.float32).T
    sin = np.sin(ang).astype(np.float32).T
    cos2 = np.concatenate([cos, cos], axis=0)
    sin2 = np.concatenate([-sin, sin], axis=0)
    rep = 128 // hd
    cos2 = np.tile(cos2, (rep, 1))
    sin2 = np.tile(sin2, (rep, 1))
    c = np.ones((128, S + CTX), np.float32)
    sn = np.zeros((128, S + CTX), np.float32)
    c[:, :S] = cos2
    sn[:, :S] = sin2
    return c, sn


class K:
    def __init__(self, test=None):
        self.test = test
        nc = bass.Bass("TRN2", target_bir_lowering=False)
        self.nc = nc
        P = Prog(nc)
        self.P = P
        dr = lambda n, s, k="ExternalInput": P.dram(n, s, F32, kind=k)
        self.xT = dr("xT", [D, S])
        self.ctxT = dr("ctxT", [D, CTX])
        self.cc = dr("cc", [128, 8, 2])
        self.vecs = dr("vecs", [128, NV])
        self.ada_w = dr("ada_w", [DEPTH, D, 6 * D])
        self.w_up = dr("ffn_w_up", [DEPTH, D, 2 * DFF])
        self.w_down = dr("ffn_w_down", [DEPTH, DFF, D])
        self.sc_w_in = dr("sc_w_in", [D, 3 * D])
        self.sc_w_out = dr("sc_w_out", [D, D])
        self.ax_w_qkv = dr("ax_w_qkv", [D, 1536])
        self.ax_w_o = dr("ax_w_o", [D, D])
        self.win_w_qkv = dr("win_w_qkv", [D, 1536])
        self.win_w_o = dr("win_w_o", [D, D])
        self.cos128 = dr("cos128", [128, S + CTX])
        self.sin128 = dr("sin128", [128, S + CTX])
        self.cos64 = dr("cos64", [128, S + CTX])
        self.sin64 = dr("sin64", [128, S + CTX])
        self.wmask = dr("wmask", [128, 1024])
        self.ml_w_in = dr("ml_w_in", [D, 3088])
        self.ml_w_out = dr("ml_w_out", [D, D])
        self.trimat = dr("trimat", [128, 256])
        self.hf = dr("hf_scratch", [D, S], "Internal")
        self.outT = dr("outT", [D, S], "ExternalOutput")
        self.hA = dr("hA", [D, S], "Internal")
        self.hB = dr("hB", [D, S], "Internal")
        self.cA = dr("cA", [D, CTX], "Internal")
        self.cB = dr("cB", [D, CTX], "Internal")
        self.ones = P.alloc("ones", [128, 128], BF16)
        self.vt = P.alloc("vt", [128, NV], F32)
        self.mod = P.alloc("mod", [128, DEPTH, 48, 2], F32)
        self.gsm = P.alloc("gsm", [128, DEPTH, 8, 2], F32)
        self.gsf = P.alloc("gsf", [128, DEPTH, 8, 2], F32)
        self.epsb = P.alloc("epsb", [128, 1], F32)
        self.ones_bd = P.alloc("ones_bd", [128, 128], BF16)
        self.zero_c = P.alloc("zero_c", [128, 1], F32)
        self.psw = [P.ps(f"psw{i}", [128, 1024]) for i in range(4)]
        self.ps = [T(self.psw[i // 2].t[:, (i % 2) * 512:(i % 2 + 1) * 512], f"ps{i}") for i in range(8)]
        self.base_mark = P.mark()

    def setup(self):
        P = self.P
        P.op("pool", lambda e: e.memset(self.ones[:], 1.0), writes=[self.ones])
        P.op("pool", lambda e: e.memset(self.ones_bd[:], 0.0), writes=[self.ones_bd])
        P.op("pool", lambda e: e.memset(self.ones_bd[0:64, 0:64], 1.0), writes=[self.ones_bd])
        P.op("pool", lambda e: e.memset(self.ones_bd[64:128, 64:128], 1.0), writes=[self.ones_bd])
        P.op("pool", lambda e: e.memset(self.epsb[:], EPS), writes=[self.epsb])
        P.op("pool", lambda e: e.memset(self.zero_c[:], 0.0), writes=[self.zero_c])
        P.dma("sp", self.vt[:], self.vecs[:], writes=[self.vt])

    def adaln(self):
        P = self.P
        m = P.mark()
        cct = P.alloc("cct", [128, 8, 2], F32)
        sig = P.alloc("sig", [128, 8, 2], F32)
        sc = P.alloc("sc", [128, 8, 2], F32)
        P.dma("sp", cct[:], self.cc[:], writes=[cct])
        P.op("act", lambda e: e.activation(out=sig[:], in_=cct[:], func=AF.Sigmoid), reads=[cct], writes=[sig])
        P.op("dve", lambda e: e.tensor_tensor(out=sc[:], in0=cct[:], in1=sig[:], op=ALU.mult), reads=[cct, sig], writes=[sc])
        NP = 8
        PW = 768
        wbuf = [P.alloc(f"adaw{i}", [128, 8, PW], F32) for i in range(2)]
        pst = self.ps[0]
        k = 0
        for l in range(DEPTH):
            for pc in range(NP):
                wb = wbuf[k % 2]
                k += 1
                P.dma("sp", wb[:], self.ada_w[l, :, pc * PW:(pc + 1) * PW].rearrange("(c p) f -> p c f", p=128), writes=[wb])
                for jj in range(PW // 128):
                    j = pc * (PW // 128) + jj
                    for c in range(8):
                        P.op("pe", lambda e, wb=wb, jj=jj, c=c, j=j: e.matmul(pst[:, 2 * j:2 * j + 2], wb[:, c, jj * 128:(jj + 1) * 128], sc[:, c, :], start=(c == 0), stop=(c == 7)),
                             reads=[wb, sc], writes=[pst])
            cb = VCOLS[f"ada_b{l}"]
            for s in range(2):
                P.op("dve", lambda e, l=l, s=s, cb=cb: e.tensor_tensor(out=self.mod[:, l, :, s], in0=pst[:, 0:96].rearrange("p (j s) -> p j s", s=2)[:, :, s], in1=self.vt[:, cb:cb + 48], op=ALU.add),
                     reads=[pst, self.vt], writes=[self.mod])
            for s in range(2):
                for (dst, vi, nm) in ((self.gsm, 1, f"nmix{l}"), (self.gsf, 4, f"nffn{l}")):
                    cn = VCOLS[nm]
                    P.op("dve", lambda e, l=l, s=s, dst=dst, vi=vi, cn=cn: e.scalar_tensor_tensor(out=dst[:, l, :, s], in0=self.mod[:, l, vi * 8:vi * 8 + 8, s], scalar=1.0, in1=self.vt[:, cn:cn + 8], op0=ALU.add, op1=ALU.mult),
                         reads=[self.mod, self.vt], writes=[dst])
        P.barrier()
        P.release(m)

    def modcol(self, l, vi, c, s):
        return self.mod[:, l, vi * 8 + c, s:s + 1]

    def norm_mod(self, src, t0, W, gs, l, shift_vi, s, a_out, ht, sq, rs, pst):
        P = self.P
        P.dma("sp", ht[:, :, 0:W], src[:, t0:t0 + W].rearrange("(c p) t -> p c t", p=128), writes=[ht])
        for c in range(8):
            q = sq[c % len(sq)]
            P.op("act", lambda e, c=c, q=q: e.activation(out=q[:, 0:W], in_=ht[:, c, 0:W], func=AF.Square), reads=[ht], writes=[q])
            P.op("pe", lambda e, c=c, q=q: e.matmul(pst[:, 0:W], self.ones[:], q[:, 0:W], start=(c == 0), stop=(c == 7)), reads=[self.ones, q], writes=[pst])
        P.op("act", lambda e: e.activation(out=rs[:, 0:W], in_=pst[:, 0:W], func=AF.Sqrt, bias=self.epsb[:], scale=1.0 / D), reads=[pst, self.epsb], writes=[rs])
        P.op("dve", lambda e: e.reciprocal(rs[:, 0:W], rs[:, 0:W]), reads=[rs], writes=[rs])
        for c in range(8):
            P.op("dve", lambda e, c=c: e.tensor_tensor(out=ht[:, c, 0:W], in0=ht[:, c, 0:W], in1=rs[:, 0:W], op=ALU.mult),
                 reads=[ht.sub(c), rs], writes=[ht.sub(c)])
            P.op("act", lambda e, c=c: e.activation(out=a_out[:, c, 0:W], in_=ht[:, c, 0:W], func=AF.Identity, bias=self.modcol(l, shift_vi, c, s), scale=gs[:, l, c, s:s + 1]),
                 reads=[ht.sub(c), self.mod, gs], writes=[a_out.sub(c)])

    def load_cast(self, dst, dst_kc, w_ap_rows, ncols, stage, k0=0):
        P = self.P
        PIECE = stage[0].t.shape[-1] if False else 2048
        c0 = 0
        k = k0
        while c0 < ncols:
            n = min(PIECE, ncols - c0)
            st = stage[k % len(stage)]
            P.dma("sp", st[:, 0:n], w_ap_rows[:, c0:c0 + n], writes=[st])
            eng = "pool" if k % 2 == 0 else "dve"
            P.op(eng, lambda e, st=st, n=n, c0=c0: e.tensor_copy(dst[:, dst_kc, c0:c0 + n], st[:, 0:n]), reads=[st], writes=[dst.sub(("w", dst_kc, c0))])
            c0 += n
            k += 1
        return k

    def ffn(self, l, src, dst, csrc, cdst, do_ctx, T_lat=S):
        P = self.P
        m = P.mark()
        wu = P.alloc("wu", [128, 8, 2 * DFF], BF16)
        wd = P.alloc("wd", [128, 22, D], BF16)
        m2 = P.mark()
        stage = [P.alloc(f"stg{i}", [128, 2048], F32) for i in range(2)]
        k = 0
        for kc in range(8):
            k = self.load_cast(wu, kc, self.w_up[l, kc * 128:(kc + 1) * 128, :], 2 * DFF, stage, k)
        for kc in range(22):
            k = self.load_cast(wd, kc, self.w_down[l, kc * 128:(kc + 1) * 128, :], D, stage, k)
        P.barrier()
        P.release(m2)
        ht = P.alloc("ht", [128, 8, 512], F32)
        sq = [P.alloc(f"sq{i}", [128, 512], BF16) for i in range(2)]
        rs = P.alloc("rs", [128, 512], F32)
        a = P.alloc("a", [128, 8, 512], BF16)
        NU = 3
        uc = [P.alloc(f"uc{i}", [128, 514], BF16) for i in range(NU)]
        yc = [P.alloc(f"yc{i}", [128, 512], BF16) for i in range(NU + 1)]
        sg = [P.alloc(f"sg{i}", [128, 512], BF16) for i in range(2)]
        carry = P.alloc("carry", [128, NFC, 2], BF16)
        gu = P.alloc("gu", [128, 22, 512], BF16)
        hr = [P.alloc(f"hr{i}", [128, 512], F32) for i in range(2)]
        cw = VCOLS[f"fconv{l}"]
        vt = self.vt
        ps_ss = self.ps[0]
        ps_up = self.ps[1:4]
        ps_dn = self.ps[4:8]
        cnt = {"u": 0, "y": 0, "d": 0, "g": 0, "h": 0}

        def up_conv(t0, W, flush):
            for i in range(22):
                ycs = []
                for half in range(2):
                    fc = half * 22 + i
                    u = uc[cnt["u"] % NU]
                    pu = ps_up[cnt["u"] % 3]
                    cnt["u"] += 1
                    y = yc[cnt["y"] % (NU + 1)]
                    cnt["y"] += 1
                    P.op("pool", lambda e, u=u, fc=fc: e.tensor_copy(u[:, 0:2], carry[:, fc, :]), reads=[carry.sub(fc)], writes=[u])
                    if flush:
                        P.op("pool", lambda e, u=u: e.memset(u[:, 2:3], 0.0), writes=[u])
                    else:
                        for c in range(8):
                            P.op("pe", lambda e, c=c, fc=fc, pu=pu, W=W: e.matmul(pu[:, 0:W], wu[:, c, fc * 128:(fc + 1) * 128], a[:, c, 0:W], start=(c == 0), stop=(c == 7)),
                                 reads=[wu, a.sub(c)], writes=[pu])
                        P.op("act", lambda e, u=u, pu=pu, W=W: e.activation(out=u[:, 2:2 + W], in_=pu[:, 0:W], func=AF.Copy), reads=[pu], writes=[u])
                        P.op("pool", lambda e, u=u, fc=fc, W=W: e.tensor_copy(carry[:, fc, :], u[:, W:W + 2]), reads=[u], writes=[carry.sub(fc)])
                    P.op("dve", lambda e, u=u, y=y, fc=fc, W=W: e.tensor_scalar(y[:, 0:W], u[:, 0:W], vt[:, cw + fc:cw + fc + 1], None, op0=ALU.mult), reads=[u, vt], writes=[y])
                    P.op("dve", lambda e, u=u, y=y, fc=fc, W=W: e.scalar_tensor_tensor(out=y[:, 0:W], in0=u[:, 1:1 + W], scalar=vt[:, cw + NFC + fc:cw + NFC + fc + 1], in1=y[:, 0:W], op0=ALU.mult, op1=ALU.add), reads=[u, vt, y], writes=[y])
                    P.op("dve", lambda e, u=u, y=y, fc=fc, W=W: e.scalar_tensor_tensor(out=y[:, 0:W], in0=u[:, 2:2 + W], scalar=vt[:, cw + 2 * NFC + fc:cw + 2 * NFC + fc + 1], in1=y[:, 0:W], op0=ALU.mult, op1=ALU.add), reads=[u, vt, y], writes=[y])
                    ycs.append(y)
                sgt = sg[cnt["g"] % 2]
                cnt["g"] += 1
                P.op("act", lambda e, sgt=sgt, y=ycs[0], W=W: e.activation(out=sgt[:, 0:W], in_=y[:, 0:W], func=AF.Silu), reads=[ycs[0]], writes=[sgt])
                P.op("dve", lambda e, sgt=sgt, y=ycs[1], i=i, W=W: e.tensor_tensor(out=gu[:, i, 0:W], in0=sgt[:, 0:W], in1=y[:, 0:W], op=ALU.mult), reads=[sgt, ycs[1]], writes=[gu.sub(i)])

        def down(hsrc, hdst, t0, W, s):
            o0 = 1 if t0 == 0 else 0
            for oc in range(8):
                pd = ps_dn[cnt["d"] % 4]
                cnt["d"] += 1
                hrt = hr[cnt["h"] % 2]
                cnt["h"] += 1
                kw = {"allow_slow_non_contiguous": True} if W - o0 == 1 else {}
                P.dma("sp", hrt[:, o0:W], hsrc[oc * 128:(oc + 1) * 128, t0 - 1 + o0:t0 - 1 + W], writes=[hrt], **kw)
                for i in range(22):
                    P.op("pe", lambda e, i=i, oc=oc, pd=pd, W=W: e.matmul(pd[:, 0:W], wd[:, i, oc * 128:(oc + 1) * 128], gu[:, i, 0:W], start=(i == 0), stop=(i == 21)),
                         reads=[wd, gu.sub(i)], writes=[pd])
                P.op("dve", lambda e, pd=pd, hrt=hrt, oc=oc, W=W, o0=o0: e.scalar_tensor_tensor(out=hrt[:, o0:W], in0=pd[:, o0:W], scalar=self.modcol(l, 5, oc, s), in1=hrt[:, o0:W], op0=ALU.mult, op1=ALU.add),
                     reads=[pd, hrt, self.mod], writes=[hrt])
                P.dma("sp", hdst[oc * 128:(oc + 1) * 128, t0 - 1 + o0:t0 - 1 + W], hrt[:, o0:W], reads=[hrt], **kw)

        def seq(hsrc, hdst, T_, s):
            P.op("pool", lambda e: e.memset(carry[:], 0.0), writes=[carry])
            tiles = [(t0, min(512, T_ - t0), False) for t0 in range(0, T_, 512)] + [(T_, 1, True)]
            self.norm_mod(hsrc, 0, tiles[0][1], self.gsf, l, 3, s, a, ht, sq, rs, ps_ss)
            for j, (t0, W, flush) in enumerate(tiles):
                up_conv(t0, W, flush)
                if j + 1 < len(tiles) and not tiles[j + 1][2]:
                    self.norm_mod(hsrc, tiles[j + 1][0], tiles[j + 1][1], self.gsf, l, 3, s, a, ht, sq, rs, ps_ss)
                down(hsrc, hdst, t0, W, s)

        seq(src, dst, T_lat, 0)
        if do_ctx:
            seq(csrc, cdst, CTX, 1)
        P.barrier()
        P.release(m)

    def proj_res(self, hsrc, hdst, t0, W, l, s, wmat, zin, nk, gate_vi, hr, ps_dn, cnt, shifted):
        P = self.P
        tb = t0 - 1 if shifted else t0
        o0 = 1 if (shifted and t0 == 0) else 0
        for oc in range(8):
            pd = ps_dn[cnt["d"] % len(ps_dn)]
            cnt["d"] += 1
            hrt = hr[cnt["h"] % len(hr)]
            cnt["h"] += 1
            kw = {"allow_slow_non_contiguous": True} if W - o0 == 1 else {}
            P.dma("sp", hrt[:, o0:W], hsrc[oc * 128:(oc + 1) * 128, tb + o0:tb + W], writes=[hrt], **kw)
            for i in range(nk):
                P.op("pe", lambda e, i=i, oc=oc, pd=pd: e.matmul(pd[:, 0:W], wmat[:, i, oc * 128:(oc + 1) * 128], zin[:, i, 0:W], start=(i == 0), stop=(i == nk - 1)),
                     reads=[wmat, zin.sub(i)], writes=[pd])
            P.op("dve", lambda e, pd=pd, hrt=hrt, oc=oc: e.scalar_tensor_tensor(out=hrt[:, o0:W], in0=pd[:, o0:W], scalar=self.modcol(l, gate_vi, oc, s), in1=hrt[:, o0:W], op0=ALU.mult, op1=ALU.add),
                 reads=[pd, hrt, self.mod], writes=[hrt])
            P.dma("sp", hdst[oc * 128:(oc + 1) * 128, tb + o0:tb + W], hrt[:, o0:W], reads=[hrt], **kw)

    def mixer_sc(self, l, src, dst, csrc, cdst, T_lat=S):
        P = self.P
        m = P.mark()
        wi = P.alloc("wi", [128, 8, 3 * D], BF16)
        wo = P.alloc("wo", [128, 8, D], BF16)
        m2 = P.mark()
        stage = [P.alloc(f"stg{i}", [128, 2048], F32) for i in range(2)]
        k = 0
        for kc in range(8):
            k = self.load_cast(wi, kc, self.sc_w_in[kc * 128:(kc + 1) * 128, :], 3 * D, stage, k)
        for kc in range(8):
            k = self.load_cast(wo, kc, self.sc_w_out[kc * 128:(kc + 1) * 128, :], D, stage, k)
        P.barrier()
        P.release(m2)
        ht = P.alloc("ht", [128, 8, 512], F32)
        sq = [P.alloc(f"sq{i}", [128, 512], BF16) for i in range(2)]
        rs = P.alloc("rs", [128, 512], F32)
        a = P.alloc("a", [128, 8, 512], BF16)
        cu = [P.alloc(f"cu{i}", [128, 514], BF16) for i in range(2)]
        us = [P.alloc(f"us{i}", [128, 512], F32) for i in range(2)]
        bb = [P.alloc(f"bb{i}", [128, 513], BF16) for i in range(2)]
        yc = [P.alloc(f"yc{i}", [128, 512], BF16) for i in range(2)]
        carry_cu = P.alloc("carry_cu", [128, 8, 2], BF16)
        carry_b = P.alloc("carry_b", [128, 8, 1], BF16)
        z = P.alloc("z", [128, 8, 512], BF16)
        hr = [P.alloc(f"hr{i}", [128, 512], F32) for i in range(2)]
        cw = VCOLS["sc_conv"]
        vt = self.vt
        ps_ss = self.ps[0]
        ps_in = self.ps[1:5]
        ps_dn = self.ps[5:8]
        cnt = {"u": 0, "p": 0, "d": 0, "h": 0}

        def inproj(pt, col0, W):
            for c in range(8):
                P.op("pe", lambda e, c=c: e.matmul(pt[:, 0:W], wi[:, c, col0:col0 + 128], a[:, c, 0:W], start=(c == 0), stop=(c == 7)), reads=[wi, a.sub(c)], writes=[pt])

        def body(t0, W, flush):
            for i in range(8):
                cut = cu[cnt["u"] % 2]
                ust = us[cnt["u"] % 2]
                bbt = bb[cnt["u"] % 2]
                y = yc[cnt["u"] % 2]
                cnt["u"] += 1
                P.op("pool", lambda e, cut=cut, i=i: e.tensor_copy(cut[:, 0:2], carry_cu[:, i, :]), reads=[carry_cu.sub(i)], writes=[cut])
                P.op("pool", lambda e, bbt=bbt, i=i: e.tensor_copy(bbt[:, 0:1], carry_b[:, i, :]), reads=[carry_b.sub(i)], writes=[bbt])
                if flush:
                    P.op("pool", lambda e, cut=cut: e.memset(cut[:, 2:3], 0.0), writes=[cut])
                else:
                    pb, pc, pu = (ps_in[(cnt["p"] + j) % 4] for j in range(3))
                    cnt["p"] += 3
                    inproj(pb, i * 128, W)
                    inproj(pc, D + i * 128, W)
                    inproj(pu, 2 * D + i * 128, W)
                    P.op("act", lambda e, ust=ust, pu=pu: e.activation(out=ust[:, 0:W], in_=pu[:, 0:W], func=AF.Copy), reads=[pu], writes=[ust])
                    P.op("act", lambda e, bbt=bbt, pb=pb: e.activation(out=bbt[:, 1:1 + W], in_=pb[:, 0:W], func=AF.Copy), reads=[pb], writes=[bbt])
                    P.op("dve", lambda e, cut=cut, pc=pc, ust=ust: e.tensor_tensor(out=cut[:, 2:2 + W], in0=pc[:, 0:W], in1=ust[:, 0:W], op=ALU.mult), reads=[pc, ust], writes=[cut])
                    P.op("pool", lambda e, cut=cut, i=i: e.tensor_copy(carry_cu[:, i, :], cut[:, W:W + 2]), reads=[cut], writes=[carry_cu.sub(i)])
                    P.op("pool", lambda e, bbt=bbt, i=i: e.tensor_copy(carry_b[:, i, :], bbt[:, W:W + 1]), reads=[bbt], writes=[carry_b.sub(i)])
                P.op("dve", lambda e, cut=cut, y=y, i=i: e.tensor_scalar(y[:, 0:W], cut[:, 0:W], vt[:, cw + i:cw + i + 1], None, op0=ALU.mult), reads=[cut, vt], writes=[y])
                P.op("dve", lambda e, cut=cut, y=y, i=i: e.scalar_tensor_tensor(out=y[:, 0:W], in0=cut[:, 1:1 + W], scalar=vt[:, cw + 8 + i:cw + 8 + i + 1], in1=y[:, 0:W], op0=ALU.mult, op1=ALU.add), reads=[cut, vt, y], writes=[y])
                P.op("dve", lambda e, cut=cut, y=y, i=i: e.scalar_tensor_tensor(out=y[:, 0:W], in0=cut[:, 2:2 + W], scalar=vt[:, cw + 16 + i:cw + 16 + i + 1], in1=y[:, 0:W], op0=ALU.mult, op1=ALU.add), reads=[cut, vt, y], writes=[y])
                P.op("dve", lambda e, bbt=bbt, y=y, i=i: e.tensor_tensor(out=z[:, i, 0:W], in0=y[:, 0:W], in1=bbt[:, 0:W], op=ALU.mult), reads=[y, bbt], writes=[z.sub(i)])

        def seq(hsrc, hdst, T_, s):
            P.op("pool", lambda e: e.memset(carry_cu[:], 0.0), writes=[carry_cu])
            P.op("pool", lambda e: e.memset(carry_b[:], 0.0), writes=[carry_b])
            tiles = [(t0, min(512, T_ - t0), False) for t0 in range(0, T_, 512)] + [(T_, 1, True)]
            self.norm_mod(hsrc, 0, tiles[0][1], self.gsm, l, 0, s, a, ht, sq, rs, ps_ss)
            for j, (t0, W, flush) in enumerate(tiles):
                body(t0, W, flush)
                if j + 1 < len(tiles) and not tiles[j + 1][2]:
                    self.norm_mod(hsrc, tiles[j + 1][0], tiles[j + 1][1], self.gsm, l, 0, s, a, ht, sq, rs, ps_ss)
                self.proj_res(hsrc, hdst, t0, W, l, s, wo, z, 8, 2, hr, ps_dn, cnt, True)

        seq(src, dst, T_lat, 0)
        seq(csrc, cdst, CTX, 1)
        P.barrier()
        P.release(m)

    def load_qkv_weights(self, w_qkv, w_o, hd, nq):
        P = self.P
        nk = 1536 - nq - (1536 - nq) // 2
        wq = P.alloc("wq", [128, 8, nq], BF16)
        wqs = P.alloc("wqs", [128, 8, nq], BF16)
        wk = P.alloc("wk", [128, 8, 256], BF16)
        wks = P.alloc("wks", [128, 8, 256], BF16)
        wv = P.alloc("wv", [128, 8, 256], BF16)
        wo = P.alloc("wo", [128, 8, D], BF16)
        m2 = P.mark()
        stage = [P.alloc(f"stg{i}", [128, 2048], F32) for i in range(2)]
        h2 = hd // 2
        for kc in range(8):
            st = stage[kc % 2]
            P.dma("sp", st[:, 0:1536], w_qkv[kc * 128:(kc + 1) * 128, :], writes=[st])
            P.op("dve", lambda e, st=st, kc=kc: e.tensor_copy(wq[:, kc, :], st[:, 0:nq]), reads=[st], writes=[wq.sub(kc)])
            P.op("pool", lambda e, st=st, kc=kc: e.tensor_copy(wk[:, kc, :], st[:, nq:nq + 256]), reads=[st], writes=[wk.sub(kc)])
            P.op("pool", lambda e, st=st, kc=kc: e.tensor_copy(wv[:, kc, :], st[:, nq + 256:nq + 512]), reads=[st], writes=[wv.sub(kc)])
            for (dstw, c0, n) in ((wqs, 0, nq), (wks, nq, 256)):
                for t in range(2):
                    P.op("dve" if t == 0 else "pool",
                         lambda e, st=st, kc=kc, dstw=dstw, c0=c0, n=n, t=t: e.tensor_copy(
                             dstw[:, kc, :].rearrange("p (h t d) -> p h t d", t=2, d=h2)[:, :, t, :],
                             st[:, c0:c0 + n].rearrange("p (h t d) -> p h t d", t=2, d=h2)[:, :, 1 - t, :]),
                         reads=[st], writes=[dstw.sub((kc, t))])
        k = 0
        for kc in range(8):
            k = self.load_cast(wo, kc, w_o[kc * 128:(kc + 1) * 128, :], D, stage, k)
        P.barrier()
        P.release(m2)
        return wq, wqs, wk, wks, wv, wo

    def qk_norm_rope(self, pq, pqs, gcol, gscol, hd, cost, sint, W, out_ap, out_buf, bufs, ps_ss2):
        P = self.P
        sqb, rsb, t1, t2 = bufs
        onesm = self.ones if hd == 128 else self.ones_bd
        vt = self.vt
        P.op("act", lambda e: e.activation(out=sqb[:, 0:W], in_=pq[:, 0:W], func=AF.Square), reads=[pq], writes=[sqb])
        P.op("pe", lambda e: e.matmul(ps_ss2[:, 0:W], onesm[:], sqb[:, 0:W], start=True, stop=True), reads=[onesm, sqb], writes=[ps_ss2])
        P.op("act", lambda e: e.activation(out=rsb[:, 0:W], in_=ps_ss2[:, 0:W], func=AF.Sqrt, bias=self.epsb[:], scale=1.0 / hd), reads=[ps_ss2, self.epsb], writes=[rsb])
        P.op("dve", lambda e: e.reciprocal(rsb[:, 0:W], rsb[:, 0:W]), reads=[rsb], writes=[rsb])
        P.op("dve", lambda e: e.scalar_tensor_tensor(out=t1[:, 0:W], in0=pq[:, 0:W], scalar=vt[:, gcol:gcol + 1], in1=rsb[:, 0:W], op0=ALU.mult, op1=ALU.mult), reads=[pq, vt, rsb], writes=[t1])
        P.op("dve", lambda e: e.scalar_tensor_tensor(out=t2[:, 0:W], in0=pqs[:, 0:W], scalar=vt[:, gscol:gscol + 1], in1=rsb[:, 0:W], op0=ALU.mult, op1=ALU.mult), reads=[pqs, vt, rsb], writes=[t2])
        P.op("dve", lambda e: e.tensor_tensor(out=t1[:, 0:W], in0=t1[:, 0:W], in1=cost[:, 0:W], op=ALU.mult), reads=[t1, cost], writes=[t1])
        P.op("dve", lambda e: e.tensor_tensor(out=t2[:, 0:W], in0=t2[:, 0:W], in1=sint[:, 0:W], op=ALU.mult), reads=[t2, sint], writes=[t2])
        P.op("dve", lambda e: e.tensor_tensor(out=out_ap, in0=t1[:, 0:W], in1=t2[:, 0:W], op=ALU.add), reads=[t1, t2], writes=[out_buf])

    def mixer_dense(self, l, src, dst, csrc, cdst, T_lat=S):
        P = self.P
        m = P.mark()
        HD = 128
        NKB = (T_lat + CTX) // 128
        wq, wqs, wk, wks, wv, wo = self.load_qkv_weights(self.ax_w_qkv, self.ax_w_o, HD, 1024)
        KT = P.alloc("KT", [128, 2, T_lat + CTX], BF16)
        V = P.alloc("V", [128, NKB, 256], BF16)
        ht = P.alloc("ht", [128, 8, 512], F32)
        sq = [P.alloc(f"sq{i}", [128, 512], BF16) for i in range(2)]
        rs = P.alloc("rs", [128, 512], F32)
        a = P.alloc("a", [128, 8, 512], BF16)
        cost = P.alloc("cost", [128, 512], F32)
        sint = P.alloc("sint", [128, 512], F32)
        nbs = [(P.alloc(f"nsq{i}", [128, 512], BF16), P.alloc(f"nrs{i}", [128, 512], F32), P.alloc(f"nt1{i}", [128, 512], F32), P.alloc(f"nt2{i}", [128, 512], F32)) for i in range(2)]
        nb = nbs[0]
        Q = P.alloc("Q", [128, 8, 512], BF16)
        Ew = [P.alloc(f"Ew{i}", [128, 1024], BF16) for i in range(3)]
        Esum = [P.alloc(f"Esum{i}", [128, 512], BF16) for i in range(2)]
        rden = rs
        att = P.alloc("att", [128, 8, 512], BF16)
        hr = [P.alloc(f"hr{i}", [128, 512], F32) for i in range(2)]
        vt = self.vt
        ps = self.ps
        gq, gqs, gk, gks = VCOLS["ax_qg"], VCOLS["ax_qgs"], VCOLS["ax_kg"], VCOLS["ax_kgs"]
        cnt = {"d": 0, "h": 0, "e": 0, "s": 0, "o": 0, "p": 0, "v": 0}

        def proj(pt, w, col0, W):
            for c in range(8):
                P.op("pe", lambda e, c=c: e.matmul(pt[:, 0:W], w[:, c, col0:col0 + 128], a[:, c, 0:W], start=(c == 0), stop=(c == 7)), reads=[w, a.sub(c)], writes=[pt])

        def load_tables(pos0, W):
            P.dma("sp", cost[:, 0:W], self.cos128[:, pos0:pos0 + W], writes=[cost])
            P.dma("sp", sint[:, 0:W], self.sin128[:, pos0:pos0 + W], writes=[sint])

        def phase_a(hsrc, T_, s, pos_base):
            for t0 in range(0, T_, 512):
                W = min(512, T_ - t0)
                pos0 = pos_base + t0
                self.norm_mod(hsrc, t0, W, self.gsm, l, 0, s, a, ht, sq, rs, ps[0])
                load_tables((S if s == 1 else 0) + t0, W)
                for g in range(2):
                    pk, pks = ps[1 + 2 * (cnt["p"] % 2)], ps[2 + 2 * (cnt["p"] % 2)]
                    cnt["p"] += 1
                    proj(pk, wk, g * 128, W)
                    proj(pks, wks, g * 128, W)
                    self.qk_norm_rope(pk, pks, gk, gks, HD, cost, sint, W, KT[:, g, pos0:pos0 + W], KT.sub((g, pos0)), nbs[g % 2], ps[5])
                for blk in range(W // 128):
                    pv = ps[6 + cnt["v"] % 2]
                    cnt["v"] += 1
                    for c in range(8):
                        P.op("pe", lambda e, c=c, blk=blk, pv=pv: e.matmul(pv[:, 0:256], a[:, c, blk * 128:(blk + 1) * 128], wv[:, c, :], start=(c == 0), stop=(c == 7)), reads=[wv, a.sub(c)], writes=[pv])
                    kb = pos0 // 128 + blk
                    P.op("act", lambda e, pv=pv, kb=kb: e.activation(out=V[:, kb, :], in_=pv[:, 0:256], func=AF.Copy), reads=[pv], writes=[V.sub(kb)])

        phase_a(src, T_lat, 0, 0)
        phase_a(csrc, CTX, 1, T_lat)

        scale = float(HD) ** -0.5

        def phase_b(hsrc, hdst, T_, s, pos_base, kblocks):
            for t0 in range(0, T_, 512):
                W = min(512, T_ - t0)
                pos0 = pos_base + t0
                self.norm_mod(hsrc, t0, W, self.gsm, l, 0, s, a, ht, sq, rs, ps[0])
                load_tables((S if s == 1 else 0) + t0, W)
                for h in range(8):
                    pa_, pb_ = (ps[1], ps[2]) if h % 2 == 0 else (ps[3], ps[4])
                    proj(pa_, wq, h * 128, W)
                    proj(pb_, wqs, h * 128, W)
                    self.qk_norm_rope(pa_, pb_, gq, gqs, HD, cost, sint, W, Q[:, h, 0:W], Q.sub(h), nbs[h % 2], ps[0] if h % 2 == 0 else ps[5])
                assert len(kblocks) % 2 == 0
                npair = len(kblocks) // 2
                items = [(h, jp) for h in range(8) for jp in range(npair)]
                banks = {}

                def issue_qk(n):
                    h, jp = items[n]
                    wide = 1 + n % 2
                    for half in range(2):
                        kb = kblocks[2 * jp + half]
                        pS = ps[2 * wide + half]
                        P.op("pe", lambda e, kb=kb, pS=pS, h=h: e.matmul(pS[:, 0:W], KT[:, h // 4, kb * 128:(kb + 1) * 128], Q[:, h, 0:W], start=True, stop=True), reads=[KT, Q.sub(h)], writes=[pS])
                    return wide

                w_next = issue_qk(0)
                for n, (h, jp) in enumerate(items):
                    g = h // 4
                    if jp == 0:
                        banks[h] = (ps[6 + h % 2], ps[h % 2])
                    po, pden = banks[h]
                    wide = w_next
                    Et = Ew[n % 3]
                    if W == 512:
                        P.op("act", lambda e, wide=wide, Et=Et: e.activation(out=Et[:, :], in_=self.psw[wide][:, :], func=AF.Exp, scale=scale), reads=[ps[2 * wide], ps[2 * wide + 1]], writes=[Et])
                    else:
                        for half in range(2):
                            P.op("act", lambda e, wide=wide, Et=Et, half=half: e.activation(out=Et[:, half * 512:half * 512 + W], in_=ps[2 * wide + half][:, 0:W], func=AF.Exp, scale=scale), reads=[ps[2 * wide + half]], writes=[Et])
                    if n + 1 < len(items):
                        w_next = issue_qk(n + 1)
                    st, sp_ = (jp == 0), (jp == npair - 1)
                    Es = Esum[n % 2]
                    P.op("dve", lambda e, Et=Et, Es=Es: e.tensor_tensor(out=Es[:, 0:W], in0=Et[:, 0:W], in1=Et[:, 512:512 + W], op=ALU.add), reads=[Et], writes=[Es])
                    for half in range(2):
                        kb = kblocks[2 * jp + half]
                        P.op("pe", lambda e, kb=kb, Et=Et, po=po, g=g, half=half, st=st, sp_=sp_: e.matmul(po[:, 0:W], V[:, kb, g * 128:(g + 1) * 128], Et[:, half * 512:half * 512 + W], start=(st and half == 0), stop=(sp_ and half == 1)), reads=[V, Et], writes=[po])
                    P.op("pe", lambda e, Es=Es, pden=pden, st=st, sp_=sp_: e.matmul(pden[:, 0:W], self.ones[:], Es[:, 0:W], start=st, stop=sp_), reads=[self.ones, Es], writes=[pden])
                    if sp_:
                        P.op("dve", lambda e, pden=pden: e.reciprocal(rden[:, 0:W], pden[:, 0:W]), reads=[pden], writes=[rden])
                        P.op("dve", lambda e, po=po, h=h: e.tensor_tensor(out=att[:, h, 0:W], in0=po[:, 0:W], in1=rden[:, 0:W], op=ALU.mult), reads=[po, rden], writes=[att.sub(h)])
                self.proj_res(hsrc, hdst, t0, W, l, s, wo, att, 8, 2, hr, [ps[1], ps[2]], cnt, False)

        phase_b(src, dst, T_lat, 0, 0, list(range(NKB)))
        phase_b(csrc, cdst, CTX, 1, T_lat, list(range(T_lat // 128, NKB)))
        P.barrier()
        P.release(m)

    def mixer_win(self, l, src, dst, csrc, cdst, T_lat=S):
        P = self.P
        m = P.mark()
        HD = 64
        NKB = (T_lat + CTX) // 128
        NLB = T_lat // 128
        wq = P.alloc("wq", [128, 8, 1024], BF16)
        wqs = P.alloc("wqs", [128, 8, 1024], BF16)
        wk = P.alloc("wk", [128, 8, 256], BF16)
        wks = P.alloc("wks", [128, 8, 256], BF16)
        wv = P.alloc("wv", [128, 8, 256], BF16)
        wo = P.alloc("wo", [128, 8, D], BF16)
        mk = P.alloc("mk", [128, 2, 512], BF16)
        es = P.alloc("es", [128, 16], F32)
        m2 = P.mark()
        stage = [P.alloc(f"stg{i}", [128, 2048], F32) for i in range(2)]
        k = 0
        for kc in range(8):
            st = stage[kc % 2]
            P.dma("sp", st[:, 0:1536], self.win_w_qkv[kc * 128:(kc + 1) * 128, :], writes=[st])
            P.op("pool", lambda e, st=st, kc=kc: e.tensor_copy(wv[:, kc, :], st[:, 1280:1536]), reads=[st], writes=[wv.sub(kc)])
            for (dw, dws, c0, n) in ((wq, wqs, 0, 1024), (wk, wks, 1024, 256)):
                for t in range(2):
                    P.op("dve" if t == 0 else "pool",
                         lambda e, st=st, kc=kc, dw=dw, c0=c0, n=n, t=t: e.tensor_copy(
                             dw[:, kc, :].rearrange("p (i t d) -> p i t d", t=2, d=64)[:, :, t, :],
                             st[:, c0:c0 + n].rearrange("p (t i d) -> p t i d", t=2, d=64)[:, t, :, :]),
                         reads=[st], writes=[dw.sub((kc, t))])
                    for u in range(2):
                        P.op("dve" if u == 0 else "pool",
                             lambda e, st=st, kc=kc, dws=dws, c0=c0, n=n, t=t, u=u: e.tensor_copy(
                                 dws[:, kc, :].rearrange("p (i t u d) -> p i t u d", t=2, u=2, d=32)[:, :, t, u, :],
                                 st[:, c0:c0 + n].rearrange("p (t i u d) -> p t i u d", t=2, u=2, d=32)[:, t, :, 1 - u, :]),
                             reads=[st], writes=[dws.sub((kc, t, u))])
        for kc in range(8):
            k = self.load_cast(wo, kc, self.win_w_o[kc * 128:(kc + 1) * 128, :], D, stage, k)
        st = stage[0]
        P.dma("sp", st[:, 0:1024], self.wmask[:, :], writes=[st])
        P.op("dve", lambda e: e.tensor_copy(mk[:].rearrange("p a b -> p (a b)"), st[:, 0:1024]), reads=[st], writes=[mk])
        sc0 = VCOLS["win_sink"]
        P.op("act", lambda e: e.activation(out=es[:], in_=self.vt[:, sc0:sc0 + 16], func=AF.Exp), reads=[self.vt], writes=[es])
        P.barrier()
        P.release(m2)
        KT = P.alloc("KT", [128, 2, T_lat + CTX], BF16)
        V = P.alloc("V", [128, NKB, 256], BF16)
        ht = P.alloc("ht", [128, 8, 512], F32)
        sq = [P.alloc(f"sq{i}", [128, 512], BF16) for i in range(2)]
        rs = P.alloc("rs", [128, 512], F32)
        a = P.alloc("a", [128, 8, 512], BF16)
        cost = P.alloc("cost", [128, 512], F32)
        sint = P.alloc("sint", [128, 512], F32)
        nbs = [(P.alloc(f"nsq{i}", [128, 512], BF16), P.alloc(f"nrs{i}", [128, 512], F32), P.alloc(f"nt1{i}", [128, 512], F32), P.alloc(f"nt2{i}", [128, 512], F32)) for i in range(2)]
        nb = nbs[0]
        Q = P.alloc("Q", [128, 8, 512], BF16)
        E = [P.alloc(f"E{i}", [128, 512], BF16) for i in range(3)]
        dtmp = P.alloc("dtmp", [128, 512], F32)
        rden = dtmp
        att = P.alloc("att", [128, 8, 512], BF16)
        hr = [P.alloc(f"hr{i}", [128, 512], F32) for i in range(2)]
        ps = self.ps
        gq, gqs, gk, gks = VCOLS["win_qg"], VCOLS["win_qgs"], VCOLS["win_kg"], VCOLS["win_kgs"]
        cnt = {"d": 0, "h": 0, "e": 0, "s": 0, "o": 0, "p": 0, "v": 0}

        def proj(pt, w, col0, W):
            for c in range(8):
                P.op("pe", lambda e, c=c: e.matmul(pt[:, 0:W], w[:, c, col0:col0 + 128], a[:, c, 0:W], start=(c == 0), stop=(c == 7)), reads=[w, a.sub(c)], writes=[pt])

        def load_tables(pos0, W):
            P.dma("sp", cost[:, 0:W], self.cos64[:, pos0:pos0 + W], writes=[cost])
            P.dma("sp", sint[:, 0:W], self.sin64[:, pos0:pos0 + W], writes=[sint])

        def phase_a(hsrc, T_, s, pos_base):
            for t0 in range(0, T_, 512):
                W = min(512, T_ - t0)
                pos0 = pos_base + t0
                self.norm_mod(hsrc, t0, W, self.gsm, l, 0, s, a, ht, sq, rs, ps[0])
                load_tables((S if s == 1 else 0) + t0, W)
                for j in range(2):
                    pk, pks = ps[1 + 2 * (cnt["p"] % 2)], ps[2 + 2 * (cnt["p"] % 2)]
                    cnt["p"] += 1
                    proj(pk, wk, j * 128, W)
                    proj(pks, wks, j * 128, W)
                    self.qk_norm_rope(pk, pks, gk, gks, HD, cost, sint, W, KT[:, j, pos0:pos0 + W], KT.sub((j, pos0)), nbs[j % 2], ps[5])
                for blk in range(W // 128):
                    pv = ps[6 + cnt["v"] % 2]
                    cnt["v"] += 1
                    for c in range(8):
                        P.op("pe", lambda e, c=c, blk=blk, pv=pv: e.matmul(pv[:, 0:256], a[:, c, blk * 128:(blk + 1) * 128], wv[:, c, :], start=(c == 0), stop=(c == 7)), reads=[wv, a.sub(c)], writes=[pv])
                    kb = pos0 // 128 + blk
                    P.op("act", lambda e, pv=pv, kb=kb: e.activation(out=V[:, kb, :], in_=pv[:, 0:256], func=AF.Copy), reads=[pv], writes=[V.sub(kb)])

        phase_a(src, T_lat, 0, 0)
        phase_a(csrc, CTX, 1, T_lat)
        scale = float(HD) ** -0.5

        def phase_b(hsrc, hdst, T_, s, pos_base, windowed):
            for t0 in range(0, T_, 512):
                W = min(512, T_ - t0)
                self.norm_mod(hsrc, t0, W, self.gsm, l, 0, s, a, ht, sq, rs, ps[0])
                load_tables((S if s == 1 else 0) + t0, W)
                for i in range(8):
                    pa_, pb_ = (ps[1], ps[2]) if i % 2 == 0 else (ps[3], ps[4])
                    proj(pa_, wq, i * 128, W)
                    proj(pb_, wqs, i * 128, W)
                    self.qk_norm_rope(pa_, pb_, gq, gqs, HD, cost, sint, W, Q[:, i, 0:W], Q.sub(i), nbs[i % 2], ps[0] if i % 2 == 0 else ps[5])
                items = []
                for qbl in range(W // 128):
                    if windowed:
                        qb = t0 // 128 + qbl
                        kbl = []
                        if qb > 0:
                            kbl.append((qb - 1, 0))
                        kbl.append((qb, None))
                        if qb < NLB - 1:
                            kbl.append((qb + 1, 1))
                        kbl += [(NLB, None), (NLB + 1, None)]
                    else:
                        kbl = [(NLB, None), (NLB + 1, None)]
                    for g in range(4):
                        for n_, (kb, mi) in enumerate(kbl):
                            items.append((qbl, g, kb, mi, n_ == 0, n_ == len(kbl) - 1))

                def issue_qk(n):
                    qbl, g, kb, mi, _, _ = items[n]
                    qs = slice(qbl * 128, (qbl + 1) * 128)
                    half, j, i0 = g // 2, g % 2, 4 * (g % 2)
                    hp = slice(half * 64, (half + 1) * 64)
                    pS = ps[2 + n % 3]
                    P.op("pe", lambda e, kb=kb, pS=pS, hp=hp, j=j, i0=i0, qs=qs: e.matmul(pS[:, 0:512], KT[hp, j, kb * 128:(kb + 1) * 128], Q[hp, i0:i0 + 4, qs], start=True, stop=True),
                         reads=[KT, Q], writes=[pS])
                    return pS

                pS_next = issue_qk(0)
                grp = 0
                for n, (qbl, g, kb, mi, st_, sp_) in enumerate(items):
                    qs = slice(qbl * 128, (qbl + 1) * 128)
                    if st_:
                        po = ps[5 + grp % 2]
                        pden = ps[7] if grp % 2 == 0 else ps[0]
                        grp += 1
                    pS = pS_next
                    Et = E[n % 3]
                    P.op("act", lambda e, pS=pS, Et=Et: e.activation(out=Et[:, :], in_=pS[:, 0:512], func=AF.Exp, scale=scale), reads=[pS], writes=[Et])
                    if n + 1 < len(items):
                        pS_next = issue_qk(n + 1)
                    if mi is not None:
                        P.op("dve", lambda e, Et=Et, mi=mi: e.tensor_tensor(out=Et[:, :], in0=Et[:, :], in1=mk[:, mi, :], op=ALU.mult), reads=[Et, mk], writes=[Et])
                    Ev = Et[:, :].rearrange("p (r q) -> p r q", q=128)
                    for par in range(2):
                        P.op("pe", lambda e, kb=kb, Ev=Ev, po=po, g=g, par=par, st_=st_, sp_=sp_: e.matmul(po[par * 64:(par + 1) * 64, 0:256], V[:, kb, g * 64:(g + 1) * 64], Ev[:, par::2, :], start=st_, stop=sp_, tile_position=(0, par * 64)),
                             reads=[V, Et], writes=[po])
                    P.op("pe", lambda e, Et=Et, pden=pden, st_=st_, sp_=sp_: e.matmul(pden[:, 0:512], self.ones[:], Et[:, :], start=st_, stop=sp_), reads=[self.ones, Et], writes=[pden])
                    if sp_:
                        for r in range(4):
                            hh = 4 * g + r
                            P.op("dve", lambda e, pden=pden, r=r, hh=hh: e.tensor_scalar(dtmp[:, r * 128:(r + 1) * 128], pden[:, r * 128:(r + 1) * 128], es[:, hh:hh + 1], None, op0=ALU.add), reads=[pden, es], writes=[dtmp])
                        P.op("dve", lambda e: e.reciprocal(rden[:, :], dtmp[:, :]), reads=[dtmp], writes=[rden])
                        for r in range(4):
                            rp = slice((r % 2) * 64, (r % 2 + 1) * 64)
                            cb = (r // 2) * 128
                            P.op("dve", lambda e, po=po, r=r, rp=rp, cb=cb, g=g, qs=qs: e.tensor_tensor(out=att[rp, 2 * g + r // 2, qs], in0=po[rp, cb:cb + 128], in1=rden[rp, r * 128:(r + 1) * 128], op=ALU.mult),
                                 reads=[po, rden], writes=[att.sub(2 * g + r // 2)])
                self.proj_res(hsrc, hdst, t0, W, l, s, wo, att, 8, 2, hr, [ps[1], ps[2]], cnt, False)

        phase_b(src, dst, T_lat, 0, 0, True)
        phase_b(csrc, cdst, CTX, 1, T_lat, False)
        P.barrier()
        P.release(m)

    def mixer_mlstm(self, l, src, dst, csrc, T_lat=S):
        P = self.P
        m = P.mark()
        wi = P.alloc("wi", [128, 8, 3088], BF16)
        wo = P.alloc("wo", [128, 8, D], BF16)
        tri = P.alloc("tri", [128, 2, 128], F32)
        trb = P.alloc("trb", [128, 2, 128], BF16)
        onesf = P.alloc("onesf", [128, 128], F32)
        m2 = P.mark()
        stage = [P.alloc(f"stg{i}", [128, 2048], F32) for i in range(2)]
        k = 0
        for kc in range(8):
            k = self.load_cast(wi, kc, self.ml_w_in[kc * 128:(kc + 1) * 128, :], 3088, stage, k)
        for kc in range(8):
            k = self.load_cast(wo, kc, self.ml_w_out[kc * 128:(kc + 1) * 128, :], D, stage, k)
        P.dma("sp", tri[:].rearrange("p a b -> p (a b)"), self.trimat[:, :], writes=[tri])
        P.op("dve", lambda e: e.tensor_copy(trb[:], tri[:]), reads=[tri], writes=[trb])
        P.op("pool", lambda e: e.memset(onesf[:], 1.0), writes=[onesf])
        P.barrier()
        P.release(m2)
        ht = P.alloc("ht", [128, 8, 512], F32)
        sq = [P.alloc(f"sq{i}", [128, 512], BF16) for i in range(2)]
        rs = P.alloc("rs", [128, 512], F32)
        a = P.alloc("a", [128, 8, 512], BF16)
        QT = P.alloc("QT", [128, 4, 512], BF16)
        KT = P.alloc("KT", [128, 4, 512], BF16)
        Ktm = P.alloc("Ktm", [128, 4, 512], F32)
        Vtm = P.alloc("Vtm", [128, 4, 1024], BF16)
        gtm = P.alloc("gtm", [128, 4, 16], F32)
        sgo = P.alloc("sgo", [128, 8, 512], BF16)
        hb = P.alloc("hb", [128, 8, 512], F32)
        hfT = P.alloc("hfT", [128, 8, 512], F32)
        z = P.alloc("z", [128, 8, 512], BF16)
        hr = [P.alloc(f"hr{i}", [128, 512], F32) for i in range(2)]
        e1 = P.alloc("e1", [128, 4], F32)
        lf = P.alloc("lf", [128, 4], F32)
        bcol = P.alloc("bcol", [128, 4], F32)
        wl = P.alloc("wl", [128, 4], F32)
        wcol = P.alloc("wcol", [128, 4], F32)
        eG = P.alloc("eG", [128, 4], F32)
        Rh = [P.alloc(f"Rh{i}", [128, 128], F32) for i in range(2)]
        Ah = [P.alloc(f"Ah{i}", [128, 128], BF16) for i in range(2)]
        ech = [P.alloc(f"ech{i}", [128, 128], F32) for i in range(2)]
        Ph = [P.alloc(f"Ph{i}", [128, 128], BF16) for i in range(2)]
        Qe = [P.alloc(f"Qe{i}", [128, 128], BF16) for i in range(2)]
        Kw = [P.alloc(f"Kw{i}", [128, 128], BF16) for i in range(2)]
        ad = [P.alloc(f"ad{i}", [128, 128], F32) for i in range(2)]
        Cst = P.alloc("Cst", [128, 4, 256], F32)
        Cb = P.alloc("Cb", [128, 4, 256], BF16)
        nst = P.alloc("nst", [128, 4, 128], F32)
        nbb = P.alloc("nbb", [128, 4, 128], BF16)
        vt = self.vt
        ps = self.ps
        cnt = {"d": 0, "h": 0, "p": 0, "r": 0}
        bg = VCOLS["ml_bg"]
        hn = VCOLS["ml_hn"]
        qscale = 128.0 ** -0.5

        def pp():
            t = ps[1 + cnt["p"] % 2]
            cnt["p"] += 1
            return t

        def project(W, with_o):
            nch = W // 128
            for h in range(4):
                for (dstT, c0, sc) in ((QT, 0, qscale), (KT, 512, 1.0)):
                    pt = pp()
                    for c in range(8):
                        P.op("pe", lambda e, c=c, pt=pt, c0=c0, h=h: e.matmul(pt[:, 0:W], wi[:, c, c0 + h * 128:c0 + (h + 1) * 128], a[:, c, 0:W], start=(c == 0), stop=(c == 7)), reads=[wi, a.sub(c)], writes=[pt])
                    P.op("act", lambda e, pt=pt, dstT=dstT, h=h, sc=sc: e.activation(out=dstT[:, h, 0:W], in_=pt[:, 0:W], func=AF.Copy, scale=sc), reads=[pt], writes=[dstT.sub(h)])
            for ch in range(nch):
                cs = slice(ch * 128, (ch + 1) * 128)
                pt = pp()
                for c in range(8):
                    P.op("pe", lambda e, c=c, pt=pt, cs=cs: e.matmul(pt[:, 0:512], a[:, c, cs], wi[:, c, 512:1024], start=(c == 0), stop=(c == 7)), reads=[wi, a.sub(c)], writes=[pt])
                P.op("act", lambda e, pt=pt, ch=ch: e.activation(out=Ktm[:, ch, :], in_=pt[:, 0:512], func=AF.Copy), reads=[pt], writes=[Ktm.sub(ch)])
                for half in range(2):
                    pt = pp()
                    for c in range(8):
                        P.op("pe", lambda e, c=c, pt=pt, cs=cs, half=half: e.matmul(pt[:, 0:512], a[:, c, cs], wi[:, c, 1024 + half * 512:1536 + half * 512], start=(c == 0), stop=(c == 7)), reads=[wi, a.sub(c)], writes=[pt])
                    P.op("act", lambda e, pt=pt, ch=ch, half=half: e.activation(out=Vtm[:, ch, half * 512:(half + 1) * 512], in_=pt[:, 0:512], func=AF.Copy), reads=[pt], writes=[Vtm.sub(ch)])
                pt = pp()
                for c in range(8):
                    P.op("pe", lambda e, c=c, pt=pt, cs=cs: e.matmul(pt[:, 0:16], a[:, c, cs], wi[:, c, 3072:3088], start=(c == 0), stop=(c == 7)), reads=[wi, a.sub(c)], writes=[pt])
                P.op("dve", lambda e, pt=pt, ch=ch: e.tensor_tensor(out=gtm[:, ch, :], in0=pt[:, 0:16], in1=vt[:, bg:bg + 16], op=ALU.add), reads=[pt, vt], writes=[gtm.sub(ch)])
            if with_o:
                for c8 in range(8):
                    pt = pp()
                    for c in range(8):
                        P.op("pe", lambda e, c=c, pt=pt, c8=c8: e.matmul(pt[:, 0:W], wi[:, c, 2048 + c8 * 128:2048 + (c8 + 1) * 128], a[:, c, 0:W], start=(c == 0), stop=(c == 7)), reads=[wi, a.sub(c)], writes=[pt])
                    P.op("act", lambda e, pt=pt, c8=c8: e.activation(out=sgo[:, c8, 0:W], in_=pt[:, 0:W], func=AF.Sigmoid), reads=[pt], writes=[sgo.sub(c8)])

        def core(ch, dirn, with_out, out_tile):
            cs = slice(ch * 128, (ch + 1) * 128)
            g0 = dirn * 8
            psA, pcr, pS, pnum, pden = ps[0], ps[3], ps[4], (ps[5], ps[6]), ps[7]
            P.op("act", lambda e: e.activation(out=e1[:], in_=gtm[:, ch, g0 + 4:g0 + 8], func=AF.Exp, scale=-1.0), reads=[gtm.sub(ch)], writes=[e1])
            P.op("act", lambda e: e.activation(out=e1[:], in_=e1[:], func=AF.Ln, bias=1.0, scale=1.0), reads=[e1], writes=[e1])
            P.op("dve", lambda e: e.tensor_scalar(lf[:], e1[:], -1.0, None, op0=ALU.mult), reads=[e1], writes=[lf])
            P.op("pe", lambda e: e.matmul(psA[:, 0:4], tri[:, dirn, :], lf[:], start=True, stop=True), reads=[tri, lf], writes=[psA])
            P.op("pe", lambda e: e.matmul(psA[:, 4:8], onesf[:], lf[:], start=True, stop=True), reads=[onesf, lf], writes=[psA])
            P.op("dve", lambda e: e.tensor_tensor(out=bcol[:], in0=gtm[:, ch, g0:g0 + 4], in1=psA[:, 0:4], op=ALU.subtract), reads=[gtm.sub(ch), psA], writes=[bcol])
            P.op("dve", lambda e: e.tensor_tensor(out=wl[:], in0=bcol[:], in1=psA[:, 4:8], op=ALU.add), reads=[bcol, psA], writes=[wl])
            P.op("act", lambda e: e.activation(out=wcol[:], in_=wl[:], func=AF.Exp), reads=[wl], writes=[wcol])
            P.op("act", lambda e: e.activation(out=eG[:], in_=psA[:, 4:8], func=AF.Exp), reads=[psA], writes=[eG])
            for h in range(4):
                i2 = cnt["r"] % 2
                cnt["r"] += 1
                R, A, ec, Pm, Qm, Kwm, adm = Rh[i2], Ah[i2], ech[i2], Ph[i2], Qe[i2], Kw[i2], ad[i2]
                hs_ = slice(h * 128, (h + 1) * 128)
                P.op("dve", lambda e, R=R, h=h: e.tensor_scalar(R[:], tri[:, dirn, :], lf[:, h:h + 1], None, op0=ALU.mult), reads=[tri, lf], writes=[R])
                P.op("pe", lambda e, R=R, hs_=hs_: e.matmul(pcr[:, hs_], onesf[:], R[:], start=True, stop=True), reads=[onesf, R], writes=[pcr])
                P.op("act", lambda e, A=A, hs_=hs_, h=h: e.activation(out=A[:], in_=pcr[:, hs_], func=AF.Exp, bias=bcol[:, h:h + 1], scale=1.0), reads=[pcr, bcol], writes=[A])
                P.op("act", lambda e, ec=ec, hs_=hs_: e.activation(out=ec[:], in_=pcr[:, hs_], func=AF.Exp), reads=[pcr], writes=[ec])
                P.op("pe", lambda e, h=h, hs_=hs_: e.matmul(pS[:, hs_], KT[:, h, cs], QT[:, h, cs], start=True, stop=True), reads=[KT.sub(h), QT.sub(h)], writes=[pS])
                P.op("dve", lambda e, A=A: e.tensor_tensor(out=A[:], in0=A[:], in1=trb[:, dirn, :], op=ALU.mult), reads=[A, trb], writes=[A])
                P.op("dve", lambda e, A=A, Pm=Pm, hs_=hs_: e.tensor_tensor(out=Pm[:], in0=pS[:, hs_], in1=A[:], op=ALU.mult), reads=[pS, A], writes=[Pm])
                if with_out:
                    P.op("dve", lambda e, Qm=Qm, ec=ec, h=h: e.tensor_tensor(out=Qm[:], in0=QT[:, h, cs], in1=ec[:], op=ALU.mult), reads=[QT.sub(h), ec], writes=[Qm])
                    pn = pnum[h // 2]
                    for vc in range(2):
                        ncol = slice((h % 2) * 256 + vc * 128, (h % 2) * 256 + (vc + 1) * 128)
                        P.op("pe", lambda e, pn=pn, ncol=ncol, h=h, vc=vc, Pm=Pm: e.matmul(pn[:, ncol], Vtm[:, ch, h * 256 + vc * 128:h * 256 + (vc + 1) * 128], Pm[:], start=True, stop=False), reads=[Vtm.sub(ch), Pm], writes=[pn])
                        P.op("pe", lambda e, pn=pn, ncol=ncol, h=h, vc=vc, Qm=Qm: e.matmul(pn[:, ncol], Cb[:, h, vc * 128:(vc + 1) * 128], Qm[:], start=False, stop=True), reads=[Cb.sub(h), Qm], writes=[pn])
                    P.op("pe", lambda e, hs_=hs_, Pm=Pm: e.matmul(pden[:, hs_], self.ones[:], Pm[:], start=True, stop=False), reads=[self.ones, Pm], writes=[pden])
                    P.op("pe", lambda e, hs_=hs_, Qm=Qm, h=h: e.matmul(pden[:, hs_], nbb[:, h, :], Qm[:], start=False, stop=True), reads=[nbb.sub(h), Qm], writes=[pden])
                    P.op("act", lambda e, adm=adm, hs_=hs_: e.activation(out=adm[:], in_=pden[:, hs_], func=AF.Abs), reads=[pden], writes=[adm])
                    P.op("dve", lambda e, adm=adm: e.tensor_scalar_max(adm[:], adm[:], 1.0), reads=[adm], writes=[adm])
                    P.op("dve", lambda e, adm=adm: e.reciprocal(adm[:], adm[:]), reads=[adm], writes=[adm])
                    for vc in range(2):
                        ncol = slice((h % 2) * 256 + vc * 128, (h % 2) * 256 + (vc + 1) * 128)
                        P.op("dve", lambda e, pn=pn, ncol=ncol, adm=adm, h=h, vc=vc: e.tensor_tensor(out=out_tile[:, 2 * h + vc, cs], in0=pn[:, ncol], in1=adm[:], op=ALU.mult), reads=[pn, adm], writes=[out_tile.sub((2 * h + vc, ch))])
                P.op("dve", lambda e, Kwm=Kwm, h=h: e.tensor_scalar(Kwm[:], Ktm[:, ch, h * 128:(h + 1) * 128], wcol[:, h:h + 1], None, op0=ALU.mult), reads=[Ktm.sub(ch), wcol], writes=[Kwm])
                pc = ps[1 + h // 2]
                ccol = slice((h % 2) * 256, (h % 2 + 1) * 256)
                P.op("pe", lambda e, pc=pc, ccol=ccol, Kwm=Kwm, h=h: e.matmul(pc[:, ccol], Kwm[:], Vtm[:, ch, h * 256:(h + 1) * 256], start=True, stop=True), reads=[Kwm, Vtm.sub(ch)], writes=[pc])
                P.op("pe", lambda e, Kwm=Kwm, hs_=hs_: e.matmul(psA[:, 128 + hs_.start // 4 * 0 + 0:128 + 0 + 128] if False else ps[0][:, 128:256], Kwm[:], self.ones[:], start=True, stop=True), reads=[Kwm, self.ones], writes=[psA])
                P.op("dve", lambda e, pc=pc, ccol=ccol, h=h: e.scalar_tensor_tensor(out=Cst[:, h, :], in0=Cst[:, h, :], scalar=eG[:, h:h + 1], in1=pc[:, ccol], op0=ALU.mult, op1=ALU.add), reads=[Cst.sub(h), eG, pc], writes=[Cst.sub(h)])
                P.op("dve", lambda e, h=h: e.scalar_tensor_tensor(out=nst[:, h, :], in0=nst[:, h, :], scalar=eG[:, h:h + 1], in1=psA[:, 128:256], op0=ALU.mult, op1=ALU.add), reads=[nst.sub(h), eG, psA], writes=[nst.sub(h)])
                P.op("act", lambda e, h=h: e.activation(out=Cb[:, h, :], in_=Cst[:, h, :], func=AF.Copy), reads=[Cst.sub(h)], writes=[Cb.sub(h)])
                P.op("pool", lambda e, h=h: e.tensor_copy(nbb[:, h, :], nst[:, h, :]), reads=[nst.sub(h)], writes=[nbb.sub(h)])

        def reset_state():
            P.op("pool", lambda e: e.memset(Cst[:], 0.0), writes=[Cst])
            P.op("pool", lambda e: e.memset(nst[:], 0.0), writes=[nst])
            P.op("pool", lambda e: e.memset(Cb[:], 0.0), writes=[Cb])
            P.op("pool", lambda e: e.memset(nbb[:], 0.0), writes=[nbb])

        reset_state()
        self.norm_mod(csrc, 0, CTX, self.gsm, l, 0, 1, a, ht, sq, rs, ps[0])
        project(CTX, False)
        for ch in range(2):
            core(ch, 0, False, None)
        for t0 in range(0, T_lat, 512):
            self.norm_mod(src, t0, 512, self.gsm, l, 0, 0, a, ht, sq, rs, ps[0])
            project(512, False)
            for ch in range(4):
                core(ch, 0, True, hb)
            P.dma("sp", self.hf[:, t0:t0 + 512].rearrange("(c p) t -> p c t", p=128), hb[:], reads=[hb], writes=[self.hf.sub(t0)])
        reset_state()
        self.norm_mod(csrc, 0, CTX, self.gsm, l, 0, 1, a, ht, sq, rs, ps[0])
        project(CTX, False)
        for ch in (1, 0):
            core(ch, 1, False, None)
        for t0 in range(T_lat - 512, -1, -512):
            self.norm_mod(src, t0, 512, self.gsm, l, 0, 0, a, ht, sq, rs, ps[0])
            project(512, True)
            P.dma("sp", hfT[:], self.hf[:, t0:t0 + 512].rearrange("(c p) t -> p c t", p=128), reads=[self.hf.sub(t0)], writes=[hfT])
            for ch in (3, 2, 1, 0):
                core(ch, 1, True, hb)
            for h in range(4):
                for vc in range(2):
                    c8 = 2 * h + vc
                    P.op("dve", lambda e, c8=c8: e.tensor_tensor(out=hb[:, c8, :], in0=hb[:, c8, :], in1=hfT[:, c8, :], op=ALU.add), reads=[hb, hfT], writes=[hb])
                    q = sq[vc]
                    P.op("act", lambda e, c8=c8, q=q: e.activation(out=q[:], in_=hb[:, c8, :], func=AF.Square), reads=[hb], writes=[q])
                    P.op("pe", lambda e, q=q, vc=vc: e.matmul(ps[0][:, :], self.ones[:], q[:], start=(vc == 0), stop=(vc == 1)), reads=[self.ones, q], writes=[ps[0]])
                P.op("act", lambda e: e.activation(out=rs[:], in_=ps[0][:, :], func=AF.Sqrt, bias=self.epsb[:], scale=1.0 / 256), reads=[ps[0], self.epsb], writes=[rs])
                P.op("dve", lambda e: e.reciprocal(rs[:], rs[:]), reads=[rs], writes=[rs])
                for vc in range(2):
                    c8 = 2 * h + vc
                    P.op("dve", lambda e, c8=c8, vc=vc: e.scalar_tensor_tensor(out=hb[:, c8, :], in0=hb[:, c8, :], scalar=vt[:, hn + vc:hn + vc + 1], in1=rs[:], op0=ALU.mult, op1=ALU.mult), reads=[hb, vt, rs], writes=[hb])
                    P.op("dve", lambda e, c8=c8: e.tensor_tensor(out=z[:, c8, :], in0=hb[:, c8, :], in1=sgo[:, c8, :], op=ALU.mult), reads=[hb, sgo.sub(c8)], writes=[z.sub(c8)])
            self.proj_res(src, dst, t0, 512, l, 0, wo, z, 8, 2, hr, [ps[1], ps[2]], cnt, False)
        P.barrier()
        P.release(m)

    def copy_dram(self, src, dst, T_):
        P = self.P
        P.dma("sp", dst[:, 0:T_], src[:, 0:T_])
        P.barrier()


def build(test=None):
    k = K(test)
    P = k.P
    k.setup()
    k.adaln()
    if test is None or test in ("full", "fulls"):
        TL = 1024 if test == "fulls" else S
        k.mixer_win(0, k.xT, k.hA, k.ctxT, k.cA, T_lat=TL)
        k.ffn(0, k.hA, k.hB, k.cA, k.cB, True, T_lat=TL)
        k.mixer_sc(1, k.hB, k.hA, k.cB, k.cA, T_lat=TL)
        k.ffn(1, k.hA, k.hB, k.cA, k.cB, True, T_lat=TL)
        k.mixer_dense(2, k.hB, k.hA, k.cB, k.cA, T_lat=TL)
        k.ffn(2, k.hA, k.hB, k.cA, k.cB, True, T_lat=TL)
        k.mixer_mlstm(3, k.hB, k.hA, k.cB, T_lat=TL)
        k.ffn(3, k.hA, k.outT, None, None, False, T_lat=TL)
    if test in ("ffn0", "small"):
        k.ffn(0, k.xT, k.outT, k.ctxT, k.cB, True, T_lat=(S if test == "ffn0" else 1024))
    if test in ("sc1", "sc1s"):
        k.mixer_sc(1, k.xT, k.outT, k.ctxT, k.cB, T_lat=(S if test == "sc1" else 1024))
    if test in ("ax2", "ax2s"):
        k.mixer_dense(2, k.xT, k.outT, k.ctxT, k.cB, T_lat=(S if test == "ax2" else 1024))
    if test in ("win0", "win0s"):
        k.mixer_win(0, k.xT, k.outT, k.ctxT, k.cB, T_lat=(S if test == "win0" else 1024))
    if test in ("ml3", "ml3s"):
        k.mixer_mlstm(3, k.xT, k.outT, k.ctxT, T_lat=(S if test == "ml3" else 1024))
    n = P.emit()
    return k.nc, n


def make_in_maps(inputs, cores):
    vecs = pack_vecs(inputs)
    c128, s128 = rope_tables(128)
    c64, s64 = rope_tables(64)
    ii = np.arange(128)[:, None]
    jj = np.arange(128)[None, :]
    wmask = np.concatenate([np.tile((ii >= jj).astype(np.float32), (1, 4)), np.tile((ii <= jj).astype(np.float32), (1, 4))], axis=1)
    trimat = np.concatenate([(ii <= jj).astype(np.float32), (ii >= jj).astype(np.float32)], axis=1)
    maps = []
    for b in cores:
        cc = np.zeros((128, 8, 2), np.float32)
        cc[:, :, 0] = inputs["c"][b].reshape(8, 128).T
        cc[:, :, 1] = inputs["c_ctx"].reshape(8, 128).T
        maps.append({
            "xT": np.ascontiguousarray(inputs["x"][b].T),
            "ctxT": np.ascontiguousarray(inputs["ctx"][b].T),
            "cc": cc, "vecs": vecs,
            "ada_w": inputs["ada_w"], "ffn_w_up": inputs["ffn_w_up"], "ffn_w_down": inputs["ffn_w_down"],
            "sc_w_in": inputs["sc_w_in"][0], "sc_w_out": inputs["sc_w_out"][0],
            "ax_w_qkv": inputs["ax_w_qkv"][0], "ax_w_o": inputs["ax_w_o"][0],
            "win_w_qkv": inputs["win_w_qkv"][0], "win_w_o": inputs["win_w_o"][0],
            "cos128": c128, "sin128": s128, "cos64": c64, "sin64": s64, "wmask": wmask,
            "ml_w_in": inputs["ml_w_in"][0], "ml_w_out": inputs["ml_w_out"][0], "trimat": trimat,
        })
    return maps


def kernel(**inputs):
    inputs = {k: np.asarray(v) for k, v in inputs.items()}
    nc, _ = build()
    cores = [0, 1, 2, 3]
    res = run_bass_kernel_spmd(nc, make_in_maps(inputs, cores), core_ids=cores)
    out = np.stack([np.ascontiguousarray(res.results[i]["outT"].T) for i in range(4)], axis=0)
    return out.astype(np.float32)
```

```python
import numpy as np
import concourse.bass as bass
import concourse.mybir as mybir
from concourse.bass_utils import run_bass_kernel_spmd

F32 = mybir.dt.float32
BF16 = mybir.dt.bfloat16
AF = mybir.ActivationFunctionType
ALU = mybir.AluOpType

D = 1024
S = 8192
CTX = 256
DEPTH = 4
DFF = 2816
NFC = 44
EPS = 1e-6
SEM_CAP = 8000
N_DMA_SEMS = 24
ENGS = ("pe", "act", "dve", "pool", "sp")


class Buf:
    __slots__ = ("name", "last_w", "readers", "parent", "subs")

    def __init__(self, name, parent=None):
        self.name = name
        self.last_w = None
        self.readers = []
        self.parent = parent
        self.subs = {}

    def sub(self, key):
        b = self.subs.get(key)
        if b is None:
            b = Buf(f"{self.name}[{key}]", parent=self)
            self.subs[key] = b
        return b


class T:
    def __init__(self, t, name):
        self.t = t
        self.buf = Buf(name)

    def __getitem__(self, idx):
        return self.t[idx]

    def sub(self, key):
        return self.buf.sub(key)


def _bufs(xs):
    out = []
    for x in xs:
        if x is None:
            continue
        out.append(x.buf if isinstance(x, T) else x)
    return out


class Op:
    __slots__ = ("eng", "fn", "is_dma", "deps", "tick", "signal", "dsem", "dval", "waits", "pre_dma_wait")

    def __init__(self, eng, fn, is_dma):
        self.eng = eng
        self.fn = fn
        self.is_dma = is_dma
        self.deps = {}
        self.signal = False
        self.tick = None
        self.dsem = None
        self.dval = None
        self.waits = []
        self.pre_dma_wait = None


class Prog:
    def __init__(self, nc, arena_words=52736):
        self.nc = nc
        self.ops = []
        self._n = 0
        self.arena = nc.alloc_sbuf_tensor("arena", [128, arena_words], F32)
        self.arena_bf = self.arena.bitcast(BF16)
        self.arena_bytes = arena_words * 4
        self.off = 0
        self._last_compute = {}
        self._dmas_since_bar = []
        self._bar_frontier = []
        self._bar_pending = set()

    def alloc(self, name, shape, dt):
        es = 4 if dt == F32 else 2
        n = 1
        for d in shape[1:]:
            n *= d
        off = (self.off + 63) // 64 * 64
        self.off = off + n * es
        assert self.off <= self.arena_bytes, f"SBUF arena overflow at {name}: {self.off}"
        base = self.arena if dt == F32 else self.arena_bf
        e0 = off // es
        ap = base[0:shape[0], e0:e0 + n]
        if len(shape) == 3:
            ap = ap.rearrange("p (a b) -> p a b", a=shape[1])
        elif len(shape) == 4:
            ap = ap.rearrange("p (a b c) -> p a b c", a=shape[1], b=shape[2])
        return T(ap, name)

    def mark(self):
        return self.off

    def release(self, m):
        self.off = m

    def barrier(self):
        self._bar_frontier = list(self._last_compute.values()) + list(self._dmas_since_bar)
        self._dmas_since_bar = []
        self._bar_pending = set(ENGS)

    def ps(self, name, shape, dt=F32):
        self._n += 1
        return T(self.nc.alloc_psum_tensor(f"{name}_{self._n}", list(shape), dt), name)

    def dram(self, name, shape, dt, kind="Internal"):
        return T(self.nc.dram_tensor(name, list(shape), dt, kind=kind), name)

    def op(self, eng, fn, reads=(), writes=(), is_dma=False):
        o = Op(eng, fn, is_dma)
        rb = _bufs(reads)
        wb = _bufs(writes)

        def hist(b):
            hs = [b]
            if b.parent is not None:
                hs.append(b.parent)
            else:
                hs.extend(b.subs.values())
            return hs

        for b in rb:
            for h in hist(b):
                if h.last_w is not None:
                    o.deps[h.last_w] = True
        for b in wb:
            for h in hist(b):
                if h.last_w is not None and h.last_w not in o.deps:
                    o.deps[h.last_w] = False
                for r in h.readers:
                    if r not in o.deps:
                        o.deps[r] = False
        if eng in self._bar_pending:
            self._bar_pending.discard(eng)
            for d in self._bar_frontier:
                o.deps[d] = True
        if is_dma:
            self._dmas_since_bar.append(o)
        else:
            self._last_compute[eng] = o
        for b in rb:
            b.readers.append(o)
        for b in wb:
            b.last_w = o
            b.readers = []
            if b.parent is None:
                for s in b.subs.values():
                    s.last_w = o
                    s.readers = []
        self.ops.append(o)
        return o

    def dma(self, eng, out_ap, in_ap, reads=(), writes=(), **kw):
        return self.op(eng, lambda e: e.dma_start(out=out_ap, in_=in_ap, **kw), reads, writes, is_dma=True)

    def emit(self):
        nc = self.nc
        streams = {e: [] for e in ENGS}
        for o in self.ops:
            streams[o.eng].append(o)

        def skip(o, d, raw):
            return (not d.is_dma) and d.eng == o.eng and (not o.is_dma) and o.eng == "pe"

        for o in self.ops:
            for d, raw in o.deps.items():
                if d.is_dma or skip(o, d, raw):
                    continue
                d.signal = True
        ctr_sems = {}
        for e in ENGS:
            k = 0
            for o in streams[e]:
                if o.signal and not o.is_dma:
                    k += 1
                    o.tick = k
            ctr_sems[e] = [nc.alloc_semaphore(f"ctr_{e}_{i}") for i in range((k + SEM_CAP - 1) // SEM_CAP)]
        finals = {}
        for e in ENGS:
            dmas = [o for o in streams[e] if o.is_dma]
            if not dmas:
                continue
            pool = [nc.alloc_semaphore(f"dma_{e}_{i}") for i in range(min(N_DMA_SEMS, len(dmas)))]
            vals = [0] * len(pool)
            for i, o in enumerate(dmas):
                j = i % len(pool)
                if vals[j] > 0:
                    o.pre_dma_wait = (pool[j], vals[j])
                vals[j] += 16
                o.dsem, o.dval = pool[j], vals[j]
            finals[e] = [(pool[j], vals[j]) for j in range(len(pool))]
        for e in ENGS:
            waited = {}
            for o in streams[e]:
                ws = {}
                cands = []
                for d, raw in o.deps.items():
                    if d.is_dma:
                        cands.append((d.dsem, d.dval, ("dma", id(d.dsem))))
                    elif not skip(o, d, raw):
                        t = d.tick - 1
                        cands.append((ctr_sems[d.eng][t // SEM_CAP], (t % SEM_CAP) + 1, (d.eng, t // SEM_CAP)))
                if o.pre_dma_wait is not None:
                    cands.append((o.pre_dma_wait[0], o.pre_dma_wait[1], ("dma", id(o.pre_dma_wait[0]))))
                for sem, val, key in cands:
                    if waited.get(key, 0) >= val:
                        continue
                    if key not in ws or ws[key][1] < val:
                        ws[key] = (sem, val)
                for key, (sem, val) in ws.items():
                    waited[key] = val
                o.waits = list(ws.values())
        engmap = {"pe": "tensor", "act": "scalar", "dve": "vector", "pool": "gpsimd", "sp": "sync"}
        with nc.Block() as block:
            for e in ENGS:
                ops = streams[e]
                if not ops:
                    continue

                def body(eng, ops=ops, e=e):
                    for o in ops:
                        for sem, val in o.waits:
                            eng.wait_ge(sem, val)
                        ins = o.fn(eng)
                        if o.is_dma:
                            ins.then_inc(o.dsem, 16)
                        elif o.signal:
                            ins.then_inc(ctr_sems[e][(o.tick - 1) // SEM_CAP], 1)
                    for sem, val in finals.get(e, []):
                        eng.wait_ge(sem, val)

                getattr(block, engmap[e])(body)
        return len(self.ops)


def _vec_layout():
    cols = {}
    n = 0

    def add(name, k):
        nonlocal n
        cols[name] = n
        n += k

    for l in range(DEPTH):
        add(f"ada_b{l}", 48)
        add(f"nmix{l}", 8)
        add(f"nffn{l}", 8)
        add(f"fconv{l}", 3 * NFC)
    add("sc_conv", 24)
    for nm in ("ax_qg", "ax_qgs", "ax_kg", "ax_kgs", "win_qg", "win_qgs", "win_kg", "win_kgs"):
        add(nm, 1)
    add("win_sink", 16)
    add("ml_bg", 16)
    add("ml_hn", 2)
    return cols, n


VCOLS, NV = _vec_layout()


def _colmajor(v):
    v = np.asarray(v, np.float32).reshape(-1, 128)
    return v.T


def pack_vecs(inp):
    out = np.zeros((128, NV), np.float32)

    def put(name, arr):
        a = _colmajor(arr)
        out[:, VCOLS[name]:VCOLS[name] + a.shape[1]] = a

    for l in range(DEPTH):
        put(f"ada_b{l}", inp["ada_b"][l])
        put(f"nmix{l}", inp["norm_mix"][l])
        put(f"nffn{l}", inp["norm_ffn"][l])
        put(f"fconv{l}", inp["ffn_conv"][l].reshape(-1))
    put("sc_conv", inp["sc_conv"][0].reshape(-1))

    def swap(v):
        h = v.shape[0] // 2
        return np.concatenate([v[h:], v[:h]])

    put("ax_qg", inp["ax_q_norm"][0])
    put("ax_qgs", swap(inp["ax_q_norm"][0]))
    put("ax_kg", inp["ax_k_norm"][0])
    put("ax_kgs", swap(inp["ax_k_norm"][0]))
    put("win_qg", np.tile(inp["win_q_norm"][0], 2))
    put("win_qgs", np.tile(swap(inp["win_q_norm"][0]), 2))
    put("win_kg", np.tile(inp["win_k_norm"][0], 2))
    put("win_kgs", np.tile(swap(inp["win_k_norm"][0]), 2))
    out[:, VCOLS["win_sink"]:VCOLS["win_sink"] + 16] = np.broadcast_to(inp["win_sink"][0][None, :], (128, 16))
    out[:, VCOLS["ml_bg"]:VCOLS["ml_bg"] + 16] = np.broadcast_to(inp["ml_b_gate"][0][None, :], (128, 16))
    put("ml_hn", inp["ml_h_norm"][0])
    return out


def rope_tables(hd):
    n_freq = hd // 4
    rows = S // 64
    row = np.repeat(np.arange(rows, dtype=np.float32), 64)
    col = np.tile(np.arange(64, dtype=np.float32), rows)
    inv = np.power(np.float32(10000.0), -np.arange(n_freq, dtype=np.float32) / np.float32(n_freq)).astype(np.float32)
    ang = np.concatenate([row[:, None] * inv, col[:, None] * inv], axis=-1).astype(np.float32)
    cos = np.cos(ang).astype(np.float32).T
    sin = np.sin(ang).astype(np.float32).T
    cos2 = np.concatenate([cos, cos], axis=0)
    sin2 = np.concatenate([-sin, sin], axis=0)
    rep = 128 // hd
    cos2 = np.tile(cos2, (rep, 1))
    sin2 = np.tile(sin2, (rep, 1))
    c = np.ones((128, S + CTX), np.float32)
    sn = np.zeros((128, S + CTX), np.float32)
    c[:, :S] = cos2
    sn[:, :S] = sin2
    return c, sn


class K:
    def __init__(self, test=None):
        self.test = test
        nc = bass.Bass("TRN2", target_bir_lowering=False)
        self.nc = nc
        P = Prog(nc)
        self.P = P
        dr = lambda n, s, k="ExternalInput": P.dram(n, s, F32, kind=k)
        self.xT = dr("xT", [D, S])
        self.ctxT = dr("ctxT", [D, CTX])
        self.cc = dr("cc", [128, 8, 2])
        self.vecs = dr("vecs", [128, NV])
        self.ada_w = dr("ada_w", [DEPTH, D, 6 * D])
        self.w_up = dr("ffn_w_up", [DEPTH, D, 2 * DFF])
        self.w_down = dr("ffn_w_down", [DEPTH, DFF, D])
        self.sc_w_in = dr("sc_w_in", [D, 3 * D])
        self.sc_w_out = dr("sc_w_out", [D, D])
        self.ax_w_qkv = dr("ax_w_qkv", [D, 1536])
        self.ax_w_o = dr("ax_w_o", [D, D])
        self.win_w_qkv = dr("win_w_qkv", [D, 1536])
        self.win_w_o = dr("win_w_o", [D, D])
        self.cos128 = dr("cos128", [128, S + CTX])
        self.sin128 = dr("sin128", [128, S + CTX])
        self.cos64 = dr("cos64", [128, S + CTX])
        self.sin64 = dr("sin64", [128, S + CTX])
        self.wmask = dr("wmask", [128, 1024])
        self.ml_w_in = dr("ml_w_in", [D, 3088])
        self.ml_w_out = dr("ml_w_out", [D, D])
        self.trimat = dr("trimat", [128, 256])
        self.hf = dr("hf_scratch", [D, S], "Internal")
        self.outT = dr("outT", [D, S], "ExternalOutput")
        self.hA = dr("hA", [D, S], "Internal")
        self.hB = dr("hB", [D, S], "Internal")
        self.cA = dr("cA", [D, CTX], "Internal")
        self.cB = dr("cB", [D, CTX], "Internal")
        self.ones = P.alloc("ones", [128, 128], BF16)
        self.vt = P.alloc("vt", [128, NV], F32)
        self.mod = P.alloc("mod", [128, DEPTH, 48, 2], F32)
        self.gsm = P.alloc("gsm", [128, DEPTH, 8, 2], F32)
        self.gsf = P.alloc("gsf", [128, DEPTH, 8, 2], F32)
        self.epsb = P.alloc("epsb", [128, 1], F32)
        self.ones_bd = P.alloc("ones_bd", [128, 128], BF16)
        self.zero_c = P.alloc("zero_c", [128, 1], F32)
        self.psw = [P.ps(f"psw{i}", [128, 1024]) for i in range(4)]
        self.ps = [T(self.psw[i // 2].t[:, (i % 2) * 512:(i % 2 + 1) * 512], f"ps{i}") for i in range(8)]
        self.base_mark = P.mark()

    def setup(self):
        P = self.P
        P.op("pool", lambda e: e.memset(self.ones[:], 1.0), writes=[self.ones])
        P.op("pool", lambda e: e.memset(self.ones_bd[:], 0.0), writes=[self.ones_bd])
        P.op("pool", lambda e: e.memset(self.ones_bd[0:64, 0:64], 1.0), writes=[self.ones_bd])
        P.op("pool", lambda e: e.memset(self.ones_bd[64:128, 64:128], 1.0), writes=[self.ones_bd])
        P.op("pool", lambda e: e.memset(self.epsb[:], EPS), writes=[self.epsb])
        P.op("pool", lambda e: e.memset(self.zero_c[:], 0.0), writes=[self.zero_c])
        P.dma("sp", self.vt[:], self.vecs[:], writes=[self.vt])

    def adaln(self):
        P = self.P
        m = P.mark()
        cct = P.alloc("cct", [128, 8, 2], F32)
        sig = P.alloc("sig", [128, 8, 2], F32)
        sc = P.alloc("sc", [128, 8, 2], F32)
        P.dma("sp", cct[:], self.cc[:], writes=[cct])
        P.op("act", lambda e: e.activation(out=sig[:], in_=cct[:], func=AF.Sigmoid), reads=[cct], writes=[sig])
        P.op("dve", lambda e: e.tensor_tensor(out=sc[:], in0=cct[:], in1=sig[:], op=ALU.mult), reads=[cct, sig], writes=[sc])
        NP = 8
        PW = 768
        wbuf = [P.alloc(f"adaw{i}", [128, 8, PW], F32) for i in range(2)]
        pst = self.ps[0]
        k = 0
        for l in range(DEPTH):
            for pc in range(NP):
                wb = wbuf[k % 2]
                k += 1
                P.dma("sp", wb[:], self.ada_w[l, :, pc * PW:(pc + 1) * PW].rearrange("(c p) f -> p c f", p=128), writes=[wb])
                for jj in range(PW // 128):
                    j = pc * (PW // 128) + jj
                    for c in range(8):
                        P.op("pe", lambda e, wb=wb, jj=jj, c=c, j=j: e.matmul(pst[:, 2 * j:2 * j + 2], wb[:, c, jj * 128:(jj + 1) * 128], sc[:, c, :], start=(c == 0), stop=(c == 7)),
                             reads=[wb, sc], writes=[pst])
            cb = VCOLS[f"ada_b{l}"]
            for s in range(2):
                P.op("dve", lambda e, l=l, s=s, cb=cb: e.tensor_tensor(out=self.mod[:, l, :, s], in0=pst[:, 0:96].rearrange("p (j s) -> p j s", s=2)[:, :, s], in1=self.vt[:, cb:cb + 48], op=ALU.add),
                     reads=[pst, self.vt], writes=[self.mod])
            for s in range(2):
                for (dst, vi, nm) in ((self.gsm, 1, f"nmix{l}"), (self.gsf, 4, f"nffn{l}")):
                    cn = VCOLS[nm]
                    P.op("dve", lambda e, l=l, s=s, dst=dst, vi=vi, cn=cn: e.scalar_tensor_tensor(out=dst[:, l, :, s], in0=self.mod[:, l, vi * 8:vi * 8 + 8, s], scalar=1.0, in1=self.vt[:, cn:cn + 8], op0=ALU.add, op1=ALU.mult),
                         reads=[self.mod, self.vt], writes=[dst])
        P.barrier()
        P.release(m)

    def modcol(self, l, vi, c, s):
        return self.mod[:, l, vi * 8 + c, s:s + 1]

    def norm_mod(self, src, t0, W, gs, l, shift_vi, s, a_out, ht, sq, rs, pst):
        P = self.P
        P.dma("sp", ht[:, :, 0:W], src[:, t0:t0 + W].rearrange("(c p) t -> p c t", p=128), writes=[ht])
        for c in range(8):
            q = sq[c % len(sq)]
            P.op("act", lambda e, c=c, q=q: e.activation(out=q[:, 0:W], in_=ht[:, c, 0:W], func=AF.Square), reads=[ht], writes=[q])
            P.op("pe", lambda e, c=c, q=q: e.matmul(pst[:, 0:W], self.ones[:], q[:, 0:W], start=(c == 0), stop=(c == 7)), reads=[self.ones, q], writes=[pst])
        P.op("act", lambda e: e.activation(out=rs[:, 0:W], in_=pst[:, 0:W], func=AF.Sqrt, bias=self.epsb[:], scale=1.0 / D), reads=[pst, self.epsb], writes=[rs])
        P.op("dve", lambda e: e.reciprocal(rs[:, 0:W], rs[:, 0:W]), reads=[rs], writes=[rs])
        for c in range(8):
            P.op("dve", lambda e, c=c: e.tensor_tensor(out=ht[:, c, 0:W], in0=ht[:, c, 0:W], in1=rs[:, 0:W], op=ALU.mult),
                 reads=[ht.sub(c), rs], writes=[ht.sub(c)])
            P.op("act", lambda e, c=c: e.activation(out=a_out[:, c, 0:W], in_=ht[:, c, 0:W], func=AF.Identity, bias=self.modcol(l, shift_vi, c, s), scale=gs[:, l, c, s:s + 1]),
                 reads=[ht.sub(c), self.mod, gs], writes=[a_out.sub(c)])

    def load_cast(self, dst, dst_kc, w_ap_rows, ncols, stage, k0=0):
        P = self.P
        PIECE = stage[0].t.shape[-1] if False else 2048
        c0 = 0
        k = k0
        while c0 < ncols:
            n = min(PIECE, ncols - c0)
            st = stage[k % len(stage)]
            P.dma("sp", st[:, 0:n], w_ap_rows[:, c0:c0 + n], writes=[st])
            eng = "pool" if k % 2 == 0 else "dve"
            P.op(eng, lambda e, st=st, n=n, c0=c0: e.tensor_copy(dst[:, dst_kc, c0:c0 + n], st[:, 0:n]), reads=[st], writes=[dst.sub(("w", dst_kc, c0))])
            c0 += n
            k += 1
        return k

    def ffn(self, l, src, dst, csrc, cdst, do_ctx, T_lat=S):
        P = self.P
        m = P.mark()
        wu = P.alloc("wu", [128, 8, 2 * DFF], BF16)
        wd = P.alloc("wd", [128, 22, D], BF16)
        m2 = P.mark()
        stage = [P.alloc(f"stg{i}", [128, 2048], F32) for i in range(2)]
        k = 0
        for kc in range(8):
            k = self.load_cast(wu, kc, self.w_up[l, kc * 128:(kc + 1) * 128, :], 2 * DFF, stage, k)
        for kc in range(22):
            k = self.load_cast(wd, kc, self.w_down[l, kc * 128:(kc + 1) * 128, :], D, stage, k)
        P.barrier()
        P.release(m2)
        ht = P.alloc("ht", [128, 8, 512], F32)
        sq = [P.alloc(f"sq{i}", [128, 512], BF16) for i in range(2)]
        rs = P.alloc("rs", [128, 512], F32)
        a = P.alloc("a", [128, 8, 512], BF16)
        NU = 3
        uc = [P.alloc(f"uc{i}", [128, 514], BF16) for i in range(NU)]
        yc = [P.alloc(f"yc{i}", [128, 512], BF16) for i in range(NU + 1)]
        sg = [P.alloc(f"sg{i}", [128, 512], BF16) for i in range(2)]
        carry = P.alloc("carry", [128, NFC, 2], BF16)
        gu = P.alloc("gu", [128, 22, 512], BF16)
        hr = [P.alloc(f"hr{i}", [128, 512], F32) for i in range(2)]
        cw = VCOLS[f"fconv{l}"]
        vt = self.vt
        ps_ss = self.ps[0]
        ps_up = self.ps[1:4]
        ps_dn = self.ps[4:8]
        cnt = {"u": 0, "y": 0, "d": 0, "g": 0, "h": 0}

        def up_conv(t0, W, flush):
            for i in range(22):
                ycs = []
                for half in range(2):
                    fc = half * 22 + i
                    u = uc[cnt["u"] % NU]
                    pu = ps_up[cnt["u"] % 3]
                    cnt["u"] += 1
                    y = yc[cnt["y"] % (NU + 1)]
                    cnt["y"] += 1
                    P.op("pool", lambda e, u=u, fc=fc: e.tensor_copy(u[:, 0:2], carry[:, fc, :]), reads=[carry.sub(fc)], writes=[u])
                    if flush:
                        P.op("pool", lambda e, u=u: e.memset(u[:, 2:3], 0.0), writes=[u])
                    else:
                        for c in range(8):
                            P.op("pe", lambda e, c=c, fc=fc, pu=pu, W=W: e.matmul(pu[:, 0:W], wu[:, c, fc * 128:(fc + 1) * 128], a[:, c, 0:W], start=(c == 0), stop=(c == 7)),
                                 reads=[wu, a.sub(c)], writes=[pu])
                        P.op("act", lambda e, u=u, pu=pu, W=W: e.activation(out=u[:, 2:2 + W], in_=pu[:, 0:W], func=AF.Copy), reads=[pu], writes=[u])
                        P.op("pool", lambda e, u=u, fc=fc, W=W: e.tensor_copy(carry[:, fc, :], u[:, W:W + 2]), reads=[u], writes=[carry.sub(fc)])
                    P.op("dve", lambda e, u=u, y=y, fc=fc, W=W: e.tensor_scalar(y[:, 0:W], u[:, 0:W], vt[:, cw + fc:cw + fc + 1], None, op0=ALU.mult), reads=[u, vt], writes=[y])
                    P.op("dve", lambda e, u=u, y=y, fc=fc, W=W: e.scalar_tensor_tensor(out=y[:, 0:W], in0=u[:, 1:1 + W], scalar=vt[:, cw + NFC + fc:cw + NFC + fc + 1], in1=y[:, 0:W], op0=ALU.mult, op1=ALU.add), reads=[u, vt, y], writes=[y])
                    P.op("dve", lambda e, u=u, y=y, fc=fc, W=W: e.scalar_tensor_tensor(out=y[:, 0:W], in0=u[:, 2:2 + W], scalar=vt[:, cw + 2 * NFC + fc:cw + 2 * NFC + fc + 1], in1=y[:, 0:W], op0=ALU.mult, op1=ALU.add), reads=[u, vt, y], writes=[y])
                    ycs.append(y)
                sgt = sg[cnt["g"] % 2]
                cnt["g"] += 1
                P.op("act", lambda e, sgt=sgt, y=ycs[0], W=W: e.activation(out=sgt[:, 0:W], in_=y[:, 0:W], func=AF.Silu), reads=[ycs[0]], writes=[sgt])
                P.op("dve", lambda e, sgt=sgt, y=ycs[1], i=i, W=W: e.tensor_tensor(out=gu[:, i, 0:W], in0=sgt[:, 0:W], in1=y[:, 0:W], op=ALU.mult), reads=[sgt, ycs[1]], writes=[gu.sub(i)])

        def down(hsrc, hdst, t0, W, s):
            o0 = 1 if t0 == 0 else 0
            for oc in range(8):
                pd = ps_dn[cnt["d"] % 4]
                cnt["d"] += 1
                hrt = hr[cnt["h"] % 2]
                cnt["h"] += 1
                kw = {"allow_slow_non_contiguous": True} if W - o0 == 1 else {}
                P.dma("sp", hrt[:, o0:W], hsrc[oc * 128:(oc + 1) * 128, t0 - 1 + o0:t0 - 1 + W], writes=[hrt], **kw)
                for i in range(22):
                    P.op("pe", lambda e, i=i, oc=oc, pd=pd, W=W: e.matmul(pd[:, 0:W], wd[:, i, oc * 128:(oc + 1) * 128], gu[:, i, 0:W], start=(i == 0), stop=(i == 21)),
                         reads=[wd, gu.sub(i)], writes=[pd])
                P.op("dve", lambda e, pd=pd, hrt=hrt, oc=oc, W=W, o0=o0: e.scalar_tensor_tensor(out=hrt[:, o0:W], in0=pd[:, o0:W], scalar=self.modcol(l, 5, oc, s), in1=hrt[:, o0:W], op0=ALU.mult, op1=ALU.add),
                     reads=[pd, hrt, self.mod], writes=[hrt])
                P.dma("sp", hdst[oc * 128:(oc + 1) * 128, t0 - 1 + o0:t0 - 1 + W], hrt[:, o0:W], reads=[hrt], **kw)

        def seq(hsrc, hdst, T_, s):
            P.op("pool", lambda e: e.memset(carry[:], 0.0), writes=[carry])
            tiles = [(t0, min(512, T_ - t0), False) for t0 in range(0, T_, 512)] + [(T_, 1, True)]
            self.norm_mod(hsrc, 0, tiles[0][1], self.gsf, l, 3, s, a, ht, sq, rs, ps_ss)
            for j, (t0, W, flush) in enumerate(tiles):
                up_conv(t0, W, flush)
                if j + 1 < len(tiles) and not tiles[j + 1][2]:
                    self.norm_mod(hsrc, tiles[j + 1][0], tiles[j + 1][1], self.gsf, l, 3, s, a, ht, sq, rs, ps_ss)
                down(hsrc, hdst, t0, W, s)

        seq(src, dst, T_lat, 0)
        if do_ctx:
            seq(csrc, cdst, CTX, 1)
        P.barrier()
        P.release(m)

    def proj_res(self, hsrc, hdst, t0, W, l, s, wmat, zin, nk, gate_vi, hr, ps_dn, cnt, shifted):
        P = self.P
        tb = t0 - 1 if shifted else t0
        o0 = 1 if (shifted and t0 == 0) else 0
        for oc in range(8):
            pd = ps_dn[cnt["d"] % len(ps_dn)]
            cnt["d"] += 1
            hrt = hr[cnt["h"] % len(hr)]
            cnt["h"] += 1
            kw = {"allow_slow_non_contiguous": True} if W - o0 == 1 else {}
            P.dma("sp", hrt[:, o0:W], hsrc[oc * 128:(oc + 1) * 128, tb + o0:tb + W], writes=[hrt], **kw)
            for i in range(nk):
                P.op("pe", lambda e, i=i, oc=oc, pd=pd: e.matmul(pd[:, 0:W], wmat[:, i, oc * 128:(oc + 1) * 128], zin[:, i, 0:W], start=(i == 0), stop=(i == nk - 1)),
                     reads=[wmat, zin.sub(i)], writes=[pd])
            P.op("dve", lambda e, pd=pd, hrt=hrt, oc=oc: e.scalar_tensor_tensor(out=hrt[:, o0:W], in0=pd[:, o0:W], scalar=self.modcol(l, gate_vi, oc, s), in1=hrt[:, o0:W], op0=ALU.mult, op1=ALU.add),
                 reads=[pd, hrt, self.mod], writes=[hrt])
            P.dma("sp", hdst[oc * 128:(oc + 1) * 128, tb + o0:tb + W], hrt[:, o0:W], reads=[hrt], **kw)

    def mixer_sc(self, l, src, dst, csrc, cdst, T_lat=S):
        P = self.P
        m = P.mark()
        wi = P.alloc("wi", [128, 8, 3 * D], BF16)
        wo = P.alloc("wo", [128, 8, D], BF16)
        m2 = P.mark()
        stage = [P.alloc(f"stg{i}", [128, 2048], F32) for i in range(2)]
        k = 0
        for kc in range(8):
            k = self.load_cast(wi, kc, self.sc_w_in[kc * 128:(kc + 1) * 128, :], 3 * D, stage, k)
        for kc in range(8):
            k = self.load_cast(wo, kc, self.sc_w_out[kc * 128:(kc + 1) * 128, :], D, stage, k)
        P.barrier()
        P.release(m2)
        ht = P.alloc("ht", [128, 8, 512], F32)
        sq = [P.alloc(f"sq{i}", [128, 512], BF16) for i in range(2)]
        rs = P.alloc("rs", [128, 512], F32)
        a = P.alloc("a", [128, 8, 512], BF16)
        cu = [P.alloc(f"cu{i}", [128, 514], BF16) for i in range(2)]
        us = [P.alloc(f"us{i}", [128, 512], F32) for i in range(2)]
        bb = [P.alloc(f"bb{i}", [128, 513], BF16) for i in range(2)]
        yc = [P.alloc(f"yc{i}", [128, 512], BF16) for i in range(2)]
        carry_cu = P.alloc("carry_cu", [128, 8, 2], BF16)
        carry_b = P.alloc("carry_b", [128, 8, 1], BF16)
        z = P.alloc("z", [128, 8, 512], BF16)
        hr = [P.alloc(f"hr{i}", [128, 512], F32) for i in range(2)]
        cw = VCOLS["sc_conv"]
        vt = self.vt
        ps_ss = self.ps[0]
        ps_in = self.ps[1:5]
        ps_dn = self.ps[5:8]
        cnt = {"u": 0, "p": 0, "d": 0, "h": 0}

        def inproj(pt, col0, W):
            for c in range(8):
                P.op("pe", lambda e, c=c: e.matmul(pt[:, 0:W], wi[:, c, col0:col0 + 128], a[:, c, 0:W], start=(c == 0), stop=(c == 7)), reads=[wi, a.sub(c)], writes=[pt])

        def body(t0, W, flush):
            for i in range(8):
                cut = cu[cnt["u"] % 2]
                ust = us[cnt["u"] % 2]
                bbt = bb[cnt["u"] % 2]
                y = yc[cnt["u"] % 2]
                cnt["u"] += 1
                P.op("pool", lambda e, cut=cut, i=i: e.tensor_copy(cut[:, 0:2], carry_cu[:, i, :]), reads=[carry_cu.sub(i)], writes=[cut])
                P.op("pool", lambda e, bbt=bbt, i=i: e.tensor_copy(bbt[:, 0:1], carry_b[:, i, :]), reads=[carry_b.sub(i)], writes=[bbt])
                if flush:
                    P.op("pool", lambda e, cut=cut: e.memset(cut[:, 2:3], 0.0), writes=[cut])
                else:
                    pb, pc, pu = (ps_in[(cnt["p"] + j) % 4] for j in range(3))
                    cnt["p"] += 3
                    inproj(pb, i * 128, W)
                    inproj(pc, D + i * 128, W)
                    inproj(pu, 2 * D + i * 128, W)
                    P.op("act", lambda e, ust=ust, pu=pu: e.activation(out=ust[:, 0:W], in_=pu[:, 0:W], func=AF.Copy), reads=[pu], writes=[ust])
                    P.op("act", lambda e, bbt=bbt, pb=pb: e.activation(out=bbt[:, 1:1 + W], in_=pb[:, 0:W], func=AF.Copy), reads=[pb], writes=[bbt])
                    P.op("dve", lambda e, cut=cut, pc=pc, ust=ust: e.tensor_tensor(out=cut[:, 2:2 + W], in0=pc[:, 0:W], in1=ust[:, 0:W], op=ALU.mult), reads=[pc, ust], writes=[cut])
                    P.op("pool", lambda e, cut=cut, i=i: e.tensor_copy(carry_cu[:, i, :], cut[:, W:W + 2]), reads=[cut], writes=[carry_cu.sub(i)])
                    P.op("pool", lambda e, bbt=bbt, i=i: e.tensor_copy(carry_b[:, i, :], bbt[:, W:W + 1]), reads=[bbt], writes=[carry_b.sub(i)])
                P.op("dve", lambda e, cut=cut, y=y, i=i: e.tensor_scalar(y[:, 0:W], cut[:, 0:W], vt[:, cw + i:cw + i + 1], None, op0=ALU.mult), reads=[cut, vt], writes=[y])
                P.op("dve", lambda e, cut=cut, y=y, i=i: e.scalar_tensor_tensor(out=y[:, 0:W], in0=cut[:, 1:1 + W], scalar=vt[:, cw + 8 + i:cw + 8 + i + 1], in1=y[:, 0:W], op0=ALU.mult, op1=ALU.add), reads=[cut, vt, y], writes=[y])
                P.op("dve", lambda e, cut=cut, y=y, i=i: e.scalar_tensor_tensor(out=y[:, 0:W], in0=cut[:, 2:2 + W], scalar=vt[:, cw + 16 + i:cw + 16 + i + 1], in1=y[:, 0:W], op0=ALU.mult, op1=ALU.add), reads=[cut, vt, y], writes=[y])
                P.op("dve", lambda e, bbt=bbt, y=y, i=i: e.tensor_tensor(out=z[:, i, 0:W], in0=y[:, 0:W], in1=bbt[:, 0:W], op=ALU.mult), reads=[y, bbt], writes=[z.sub(i)])

        def seq(hsrc, hdst, T_, s):
            P.op("pool", lambda e: e.memset(carry_cu[:], 0.0), writes=[carry_cu])
            P.op("pool", lambda e: e.memset(carry_b[:], 0.0), writes=[carry_b])
            tiles = [(t0, min(512, T_ - t0), False) for t0 in range(0, T_, 512)] + [(T_, 1, True)]
            self.norm_mod(hsrc, 0, tiles[0][1], self.gsm, l, 0, s, a, ht, sq, rs, ps_ss)
            for j, (t0, W, flush) in enumerate(tiles):
                body(t0, W, flush)
                if j + 1 < len(tiles) and not tiles[j + 1][2]:
                    self.norm_mod(hsrc, tiles[j + 1][0], tiles[j + 1][1], self.gsm, l, 0, s, a, ht, sq, rs, ps_ss)
                self.proj_res(hsrc, hdst, t0, W, l, s, wo, z, 8, 2, hr, ps_dn, cnt, True)

        seq(src, dst, T_lat, 0)
        seq(csrc, cdst, CTX, 1)
        P.barrier()
        P.release(m)

    def load_qkv_weights(self, w_qkv, w_o, hd, nq):
        P = self.P
        nk = 1536 - nq - (1536 - nq) // 2
        wq = P.alloc("wq", [128, 8, nq], BF16)
        wqs = P.alloc("wqs", [128, 8, nq], BF16)
        wk = P.alloc("wk", [128, 8, 256], BF16)
        wks = P.alloc("wks", [128, 8, 256], BF16)
        wv = P.alloc("wv", [128, 8, 256], BF16)
        wo = P.alloc("wo", [128, 8, D], BF16)
        m2 = P.mark()
        stage = [P.alloc(f"stg{i}", [128, 2048], F32) for i in range(2)]
        h2 = hd // 2
        for kc in range(8):
            st = stage[kc % 2]
            P.dma("sp", st[:, 0:1536], w_qkv[kc * 128:(kc + 1) * 128, :], writes=[st])
            P.op("dve", lambda e, st=st, kc=kc: e.tensor_copy(wq[:, kc, :], st[:, 0:nq]), reads=[st], writes=[wq.sub(kc)])
            P.op("pool", lambda e, st=st, kc=kc: e.tensor_copy(wk[:, kc, :], st[:, nq:nq + 256]), reads=[st], writes=[wk.sub(kc)])
            P.op("pool", lambda e, st=st, kc=kc: e.tensor_copy(wv[:, kc, :], st[:, nq + 256:nq + 512]), reads=[st], writes=[wv.sub(kc)])
            for (dstw, c0, n) in ((wqs, 0, nq), (wks, nq, 256)):
                for t in range(2):
                    P.op("dve" if t == 0 else "pool",
                         lambda e, st=st, kc=kc, dstw=dstw, c0=c0, n=n, t=t: e.tensor_copy(
                             dstw[:, kc, :].rearrange("p (h t d) -> p h t d", t=2, d=h2)[:, :, t, :],
                             st[:, c0:c0 + n].rearrange("p (h t d) -> p h t d", t=2, d=h2)[:, :, 1 - t, :]),
                         reads=[st], writes=[dstw.sub((kc, t))])
        k = 0
        for kc in range(8):
            k = self.load_cast(wo, kc, w_o[kc * 128:(kc + 1) * 128, :], D, stage, k)
        P.barrier()
        P.release(m2)
        return wq, wqs, wk, wks, wv, wo

    def qk_norm_rope(self, pq, pqs, gcol, gscol, hd, cost, sint, W, out_ap, out_buf, bufs, ps_ss2):
        P = self.P
        sqb, rsb, t1, t2 = bufs
        onesm = self.ones if hd == 128 else self.ones_bd
        vt = self.vt
        P.op("act", lambda e: e.activation(out=sqb[:, 0:W], in_=pq[:, 0:W], func=AF.Square), reads=[pq], writes=[sqb])
        P.op("pe", lambda e: e.matmul(ps_ss2[:, 0:W], onesm[:], sqb[:, 0:W], start=True, stop=True), reads=[onesm, sqb], writes=[ps_ss2])
        P.op("act", lambda e: e.activation(out=rsb[:, 0:W], in_=ps_ss2[:, 0:W], func=AF.Sqrt, bias=self.epsb[:], scale=1.0 / hd), reads=[ps_ss2, self.epsb], writes=[rsb])
        P.op("dve", lambda e: e.reciprocal(rsb[:, 0:W], rsb[:, 0:W]), reads=[rsb], writes=[rsb])
        P.op("dve", lambda e: e.scalar_tensor_tensor(out=t1[:, 0:W], in0=pq[:, 0:W], scalar=vt[:, gcol:gcol + 1], in1=rsb[:, 0:W], op0=ALU.mult, op1=ALU.mult), reads=[pq, vt, rsb], writes=[t1])
        P.op("dve", lambda e: e.scalar_tensor_tensor(out=t2[:, 0:W], in0=pqs[:, 0:W], scalar=vt[:, gscol:gscol + 1], in1=rsb[:, 0:W], op0=ALU.mult, op1=ALU.mult), reads=[pqs, vt, rsb], writes=[t2])
        P.op("dve", lambda e: e.tensor_tensor(out=t1[:, 0:W], in0=t1[:, 0:W], in1=cost[:, 0:W], op=ALU.mult), reads=[t1, cost], writes=[t1])
        P.op("dve", lambda e: e.tensor_tensor(out=t2[:, 0:W], in0=t2[:, 0:W], in1=sint[:, 0:W], op=ALU.mult), reads=[t2, sint], writes=[t2])
        P.op("dve", lambda e: e.tensor_tensor(out=out_ap, in0=t1[:, 0:W], in1=t2[:, 0:W], op=ALU.add), reads=[t1, t2], writes=[out_buf])

    def mixer_dense(self, l, src, dst, csrc, cdst, T_lat=S):
        P = self.P
        m = P.mark()
        HD = 128
        NKB = (T_lat + CTX) // 128
        wq, wqs, wk, wks, wv, wo = self.load_qkv_weights(self.ax_w_qkv, self.ax_w_o, HD, 1024)
        KT = P.alloc("KT", [128, 2, T_lat + CTX], BF16)
        V = P.alloc("V", [128, NKB, 256], BF16)
        ht = P.alloc("ht", [128, 8, 512], F32)
        sq = [P.alloc(f"sq{i}", [128, 512], BF16) for i in range(2)]
        rs = P.alloc("rs", [128, 512], F32)
        a = P.alloc("a", [128, 8, 512], BF16)
        cost = P.alloc("cost", [128, 512], F32)
        sint = P.alloc("sint", [128, 512], F32)
        nbs = [(P.alloc(f"nsq{i}", [128, 512], BF16), P.alloc(f"nrs{i}", [128, 512], F32), P.alloc(f"nt1{i}", [128, 512], F32), P.alloc(f"nt2{i}", [128, 512], F32)) for i in range(2)]
        nb = nbs[0]
        Q = P.alloc("Q", [128, 8, 512], BF16)
        Ew = [P.alloc(f"Ew{i}", [128, 1024], BF16) for i in range(3)]
        Esum = [P.alloc(f"Esum{i}", [128, 512], BF16) for i in range(2)]
        rden = rs
        att = P.alloc("att", [128, 8, 512], BF16)
        hr = [P.alloc(f"hr{i}", [128, 512], F32) for i in range(2)]
        vt = self.vt
        ps = self.ps
        gq, gqs, gk, gks = VCOLS["ax_qg"], VCOLS["ax_qgs"], VCOLS["ax_kg"], VCOLS["ax_kgs"]
        cnt = {"d": 0, "h": 0, "e": 0, "s": 0, "o": 0, "p": 0, "v": 0}

        def proj(pt, w, col0, W):
            for c in range(8):
                P.op("pe", lambda e, c=c: e.matmul(pt[:, 0:W], w[:, c, col0:col0 + 128], a[:, c, 0:W], start=(c == 0), stop=(c == 7)), reads=[w, a.sub(c)], writes=[pt])

        def load_tables(pos0, W):
            P.dma("sp", cost[:, 0:W], self.cos128[:, pos0:pos0 + W], writes=[cost])
            P.dma("sp", sint[:, 0:W], self.sin128[:, pos0:pos0 + W], writes=[sint])

        def phase_a(hsrc, T_, s, pos_base):
            for t0 in range(0, T_, 512):
                W = min(512, T_ - t0)
                pos0 = pos_base + t0
                self.norm_mod(hsrc, t0, W, self.gsm, l, 0, s, a, ht, sq, rs, ps[0])
                load_tables((S if s == 1 else 0) + t0, W)
                for g in range(2):
                    pk, pks = ps[1 + 2 * (cnt["p"] % 2)], ps[2 + 2 * (cnt["p"] % 2)]
                    cnt["p"] += 1
                    proj(pk, wk, g * 128, W)
                    proj(pks, wks, g * 128, W)
                    self.qk_norm_rope(pk, pks, gk, gks, HD, cost, sint, W, KT[:, g, pos0:pos0 + W], KT.sub((g, pos0)), nbs[g % 2], ps[5])
                for blk in range(W // 128):
                    pv = ps[6 + cnt["v"] % 2]
                    cnt["v"] += 1
                    for c in range(8):
                        P.op("pe", lambda e, c=c, blk=blk, pv=pv: e.matmul(pv[:, 0:256], a[:, c, blk * 128:(blk + 1) * 128], wv[:, c, :], start=(c == 0), stop=(c == 7)), reads=[wv, a.sub(c)], writes=[pv])
                    kb = pos0 // 128 + blk
                    P.op("act", lambda e, pv=pv, kb=kb: e.activation(out=V[:, kb, :], in_=pv[:, 0:256], func=AF.Copy), reads=[pv], writes=[V.sub(kb)])

        phase_a(src, T_lat, 0, 0)
        phase_a(csrc, CTX, 1, T_lat)

        scale = float(HD) ** -0.5

        def phase_b(hsrc, hdst, T_, s, pos_base, kblocks):
            for t0 in range(0, T_, 512):
                W = min(512, T_ - t0)
                pos0 = pos_base + t0
                self.norm_mod(hsrc, t0, W, self.gsm, l, 0, s, a, ht, sq, rs, ps[0])
                load_tables((S if s == 1 else 0) + t0, W)
                for h in range(8):
                    pa_, pb_ = (ps[1], ps[2]) if h % 2 == 0 else (ps[3], ps[4])
                    proj(pa_, wq, h * 128, W)
                    proj(pb_, wqs, h * 128, W)
                    self.qk_norm_rope(pa_, pb_, gq, gqs, HD, cost, sint, W, Q[:, h, 0:W], Q.sub(h), nbs[h % 2], ps[0] if h % 2 == 0 else ps[5])
                assert len(kblocks) % 2 == 0
                npair = len(kblocks) // 2
                items = [(h, jp) for h in range(8) for jp in range(npair)]
                banks = {}

                def issue_qk(n):
                    h, jp = items[n]
                    wide = 1 + n % 2
                    for half in range(2):
                        kb = kblocks[2 * jp + half]
                        pS = ps[2 * wide + half]
                        P.op("pe", lambda e, kb=kb, pS=pS, h=h: e.matmul(pS[:, 0:W], KT[:, h // 4, kb * 128:(kb + 1) * 128], Q[:, h, 0:W], start=True, stop=True), reads=[KT, Q.sub(h)], writes=[pS])
                    return wide

                w_next = issue_qk(0)
                for n, (h, jp) in enumerate(items):
                    g = h // 4
                    if jp == 0:
                        banks[h] = (ps[6 + h % 2], ps[h % 2])
                    po, pden = banks[h]
                    wide = w_next
                    Et = Ew[n % 3]
                    if W == 512:
                        P.op("act", lambda e, wide=wide, Et=Et: e.activation(out=Et[:, :], in_=self.psw[wide][:, :], func=AF.Exp, scale=scale), reads=[ps[2 * wide], ps[2 * wide + 1]], writes=[Et])
                    else:
                        for half in range(2):
                            P.op("act", lambda e, wide=wide, Et=Et, half=half: e.activation(out=Et[:, half * 512:half * 512 + W], in_=ps[2 * wide + half][:, 0:W], func=AF.Exp, scale=scale), reads=[ps[2 * wide + half]], writes=[Et])
                    if n + 1 < len(items):
                        w_next = issue_qk(n + 1)
                    st, sp_ = (jp == 0), (jp == npair - 1)
                    for half in range(2):
                        kb = kblocks[2 * jp + half]
                        P.op("pe", lambda e, kb=kb, Et=Et, po=po, g=g, half=half, st=st, sp_=sp_: e.matmul(po[:, 0:W], V[:, kb, g * 128:(g + 1) * 128], Et[:, half * 512:half * 512 + W], start=(st and half == 0), stop=(sp_ and half == 1)), reads=[V, Et], writes=[po])
                    for half in range(2):
                        P.op("pe", lambda e, Et=Et, pden=pden, half=half, st=st, sp_=sp_: e.matmul(pden[:, 0:W], self.ones[:], Et[:, half * 512:half * 512 + W], start=(st and half == 0), stop=(sp_ and half == 1)), reads=[self.ones, Et], writes=[pden])
                    if sp_:
                        P.op("dve", lambda e, pden=pden: e.reciprocal(rden[:, 0:W], pden[:, 0:W]), reads=[pden], writes=[rden])
                        P.op("dve", lambda e, po=po, h=h: e.tensor_tensor(out=att[:, h, 0:W], in0=po[:, 0:W], in1=rden[:, 0:W], op=ALU.mult), reads=[po, rden], writes=[att.sub(h)])
                self.proj_res(hsrc, hdst, t0, W, l, s, wo, att, 8, 2, hr, [ps[1], ps[2]], cnt, False)

        phase_b(src, dst, T_lat, 0, 0, list(range(NKB)))
        phase_b(csrc, cdst, CTX, 1, T_lat, list(range(T_lat // 128, NKB)))
        P.barrier()
        P.release(m)

    def mixer_win(self, l, src, dst, csrc, cdst, T_lat=S):
        P = self.P
        m = P.mark()
        HD = 64
        NKB = (T_lat + CTX) // 128
        NLB = T_lat // 128
        wq = P.alloc("wq", [128, 8, 1024], BF16)
        wqs = P.alloc("wqs", [128, 8, 1024], BF16)
        wk = P.alloc("wk", [128, 8, 256], BF16)
        wks = P.alloc("wks", [128, 8, 256], BF16)
        wv = P.alloc("wv", [128, 8, 256], BF16)
        wo = P.alloc("wo", [128, 8, D], BF16)
        mk = P.alloc("mk", [128, 2, 512], BF16)
        es = P.alloc("es", [128, 16], F32)
        m2 = P.mark()
        stage = [P.alloc(f"stg{i}", [128, 2048], F32) for i in range(2)]
        k = 0
        for kc in range(8):
            st = stage[kc % 2]
            P.dma("sp", st[:, 0:1536], self.win_w_qkv[kc * 128:(kc + 1) * 128, :], writes=[st])
            P.op("pool", lambda e, st=st, kc=kc: e.tensor_copy(wv[:, kc, :], st[:, 1280:1536]), reads=[st], writes=[wv.sub(kc)])
            for (dw, dws, c0, n) in ((wq, wqs, 0, 1024), (wk, wks, 1024, 256)):
                for t in range(2):
                    P.op("dve" if t == 0 else "pool",
                         lambda e, st=st, kc=kc, dw=dw, c0=c0, n=n, t=t: e.tensor_copy(
                             dw[:, kc, :].rearrange("p (i t d) -> p i t d", t=2, d=64)[:, :, t, :],
                             st[:, c0:c0 + n].rearrange("p (t i d) -> p t i d", t=2, d=64)[:, t, :, :]),
                         reads=[st], writes=[dw.sub((kc, t))])
                    for u in range(2):
                        P.op("dve" if u == 0 else "pool",
                             lambda e, st=st, kc=kc, dws=dws, c0=c0, n=n, t=t, u=u: e.tensor_copy(
                                 dws[:, kc, :].rearrange("p (i t u d) -> p i t u d", t=2, u=2, d=32)[:, :, t, u, :],
                                 st[:, c0:c0 + n].rearrange("p (t i u d) -> p t i u d", t=2, u=2, d=32)[:, t, :, 1 - u, :]),
                             reads=[st], writes=[dws.sub((kc, t, u))])
        for kc in range(8):
            k = self.load_cast(wo, kc, self.win_w_o[kc * 128:(kc + 1) * 128, :], D, stage, k)
        st = stage[0]
        P.dma("sp", st[:, 0:1024], self.wmask[:, :], writes=[st])
        P.op("dve", lambda e: e.tensor_copy(mk[:].rearrange("p a b -> p (a b)"), st[:, 0:1024]), reads=[st], writes=[mk])
        sc0 = VCOLS["win_sink"]
        P.op("act", lambda e: e.activation(out=es[:], in_=self.vt[:, sc0:sc0 + 16], func=AF.Exp), reads=[self.vt], writes=[es])
        P.barrier()
        P.release(m2)
        KT = P.alloc("KT", [128, 2, T_lat + CTX], BF16)
        V = P.alloc("V", [128, NKB, 256], BF16)
        ht = P.alloc("ht", [128, 8, 512], F32)
        sq = [P.alloc(f"sq{i}", [128, 512], BF16) for i in range(2)]
        rs = P.alloc("rs", [128, 512], F32)
        a = P.alloc("a", [128, 8, 512], BF16)
        cost = P.alloc("cost", [128, 512], F32)
        sint = P.alloc("sint", [128, 512], F32)
        nbs = [(P.alloc(f"nsq{i}", [128, 512], BF16), P.alloc(f"nrs{i}", [128, 512], F32), P.alloc(f"nt1{i}", [128, 512], F32), P.alloc(f"nt2{i}", [128, 512], F32)) for i in range(2)]
        nb = nbs[0]
        Q = P.alloc("Q", [128, 8, 512], BF16)
        E = [P.alloc(f"E{i}", [128, 512], BF16) for i in range(3)]
        dtmp = P.alloc("dtmp", [128, 512], F32)
        rden = dtmp
        att = P.alloc("att", [128, 8, 512], BF16)
        hr = [P.alloc(f"hr{i}", [128, 512], F32) for i in range(2)]
        ps = self.ps
        gq, gqs, gk, gks = VCOLS["win_qg"], VCOLS["win_qgs"], VCOLS["win_kg"], VCOLS["win_kgs"]
        cnt = {"d": 0, "h": 0, "e": 0, "s": 0, "o": 0, "p": 0, "v": 0}

        def proj(pt, w, col0, W):
            for c in range(8):
                P.op("pe", lambda e, c=c: e.matmul(pt[:, 0:W], w[:, c, col0:col0 + 128], a[:, c, 0:W], start=(c == 0), stop=(c == 7)), reads=[w, a.sub(c)], writes=[pt])

        def load_tables(pos0, W):
            P.dma("sp", cost[:, 0:W], self.cos64[:, pos0:pos0 + W], writes=[cost])
            P.dma("sp", sint[:, 0:W], self.sin64[:, pos0:pos0 + W], writes=[sint])

        def phase_a(hsrc, T_, s, pos_base):
            for t0 in range(0, T_, 512):
                W = min(512, T_ - t0)
                pos0 = pos_base + t0
                self.norm_mod(hsrc, t0, W, self.gsm, l, 0, s, a, ht, sq, rs, ps[0])
                load_tables((S if s == 1 else 0) + t0, W)
                for j in range(2):
                    pk, pks = ps[1 + 2 * (cnt["p"] % 2)], ps[2 + 2 * (cnt["p"] % 2)]
                    cnt["p"] += 1
                    proj(pk, wk, j * 128, W)
                    proj(pks, wks, j * 128, W)
                    self.qk_norm_rope(pk, pks, gk, gks, HD, cost, sint, W, KT[:, j, pos0:pos0 + W], KT.sub((j, pos0)), nbs[j % 2], ps[5])
                for blk in range(W // 128):
                    pv = ps[6 + cnt["v"] % 2]
                    cnt["v"] += 1
                    for c in range(8):
                        P.op("pe", lambda e, c=c, blk=blk, pv=pv: e.matmul(pv[:, 0:256], a[:, c, blk * 128:(blk + 1) * 128], wv[:, c, :], start=(c == 0), stop=(c == 7)), reads=[wv, a.sub(c)], writes=[pv])
                    kb = pos0 // 128 + blk
                    P.op("act", lambda e, pv=pv, kb=kb: e.activation(out=V[:, kb, :], in_=pv[:, 0:256], func=AF.Copy), reads=[pv], writes=[V.sub(kb)])

        phase_a(src, T_lat, 0, 0)
        phase_a(csrc, CTX, 1, T_lat)
        scale = float(HD) ** -0.5

        def phase_b(hsrc, hdst, T_, s, pos_base, windowed):
            for t0 in range(0, T_, 512):
                W = min(512, T_ - t0)
                self.norm_mod(hsrc, t0, W, self.gsm, l, 0, s, a, ht, sq, rs, ps[0])
                load_tables((S if s == 1 else 0) + t0, W)
                for i in range(8):
                    pa_, pb_ = (ps[1], ps[2]) if i % 2 == 0 else (ps[3], ps[4])
                    proj(pa_, wq, i * 128, W)
                    proj(pb_, wqs, i * 128, W)
                    self.qk_norm_rope(pa_, pb_, gq, gqs, HD, cost, sint, W, Q[:, i, 0:W], Q.sub(i), nbs[i % 2], ps[0] if i % 2 == 0 else ps[5])
                items = []
                for qbl in range(W // 128):
                    if windowed:
                        qb = t0 // 128 + qbl
                        kbl = []
                        if qb > 0:
                            kbl.append((qb - 1, 0))
                        kbl.append((qb, None))
                        if qb < NLB - 1:
                            kbl.append((qb + 1, 1))
                        kbl += [(NLB, None), (NLB + 1, None)]
                    else:
                        kbl = [(NLB, None), (NLB + 1, None)]
                    for g in range(4):
                        for n_, (kb, mi) in enumerate(kbl):
                            items.append((qbl, g, kb, mi, n_ == 0, n_ == len(kbl) - 1))

                def issue_qk(n):
                    qbl, g, kb, mi, _, _ = items[n]
                    qs = slice(qbl * 128, (qbl + 1) * 128)
                    half, j, i0 = g // 2, g % 2, 4 * (g % 2)
                    hp = slice(half * 64, (half + 1) * 64)
                    pS = ps[2 + n % 3]
                    P.op("pe", lambda e, kb=kb, pS=pS, hp=hp, j=j, i0=i0, qs=qs: e.matmul(pS[:, 0:512], KT[hp, j, kb * 128:(kb + 1) * 128], Q[hp, i0:i0 + 4, qs], start=True, stop=True),
                         reads=[KT, Q], writes=[pS])
                    return pS

                pS_next = issue_qk(0)
                grp = 0
                for n, (qbl, g, kb, mi, st_, sp_) in enumerate(items):
                    qs = slice(qbl * 128, (qbl + 1) * 128)
                    if st_:
                        po = ps[5 + grp % 2]
                        pden = ps[7] if grp % 2 == 0 else ps[0]
                        grp += 1
                    pS = pS_next
                    Et = E[n % 3]
                    P.op("act", lambda e, pS=pS, Et=Et: e.activation(out=Et[:, :], in_=pS[:, 0:512], func=AF.Exp, scale=scale), reads=[pS], writes=[Et])
                    if n + 1 < len(items):
                        pS_next = issue_qk(n + 1)
                    if mi is not None:
                        P.op("dve", lambda e, Et=Et, mi=mi: e.tensor_tensor(out=Et[:, :], in0=Et[:, :], in1=mk[:, mi, :], op=ALU.mult), reads=[Et, mk], writes=[Et])
                    Ev = Et[:, :].rearrange("p (r q) -> p r q", q=128)
                    for par in range(2):
                        P.op("pe", lambda e, kb=kb, Ev=Ev, po=po, g=g, par=par, st_=st_, sp_=sp_: e.matmul(po[par * 64:(par + 1) * 64, 0:256], V[:, kb, g * 64:(g + 1) * 64], Ev[:, par::2, :], start=st_, stop=sp_, tile_position=(0, par * 64)),
                             reads=[V, Et], writes=[po])
                    P.op("pe", lambda e, Et=Et, pden=pden, st_=st_, sp_=sp_: e.matmul(pden[:, 0:512], self.ones[:], Et[:, :], start=st_, stop=sp_), reads=[self.ones, Et], writes=[pden])
                    if sp_:
                        for r in range(4):
                            hh = 4 * g + r
                            P.op("dve", lambda e, pden=pden, r=r, hh=hh: e.tensor_scalar(dtmp[:, r * 128:(r + 1) * 128], pden[:, r * 128:(r + 1) * 128], es[:, hh:hh + 1], None, op0=ALU.add), reads=[pden, es], writes=[dtmp])
                        P.op("dve", lambda e: e.reciprocal(rden[:, :], dtmp[:, :]), reads=[dtmp], writes=[rden])
                        for r in range(4):
                            rp = slice((r % 2) * 64, (r % 2 + 1) * 64)
                            cb = (r // 2) * 128
                            P.op("dve", lambda e, po=po, r=r, rp=rp, cb=cb, g=g, qs=qs: e.tensor_tensor(out=att[rp, 2 * g + r // 2, qs], in0=po[rp, cb:cb + 128], in1=rden[rp, r * 128:(r + 1) * 128], op=ALU.mult),
                                 reads=[po, rden], writes=[att.sub(2 * g + r // 2)])
                self.proj_res(hsrc, hdst, t0, W, l, s, wo, att, 8, 2, hr, [ps[1], ps[2]], cnt, False)

        phase_b(src, dst, T_lat, 0, 0, True)
        phase_b(csrc, cdst, CTX, 1, T_lat, False)
        P.barrier()
        P.release(m)

    def mixer_mlstm(self, l, src, dst, csrc, T_lat=S):
        P = self.P
        m = P.mark()
        wi = P.alloc("wi", [128, 8, 3088], BF16)
        wo = P.alloc("wo", [128, 8, D], BF16)
        tri = P.alloc("tri", [128, 2, 128], F32)
        trb = P.alloc("trb", [128, 2, 128], BF16)
        onesf = P.alloc("onesf", [128, 128], F32)
        m2 = P.mark()
        stage = [P.alloc(f"stg{i}", [128, 2048], F32) for i in range(2)]
        k = 0
        for kc in range(8):
            k = self.load_cast(wi, kc, self.ml_w_in[kc * 128:(kc + 1) * 128, :], 3088, stage, k)
        for kc in range(8):
            k = self.load_cast(wo, kc, self.ml_w_out[kc * 128:(kc + 1) * 128, :], D, stage, k)
        P.dma("sp", tri[:].rearrange("p a b -> p (a b)"), self.trimat[:, :], writes=[tri])
        P.op("dve", lambda e: e.tensor_copy(trb[:], tri[:]), reads=[tri], writes=[trb])
        P.op("pool", lambda e: e.memset(onesf[:], 1.0), writes=[onesf])
        P.barrier()
        P.release(m2)
        ht = P.alloc("ht", [128, 8, 512], F32)
        sq = [P.alloc(f"sq{i}", [128, 512], BF16) for i in range(2)]
        rs = P.alloc("rs", [128, 512], F32)
        a = P.alloc("a", [128, 8, 512], BF16)
        QT = P.alloc("QT", [128, 4, 512], BF16)
        KT = P.alloc("KT", [128, 4, 512], BF16)
        Ktm = P.alloc("Ktm", [128, 4, 512], F32)
        Vtm = P.alloc("Vtm", [128, 4, 1024], BF16)
        gtm = P.alloc("gtm", [128, 4, 16], F32)
        sgo = P.alloc("sgo", [128, 8, 512], BF16)
        hb = P.alloc("hb", [128, 8, 512], F32)
        hfT = P.alloc("hfT", [128, 8, 512], F32)
        z = P.alloc("z", [128, 8, 512], BF16)
        hr = [P.alloc(f"hr{i}", [128, 512], F32) for i in range(2)]
        e1 = P.alloc("e1", [128, 4], F32)
        lf = P.alloc("lf", [128, 4], F32)
        bcol = P.alloc("bcol", [128, 4], F32)
        wl = P.alloc("wl", [128, 4], F32)
        wcol = P.alloc("wcol", [128, 4], F32)
        eG = P.alloc("eG", [128, 4], F32)
        Rh = [P.alloc(f"Rh{i}", [128, 128], F32) for i in range(2)]
        Ah = [P.alloc(f"Ah{i}", [128, 128], BF16) for i in range(2)]
        ech = [P.alloc(f"ech{i}", [128, 128], F32) for i in range(2)]
        Ph = [P.alloc(f"Ph{i}", [128, 128], BF16) for i in range(2)]
        Qe = [P.alloc(f"Qe{i}", [128, 128], BF16) for i in range(2)]
        Kw = [P.alloc(f"Kw{i}", [128, 128], BF16) for i in range(2)]
        ad = [P.alloc(f"ad{i}", [128, 128], F32) for i in range(2)]
        Cst = P.alloc("Cst", [128, 4, 256], F32)
        Cb = P.alloc("Cb", [128, 4, 256], BF16)
        nst = P.alloc("nst", [128, 4, 128], F32)
        nbb = P.alloc("nbb", [128, 4, 128], BF16)
        vt = self.vt
        ps = self.ps
        cnt = {"d": 0, "h": 0, "p": 0, "r": 0}
        bg = VCOLS["ml_bg"]
        hn = VCOLS["ml_hn"]
        qscale = 128.0 ** -0.5

        def pp():
            t = ps[1 + cnt["p"] % 2]
            cnt["p"] += 1
            return t

        def project(W, with_o):
            nch = W // 128
            for h in range(4):
                for (dstT, c0, sc) in ((QT, 0, qscale), (KT, 512, 1.0)):
                    pt = pp()
                    for c in range(8):
                        P.op("pe", lambda e, c=c, pt=pt, c0=c0, h=h: e.matmul(pt[:, 0:W], wi[:, c, c0 + h * 128:c0 + (h + 1) * 128], a[:, c, 0:W], start=(c == 0), stop=(c == 7)), reads=[wi, a.sub(c)], writes=[pt])
                    P.op("act", lambda e, pt=pt, dstT=dstT, h=h, sc=sc: e.activation(out=dstT[:, h, 0:W], in_=pt[:, 0:W], func=AF.Copy, scale=sc), reads=[pt], writes=[dstT.sub(h)])
            for ch in range(nch):
                cs = slice(ch * 128, (ch + 1) * 128)
                pt = pp()
                for c in range(8):
                    P.op("pe", lambda e, c=c, pt=pt, cs=cs: e.matmul(pt[:, 0:512], a[:, c, cs], wi[:, c, 512:1024], start=(c == 0), stop=(c == 7)), reads=[wi, a.sub(c)], writes=[pt])
                P.op("act", lambda e, pt=pt, ch=ch: e.activation(out=Ktm[:, ch, :], in_=pt[:, 0:512], func=AF.Copy), reads=[pt], writes=[Ktm.sub(ch)])
                for half in range(2):
                    pt = pp()
                    for c in range(8):
                        P.op("pe", lambda e, c=c, pt=pt, cs=cs, half=half: e.matmul(pt[:, 0:512], a[:, c, cs], wi[:, c, 1024 + half * 512:1536 + half * 512], start=(c == 0), stop=(c == 7)), reads=[wi, a.sub(c)], writes=[pt])
                    P.op("act", lambda e, pt=pt, ch=ch, half=half: e.activation(out=Vtm[:, ch, half * 512:(half + 1) * 512], in_=pt[:, 0:512], func=AF.Copy), reads=[pt], writes=[Vtm.sub(ch)])
                pt = pp()
                for c in range(8):
                    P.op("pe", lambda e, c=c, pt=pt, cs=cs: e.matmul(pt[:, 0:16], a[:, c, cs], wi[:, c, 3072:3088], start=(c == 0), stop=(c == 7)), reads=[wi, a.sub(c)], writes=[pt])
                P.op("dve", lambda e, pt=pt, ch=ch: e.tensor_tensor(out=gtm[:, ch, :], in0=pt[:, 0:16], in1=vt[:, bg:bg + 16], op=ALU.add), reads=[pt, vt], writes=[gtm.sub(ch)])
            if with_o:
                for c8 in range(8):
                    pt = pp()
                    for c in range(8):
                        P.op("pe", lambda e, c=c, pt=pt, c8=c8: e.matmul(pt[:, 0:W], wi[:, c, 2048 + c8 * 128:2048 + (c8 + 1) * 128], a[:, c, 0:W], start=(c == 0), stop=(c == 7)), reads=[wi, a.sub(c)], writes=[pt])
                    P.op("act", lambda e, pt=pt, c8=c8: e.activation(out=sgo[:, c8, 0:W], in_=pt[:, 0:W], func=AF.Sigmoid), reads=[pt], writes=[sgo.sub(c8)])

        def core(ch, dirn, with_out, out_tile):
            cs = slice(ch * 128, (ch + 1) * 128)
            g0 = dirn * 8
            psA, pcr, pS, pnum, pden = ps[0], ps[3], ps[4], (ps[5], ps[6]), ps[7]
            P.op("act", lambda e: e.activation(out=e1[:], in_=gtm[:, ch, g0 + 4:g0 + 8], func=AF.Exp, scale=-1.0), reads=[gtm.sub(ch)], writes=[e1])
            P.op("act", lambda e: e.activation(out=e1[:], in_=e1[:], func=AF.Ln, bias=1.0, scale=1.0), reads=[e1], writes=[e1])
            P.op("dve", lambda e: e.tensor_scalar(lf[:], e1[:], -1.0, None, op0=ALU.mult), reads=[e1], writes=[lf])
            P.op("pe", lambda e: e.matmul(psA[:, 0:4], tri[:, dirn, :], lf[:], start=True, stop=True), reads=[tri, lf], writes=[psA])
            P.op("pe", lambda e: e.matmul(psA[:, 4:8], onesf[:], lf[:], start=True, stop=True), reads=[onesf, lf], writes=[psA])
            P.op("dve", lambda e: e.tensor_tensor(out=bcol[:], in0=gtm[:, ch, g0:g0 + 4], in1=psA[:, 0:4], op=ALU.subtract), reads=[gtm.sub(ch), psA], writes=[bcol])
            P.op("dve", lambda e: e.tensor_tensor(out=wl[:], in0=bcol[:], in1=psA[:, 4:8], op=ALU.add), reads=[bcol, psA], writes=[wl])
            P.op("act", lambda e: e.activation(out=wcol[:], in_=wl[:], func=AF.Exp), reads=[wl], writes=[wcol])
            P.op("act", lambda e: e.activation(out=eG[:], in_=psA[:, 4:8], func=AF.Exp), reads=[psA], writes=[eG])
            for h in range(4):
                i2 = cnt["r"] % 2
                cnt["r"] += 1
                R, A, ec, Pm, Qm, Kwm, adm = Rh[i2], Ah[i2], ech[i2], Ph[i2], Qe[i2], Kw[i2], ad[i2]
                hs_ = slice(h * 128, (h + 1) * 128)
                P.op("dve", lambda e, R=R, h=h: e.tensor_scalar(R[:], tri[:, dirn, :], lf[:, h:h + 1], None, op0=ALU.mult), reads=[tri, lf], writes=[R])
                P.op("pe", lambda e, R=R, hs_=hs_: e.matmul(pcr[:, hs_], onesf[:], R[:], start=True, stop=True), reads=[onesf, R], writes=[pcr])
                P.op("act", lambda e, A=A, hs_=hs_, h=h: e.activation(out=A[:], in_=pcr[:, hs_], func=AF.Exp, bias=bcol[:, h:h + 1], scale=1.0), reads=[pcr, bcol], writes=[A])
                P.op("act", lambda e, ec=ec, hs_=hs_: e.activation(out=ec[:], in_=pcr[:, hs_], func=AF.Exp), reads=[pcr], writes=[ec])
                P.op("pe", lambda e, h=h, hs_=hs_: e.matmul(pS[:, hs_], KT[:, h, cs], QT[:, h, cs], start=True, stop=True), reads=[KT.sub(h), QT.sub(h)], writes=[pS])
                P.op("dve", lambda e, A=A: e.tensor_tensor(out=A[:], in0=A[:], in1=trb[:, dirn, :], op=ALU.mult), reads=[A, trb], writes=[A])
                P.op("dve", lambda e, A=A, Pm=Pm, hs_=hs_: e.tensor_tensor(out=Pm[:], in0=pS[:, hs_], in1=A[:], op=ALU.mult), reads=[pS, A], writes=[Pm])
                if with_out:
                    P.op("dve", lambda e, Qm=Qm, ec=ec, h=h: e.tensor_tensor(out=Qm[:], in0=QT[:, h, cs], in1=ec[:], op=ALU.mult), reads=[QT.sub(h), ec], writes=[Qm])
                    pn = pnum[h // 2]
                    for vc in range(2):
                        ncol = slice((h % 2) * 256 + vc * 128, (h % 2) * 256 + (vc + 1) * 128)
                        P.op("pe", lambda e, pn=pn, ncol=ncol, h=h, vc=vc, Pm=Pm: e.matmul(pn[:, ncol], Vtm[:, ch, h * 256 + vc * 128:h * 256 + (vc + 1) * 128], Pm[:], start=True, stop=False), reads=[Vtm.sub(ch), Pm], writes=[pn])
                        P.op("pe", lambda e, pn=pn, ncol=ncol, h=h, vc=vc, Qm=Qm: e.matmul(pn[:, ncol], Cb[:, h, vc * 128:(vc + 1) * 128], Qm[:], start=False, stop=True), reads=[Cb.sub(h), Qm], writes=[pn])
                    P.op("pe", lambda e, hs_=hs_, Pm=Pm: e.matmul(pden[:, hs_], self.ones[:], Pm[:], start=True, stop=False), reads=[self.ones, Pm], writes=[pden])
                    P.op("pe", lambda e, hs_=hs_, Qm=Qm, h=h: e.matmul(pden[:, hs_], nbb[:, h, :], Qm[:], start=False, stop=True), reads=[nbb.sub(h), Qm], writes=[pden])
                    P.op("act", lambda e, adm=adm, hs_=hs_: e.activation(out=adm[:], in_=pden[:, hs_], func=AF.Abs), reads=[pden], writes=[adm])
                    P.op("dve", lambda e, adm=adm: e.tensor_scalar_max(adm[:], adm[:], 1.0), reads=[adm], writes=[adm])
                    P.op("dve", lambda e, adm=adm: e.reciprocal(adm[:], adm[:]), reads=[adm], writes=[adm])
                    for vc in range(2):
                        ncol = slice((h % 2) * 256 + vc * 128, (h % 2) * 256 + (vc + 1) * 128)
                        P.op("dve", lambda e, pn=pn, ncol=ncol, adm=adm, h=h, vc=vc: e.tensor_tensor(out=out_tile[:, 2 * h + vc, cs], in0=pn[:, ncol], in1=adm[:], op=ALU.mult), reads=[pn, adm], writes=[out_tile.sub((2 * h + vc, ch))])
                P.op("dve", lambda e, Kwm=Kwm, h=h: e.tensor_scalar(Kwm[:], Ktm[:, ch, h * 128:(h + 1) * 128], wcol[:, h:h + 1], None, op0=ALU.mult), reads=[Ktm.sub(ch), wcol], writes=[Kwm])
                pc = ps[1 + h // 2]
                ccol = slice((h % 2) * 256, (h % 2 + 1) * 256)
                P.op("pe", lambda e, pc=pc, ccol=ccol, Kwm=Kwm, h=h: e.matmul(pc[:, ccol], Kwm[:], Vtm[:, ch, h * 256:(h + 1) * 256], start=True, stop=True), reads=[Kwm, Vtm.sub(ch)], writes=[pc])
                P.op("pe", lambda e, Kwm=Kwm, hs_=hs_: e.matmul(psA[:, 128 + hs_.start // 4 * 0 + 0:128 + 0 + 128] if False else ps[0][:, 128:256], Kwm[:], self.ones[:], start=True, stop=True), reads=[Kwm, self.ones], writes=[psA])
                P.op("dve", lambda e, pc=pc, ccol=ccol, h=h: e.scalar_tensor_tensor(out=Cst[:, h, :], in0=Cst[:, h, :], scalar=eG[:, h:h + 1], in1=pc[:, ccol], op0=ALU.mult, op1=ALU.add), reads=[Cst.sub(h), eG, pc], writes=[Cst.sub(h)])
                P.op("dve", lambda e, h=h: e.scalar_tensor_tensor(out=nst[:, h, :], in0=nst[:, h, :], scalar=eG[:, h:h + 1], in1=psA[:, 128:256], op0=ALU.mult, op1=ALU.add), reads=[nst.sub(h), eG, psA], writes=[nst.sub(h)])
                P.op("act", lambda e, h=h: e.activation(out=Cb[:, h, :], in_=Cst[:, h, :], func=AF.Copy), reads=[Cst.sub(h)], writes=[Cb.sub(h)])
                P.op("pool", lambda e, h=h: e.tensor_copy(nbb[:, h, :], nst[:, h, :]), reads=[nst.sub(h)], writes=[nbb.sub(h)])

        def reset_state():
            P.op("pool", lambda e: e.memset(Cst[:], 0.0), writes=[Cst])
            P.op("pool", lambda e: e.memset(nst[:], 0.0), writes=[nst])
            P.op("pool", lambda e: e.memset(Cb[:], 0.0), writes=[Cb])
            P.op("pool", lambda e: e.memset(nbb[:], 0.0), writes=[nbb])

        reset_state()
        self.norm_mod(csrc, 0, CTX, self.gsm, l, 0, 1, a, ht, sq, rs, ps[0])
        project(CTX, False)
        for ch in range(2):
            core(ch, 0, False, None)
        for t0 in range(0, T_lat, 512):
            self.norm_mod(src, t0, 512, self.gsm, l, 0, 0, a, ht, sq, rs, ps[0])
            project(512, False)
            for ch in range(4):
                core(ch, 0, True, hb)
            P.dma("sp", self.hf[:, t0:t0 + 512].rearrange("(c p) t -> p c t", p=128), hb[:], reads=[hb], writes=[self.hf.sub(t0)])
        reset_state()
        self.norm_mod(csrc, 0, CTX, self.gsm, l, 0, 1, a, ht, sq, rs, ps[0])
        project(CTX, False)
        for ch in (1, 0):
            core(ch, 1, False, None)
        for t0 in range(T_lat - 512, -1, -512):
            self.norm_mod(src, t0, 512, self.gsm, l, 0, 0, a, ht, sq, rs, ps[0])
            project(512, True)
            P.dma("sp", hfT[:], self.hf[:, t0:t0 + 512].rearrange("(c p) t -> p c t", p=128), reads=[self.hf.sub(t0)], writes=[hfT])
            for ch in (3, 2, 1, 0):
                core(ch, 1, True, hb)
            for h in range(4):
                for vc in range(2):
                    c8 = 2 * h + vc
                    P.op("dve", lambda e, c8=c8: e.tensor_tensor(out=hb[:, c8, :], in0=hb[:, c8, :], in1=hfT[:, c8, :], op=ALU.add), reads=[hb, hfT], writes=[hb])
                    q = sq[vc]
                    P.op("act", lambda e, c8=c8, q=q: e.activation(out=q[:], in_=hb[:, c8, :], func=AF.Square), reads=[hb], writes=[q])
                    P.op("pe", lambda e, q=q, vc=vc: e.matmul(ps[0][:, :], self.ones[:], q[:], start=(vc == 0), stop=(vc == 1)), reads=[self.ones, q], writes=[ps[0]])
                P.op("act", lambda e: e.activation(out=rs[:], in_=ps[0][:, :], func=AF.Sqrt, bias=self.epsb[:], scale=1.0 / 256), reads=[ps[0], self.epsb], writes=[rs])
                P.op("dve", lambda e: e.reciprocal(rs[:], rs[:]), reads=[rs], writes=[rs])
                for vc in range(2):
                    c8 = 2 * h + vc
                    P.op("dve", lambda e, c8=c8, vc=vc: e.scalar_tensor_tensor(out=hb[:, c8, :], in0=hb[:, c8, :], scalar=vt[:, hn + vc:hn + vc + 1], in1=rs[:], op0=ALU.mult, op1=ALU.mult), reads=[hb, vt, rs], writes=[hb])
                    P.op("dve", lambda e, c8=c8: e.tensor_tensor(out=z[:, c8, :], in0=hb[:, c8, :], in1=sgo[:, c8, :], op=ALU.mult), reads=[hb, sgo.sub(c8)], writes=[z.sub(c8)])
            self.proj_res(src, dst, t0, 512, l, 0, wo, z, 8, 2, hr, [ps[1], ps[2]], cnt, False)
        P.barrier()
        P.release(m)

    def copy_dram(self, src, dst, T_):
        P = self.P
        P.dma("sp", dst[:, 0:T_], src[:, 0:T_])
        P.barrier()


def build(test=None):
    k = K(test)
    P = k.P
    k.setup()
    k.adaln()
    if test is None or test in ("full", "fulls"):
        TL = 1024 if test == "fulls" else S
        k.mixer_win(0, k.xT, k.hA, k.ctxT, k.cA, T_lat=TL)
        k.ffn(0, k.hA, k.hB, k.cA, k.cB, True, T_lat=TL)
        k.mixer_sc(1, k.hB, k.hA, k.cB, k.cA, T_lat=TL)
        k.ffn(1, k.hA, k.hB, k.cA, k.cB, True, T_lat=TL)
        k.mixer_dense(2, k.hB, k.hA, k.cB, k.cA, T_lat=TL)
        k.ffn(2, k.hA, k.hB, k.cA, k.cB, True, T_lat=TL)
        k.mixer_mlstm(3, k.hB, k.hA, k.cB, T_lat=TL)
        k.ffn(3, k.hA, k.outT, None, None, False, T_lat=TL)
    if test in ("ffn0", "small"):
        k.ffn(0, k.xT, k.outT, k.ctxT, k.cB, True, T_lat=(S if test == "ffn0" else 1024))
    if test in ("sc1", "sc1s"):
        k.mixer_sc(1, k.xT, k.outT, k.ctxT, k.cB, T_lat=(S if test == "sc1" else 1024))
    if test in ("ax2", "ax2s"):
        k.mixer_dense(2, k.xT, k.outT, k.ctxT, k.cB, T_lat=(S if test == "ax2" else 1024))
    if test in ("win0", "win0s"):
        k.mixer_win(0, k.xT, k.outT, k.ctxT, k.cB, T_lat=(S if test == "win0" else 1024))
    if test in ("ml3", "ml3s"):
        k.mixer_mlstm(3, k.xT, k.outT, k.ctxT, T_lat=(S if test == "ml3" else 1024))
    n = P.emit()
    return k.nc, n


def make_in_maps(inputs, cores):
    vecs = pack_vecs(inputs)
    c128, s128 = rope_tables(128)
    c64, s64 = rope_tables(64)
    ii = np.arange(128)[:, None]
    jj = np.arange(128)[None, :]
    wmask = np.concatenate([np.tile((ii >= jj).astype(np.float32), (1, 4)), np.tile((ii <= jj).astype(np.float32), (1, 4))], axis=1)
    trimat = np.concatenate([(ii <= jj).astype(np.float32), (ii >= jj).astype(np.float32)], axis=1)
    maps = []
    for b in cores:
        cc = np.zeros((128, 8, 2), np.float32)
        cc[:, :, 0] = inputs["c"][b].reshape(8, 128).T
        cc[:, :, 1] = inputs["c_ctx"].reshape(8, 128).T
        maps.append({
            "xT": np.ascontiguousarray(inputs["x"][b].T),
            "ctxT": np.ascontiguousarray(inputs["ctx"][b].T),
            "cc": cc, "vecs": vecs,
            "ada_w": inputs["ada_w"], "ffn_w_up": inputs["ffn_w_up"], "ffn_w_down": inputs["ffn_w_down"],
            "sc_w_in": inputs["sc_w_in"][0], "sc_w_out": inputs["sc_w_out"][0],
            "ax_w_qkv": inputs["ax_w_qkv"][0], "ax_w_o": inputs["ax_w_o"][0],
            "win_w_qkv": inputs["win_w_qkv"][0], "win_w_o": inputs["win_w_o"][0],
            "cos128": c128, "sin128": s128, "cos64": c64, "sin64": s64, "wmask": wmask,
            "ml_w_in": inputs["ml_w_in"][0], "ml_w_out": inputs["ml_w_out"][0], "trimat": trimat,
        })
    return maps


def kernel(**inputs):
    inputs = {k: np.asarray(v) for k, v in inputs.items()}
    nc, _ = build()
    cores = [0, 1, 2, 3]
    res = run_bass_kernel_spmd(nc, make_in_maps(inputs, cores), core_ids=cores)
    out = np.stack([np.ascontiguousarray(res.results[i]["outT"].T) for i in range(4)], axis=0)
    return out.astype(np.float32)
```
